# Optimizing a Trainium2 kernel written in Bass

```python
import math
import jax, jax.numpy as jnp
from jax import lax
import numpy as np

D_MODEL = 2048
BATCH = 2
SEQ = 8192
DEPTH = 1

CHUNK = 64
LN_EPS = 1e-5
NORM_EPS = 1e-6
SSM_GROUP_DIM = 16
SSM_STATE = 64
SSM_WIDTH = D_MODEL // 2
SSM_GROUPS = SSM_WIDTH // SSM_GROUP_DIM
DN_HEAD_DIM = 128
DN_HEADS = D_MODEL // DN_HEAD_DIM
DN_WIDTH = DN_HEADS * DN_HEAD_DIM
CONV_WIDTH = 4
N_EXPERTS = 32
TOP_K = 4
D_FF = D_MODEL
SWIGLU_LIMIT = 7.0
SWIGLU_ALPHA = 1.702
EXPERT_BLOCK = 256
DEEPNORM_ALPHA = (2 * DEPTH) ** 0.25
DEEPNORM_BETA = (8 * DEPTH) ** -0.25
IN_SPLITS = (SSM_WIDTH, DN_WIDTH, DN_WIDTH, DN_WIDTH, DN_WIDTH, DN_HEADS, DN_HEADS, D_MODEL, D_MODEL)
IN_WIDTH = SSM_WIDTH + 4 * DN_WIDTH + 2 * DN_HEADS + 2 * D_MODEL

kernel_name = 'hybrid_s5_gdn_moe_deepnorm'


def split_points():
    return [int(s) for s in np.cumsum(IN_SPLITS)[:-1]]


def layer_norm(x, g, b):
    xf = x.astype(jnp.float32)
    mu = jnp.mean(xf, axis=-1, keepdims=True)
    var = jnp.mean(jnp.square(xf - mu), axis=-1, keepdims=True)
    return ((xf - mu) * lax.rsqrt(var + LN_EPS) * g + b).astype(x.dtype)


def l2_normalize(t):
    return t * lax.rsqrt(jnp.sum(jnp.square(t), axis=-1, keepdims=True) + NORM_EPS)


def s5_mixer(u, lam_re, lam_im, log_step, b_re, b_im, c_re, c_im, d_skip, w_glu, b_glu):
    f32 = jnp.float32
    bsz, seq, _ = u.shape
    uf = u.astype(f32).reshape(bsz, seq, SSM_GROUPS, SSM_GROUP_DIM)
    lr, li = lam_re.astype(f32), lam_im.astype(f32)
    step = jnp.exp(log_step.astype(f32))[:, None]
    mag = jnp.exp(lr * step)
    a_re, a_im = mag * jnp.cos(li * step), mag * jnp.sin(li * step)
    den = lr * lr + li * li
    nr, ni = a_re - 1.0, a_im
    f_re = (nr * lr + ni * li) / den
    f_im = (ni * lr - nr * li) / den
    br, bi = b_re.astype(f32), b_im.astype(f32)
    bb_re = f_re[..., None] * br - f_im[..., None] * bi
    bb_im = f_re[..., None] * bi + f_im[..., None] * br
    bu_re = jnp.einsum('blgh,gph->blgp', uf, bb_re)
    bu_im = jnp.einsum('blgh,gph->blgp', uf, bb_im)
    ar = jnp.broadcast_to(a_re, bu_re.shape)
    ai = jnp.broadcast_to(a_im, bu_re.shape)

    def combine(e1, e2):
        a1r, a1i, b1r, b1i = e1
        a2r, a2i, b2r, b2i = e2
        return (a2r * a1r - a2i * a1i,
                a2r * a1i + a2i * a1r,
                a2r * b1r - a2i * b1i + b2r,
                a2r * b1i + a2i * b1r + b2i)

    _, _, s_re, s_im = lax.associative_scan(combine, (ar, ai, bu_re, bu_im), axis=1)
    y = (jnp.einsum('blgp,ghp->blgh', s_re, c_re.astype(f32))
         - jnp.einsum('blgp,ghp->blgh', s_im, c_im.astype(f32))
         + d_skip.astype(f32) * uf)
    y = jax.nn.gelu(y.reshape(bsz, seq, SSM_WIDTH), approximate=False)
    return y * jax.nn.sigmoid(y @ w_glu.astype(f32) + b_glu.astype(f32))


def causal_depthwise_conv(x, w):
    return lax.conv_general_dilated(x, w.astype(x.dtype), window_strides=(1,),
                                    padding=[(CONV_WIDTH - 1, 0)],
                                    dimension_numbers=('NWC', 'WIO', 'NWC'),
                                    feature_group_count=x.shape[-1])


def gated_delta_rule_chunked(q, k, v, g, beta):
    bsz, seq, heads, dk = q.shape
    dv = v.shape[-1]
    n = seq // CHUNK

    def to_chunks(t):
        t = t.reshape((bsz, n, CHUNK, heads) + t.shape[3:])
        return jnp.moveaxis(t, (1, 3), (0, 2))

    qc, kc, vc = to_chunks(q), to_chunks(k), to_chunks(v)
    gc = jnp.cumsum(to_chunks(g), axis=-1)
    bc = to_chunks(beta)
    idx = jnp.arange(CHUNK)
    causal = idx[:, None] >= idx[None, :]
    strict = idx[:, None] > idx[None, :]
    decay = jnp.exp(jnp.where(causal, gc[..., :, None] - gc[..., None, :], -jnp.inf))
    kb = kc * bc[..., None]
    m = jnp.where(strict, jnp.einsum('nbhcd,nbhsd->nbhcs', kb, kc) * decay, 0.0)
    rhs = jnp.concatenate([vc * bc[..., None], kb * jnp.exp(gc)[..., None]], axis=-1)
    sol = lax.linalg.triangular_solve(m + jnp.eye(CHUNK, dtype=m.dtype), rhs,
                                      left_side=True, lower=True, unit_diagonal=True)
    u_c, w_c = sol[..., :dv], sol[..., dv:]
    qk = jnp.where(causal, jnp.einsum('nbhcd,nbhsd->nbhcs', qc, kc) * decay, 0.0)

    def step(state, xs):
        q_i, k_i, u_i, w_i, g_i, qk_i = xs
        v_new = u_i - jnp.einsum('bhcd,bhde->bhce', w_i, state)
        o_i = (jnp.einsum('bhcd,bhde->bhce', q_i * jnp.exp(g_i)[..., None], state)
               + jnp.einsum('bhcs,bhse->bhce', qk_i, v_new))
        g_last = g_i[..., -1]
        state = (state * jnp.exp(g_last)[..., None, None]
                 + jnp.einsum('bhcd,bhce->bhde', k_i * jnp.exp(g_last[..., None] - g_i)[..., None], v_new))
        return state, o_i

    s0 = jnp.zeros((bsz, heads, dk, dv), jnp.float32)
    _, o = lax.scan(step, s0, (qc, kc, u_c, w_c, gc, qk))
    return jnp.moveaxis(o, (0, 2), (1, 3)).reshape(bsz, seq, heads, dv)


def gated_deltanet_mixer(q, k, v, z, a, b, conv_w, a_log, dt_bias, norm_w):
    f32 = jnp.float32
    bsz, seq, _ = q.shape
    qkv = jax.nn.silu(causal_depthwise_conv(jnp.concatenate([q, k, v], axis=-1), conv_w)).astype(f32)
    qh, kh, vh = jnp.split(qkv, [DN_WIDTH, 2 * DN_WIDTH], axis=-1)
    shape = (bsz, seq, DN_HEADS, DN_HEAD_DIM)
    qh = l2_normalize(qh.reshape(shape)) * (DN_HEAD_DIM ** -0.5)
    kh = l2_normalize(kh.reshape(shape))
    vh = vh.reshape(shape)
    beta = jax.nn.sigmoid(b.astype(f32))
    g = -jnp.exp(a_log.astype(f32)) * jax.nn.softplus(a.astype(f32) + dt_bias.astype(f32))
    o = gated_delta_rule_chunked(qh, kh, vh, g, beta)
    o = o * lax.rsqrt(jnp.mean(jnp.square(o), axis=-1, keepdims=True) + NORM_EPS) * norm_w.astype(f32)
    o = o * jax.nn.silu(z.astype(f32).reshape(shape))
    return o.reshape(bsz, seq, DN_WIDTH)


def token_mixer(h, w_in, lam_re, lam_im, log_step, ssm_b_re, ssm_b_im, ssm_c_re, ssm_c_im, ssm_d,
                w_glu, b_glu, conv_w, a_log, dt_bias, dn_norm_w, w_proj_ssm, w_proj_dn, w_out):
    proj = h @ w_in
    u, q, k, v, z, a, b, gate_ssm, gate_dn = jnp.split(proj, split_points(), axis=-1)
    y_ssm = s5_mixer(u, lam_re, lam_im, log_step, ssm_b_re, ssm_b_im, ssm_c_re, ssm_c_im, ssm_d, w_glu, b_glu)
    y_dn = gated_deltanet_mixer(q, k, v, z, a, b, conv_w, a_log, dt_bias, dn_norm_w)
    merged = (jax.nn.sigmoid(gate_ssm.astype(jnp.float32)) * (y_ssm @ w_proj_ssm)
              + jax.nn.sigmoid(gate_dn.astype(jnp.float32)) * (y_dn @ w_proj_dn))
    return (merged @ w_out).astype(h.dtype)


def moe_ffn(h, w_router, b_router, w_gate_up, b_gate_up, w_down, b_down):
    bsz, seq, dm = h.shape
    n_tok = bsz * seq
    xf = h.reshape(n_tok, dm)
    logits = (xf @ w_router + b_router).astype(jnp.float32)
    top_val, top_idx = lax.top_k(logits, TOP_K)
    gates = jax.nn.softmax(top_val, axis=-1)
    n_assign = n_tok * TOP_K
    e_flat = top_idx.reshape(n_assign)
    order = jnp.argsort(e_flat, stable=True)
    e_sorted = e_flat[order]
    tok_sorted = (order // TOP_K).astype(jnp.int32)
    gate_sorted = gates.reshape(n_assign)[order]
    counts = jnp.bincount(e_flat, length=N_EXPERTS)
    starts = jnp.cumsum(counts) - counts
    padded = (counts + EXPERT_BLOCK - 1) // EXPERT_BLOCK * EXPERT_BLOCK
    pad_ends = jnp.cumsum(padded)
    pad_starts = pad_ends - padded
    dest = pad_starts[e_sorted] + (jnp.arange(n_assign) - starts[e_sorted])
    n_rows = n_assign + N_EXPERTS * EXPERT_BLOCK
    n_blocks = n_rows // EXPERT_BLOCK
    row_tok = jnp.full((n_rows,), n_tok, jnp.int32).at[dest].set(tok_sorted)
    row_gate = jnp.zeros((n_rows,), jnp.float32).at[dest].set(gate_sorted)
    block_expert = jnp.minimum(
        jnp.searchsorted(pad_ends, jnp.arange(n_blocks) * EXPERT_BLOCK, side='right'), N_EXPERTS - 1)
    x_pad = jnp.concatenate([xf, jnp.zeros((1, dm), xf.dtype)], axis=0)
    xb = x_pad[row_tok].reshape(n_blocks, EXPERT_BLOCK, dm)

    def expert_block(args):
        xblk, e = args
        gu = xblk @ w_gate_up[e] + b_gate_up[e]
        gate, up = gu[:, :D_FF], gu[:, D_FF:]
        gate = jnp.minimum(gate, SWIGLU_LIMIT)
        up = jnp.clip(up, -SWIGLU_LIMIT, SWIGLU_LIMIT)
        act = gate * jax.nn.sigmoid(SWIGLU_ALPHA * gate) * (up + 1.0)
        return act @ w_down[e] + b_down[e]

    yb = lax.map(expert_block, (xb, block_expert))
    y = yb.reshape(n_rows, dm) * row_gate[:, None]
    out = jnp.zeros((n_tok + 1, dm), y.dtype).at[row_tok].add(y)[:n_tok]
    return out.reshape(bsz, seq, dm).astype(h.dtype)


def setup_inputs(seed: int = 0) -> dict:
    key = jax.random.key(seed)
    ks = jax.random.split(key, 40)
    f32 = jnp.float32
    L = DEPTH

    def nrm(k, shape, scale):
        return jax.random.normal(k, shape, f32) * scale

    def log_uniform(k, shape, lo, hi):
        return jax.random.uniform(k, shape, f32, math.log(lo), math.log(hi))

    n_idx = jnp.arange(SSM_STATE, dtype=f32)
    dt = jnp.exp(log_uniform(ks[14], (L, DN_HEADS), 1e-3, 1e-1))
    return {
        'x': nrm(ks[0], (BATCH, SEQ, D_MODEL), 1.0),
        'ln_in_g': 1.0 + nrm(ks[1], (D_MODEL,), 0.02),
        'ln_in_b': nrm(ks[2], (D_MODEL,), 0.02),
        'w_in': nrm(ks[3], (L, D_MODEL, IN_WIDTH), D_MODEL ** -0.5),
        'lam_re': -0.5 + nrm(ks[4], (L, SSM_GROUPS, SSM_STATE), 0.01),
        'lam_im': math.pi * n_idx + nrm(ks[5], (L, SSM_GROUPS, SSM_STATE), 0.01),
        'log_step': log_uniform(ks[6], (L, SSM_GROUPS), 1e-3, 1e-1),
        'ssm_b_re': nrm(ks[7], (L, SSM_GROUPS, SSM_STATE, SSM_GROUP_DIM), (2 * SSM_GROUP_DIM) ** -0.5),
        'ssm_b_im': nrm(ks[8], (L, SSM_GROUPS, SSM_STATE, SSM_GROUP_DIM), (2 * SSM_GROUP_DIM) ** -0.5),
        'ssm_c_re': nrm(ks[9], (L, SSM_GROUPS, SSM_GROUP_DIM, SSM_STATE), (2 * SSM_STATE) ** -0.5),
        'ssm_c_im': nrm(ks[10], (L, SSM_GROUPS, SSM_GROUP_DIM, SSM_STATE), (2 * SSM_STATE) ** -0.5),
        'ssm_d': nrm(ks[11], (L, SSM_GROUPS, SSM_GROUP_DIM), 0.5),
        'w_glu': nrm(ks[12], (L, SSM_WIDTH, SSM_WIDTH), SSM_WIDTH ** -0.5),
        'b_glu': nrm(ks[13], (L, SSM_WIDTH), 0.02),
        'conv_w': nrm(ks[15], (L, CONV_WIDTH, 1, 3 * DN_WIDTH), CONV_WIDTH ** -0.5),
        'a_log': jnp.log(jax.random.uniform(ks[16], (L, DN_HEADS), f32, 1.0, 16.0)),
        'dt_bias': dt + jnp.log(-jnp.expm1(-dt)),
        'dn_norm_w': 1.0 + nrm(ks[17], (L, DN_HEAD_DIM), 0.02),
        'w_proj_ssm': nrm(ks[18], (L, SSM_WIDTH, D_MODEL), DEEPNORM_BETA * SSM_WIDTH ** -0.5),
        'w_proj_dn': nrm(ks[19], (L, DN_WIDTH, D_MODEL), DEEPNORM_BETA * DN_WIDTH ** -0.5),
        'w_out': nrm(ks[20], (L, D_MODEL, D_MODEL), DEEPNORM_BETA * D_MODEL ** -0.5),
        'ln1_g': 1.0 + nrm(ks[21], (L, D_MODEL), 0.02),
        'ln1_b': nrm(ks[22], (L, D_MODEL), 0.02),
        'w_router': nrm(ks[23], (L, D_MODEL, N_EXPERTS), D_MODEL ** -0.5),
        'b_router': nrm(ks[24], (L, N_EXPERTS), 0.01),
        'w_gate_up': nrm(ks[25], (L, N_EXPERTS, D_MODEL, 2 * D_FF), DEEPNORM_BETA * D_MODEL ** -0.5),
        'b_gate_up': nrm(ks[26], (L, N_EXPERTS, 2 * D_FF), 0.01),
        'w_down': nrm(ks[27], (L, N_EXPERTS, D_FF, D_MODEL), DEEPNORM_BETA * D_FF ** -0.5),
        'b_down': nrm(ks[28], (L, N_EXPERTS, D_MODEL), 0.01),
        'ln2_g': 1.0 + nrm(ks[29], (L, D_MODEL), 0.02),
        'ln2_b': nrm(ks[30], (L, D_MODEL), 0.02),
    }


def reference(x, ln_in_g, ln_in_b, w_in, lam_re, lam_im, log_step, ssm_b_re, ssm_b_im, ssm_c_re,
              ssm_c_im, ssm_d, w_glu, b_glu, conv_w, a_log, dt_bias, dn_norm_w, w_proj_ssm,
              w_proj_dn, w_out, ln1_g, ln1_b, w_router, b_router, w_gate_up, b_gate_up, w_down,
              b_down, ln2_g, ln2_b):
    h = layer_norm(x, ln_in_g, ln_in_b)
    for i in range(DEPTH):
        mix = token_mixer(h, w_in[i], lam_re[i], lam_im[i], log_step[i], ssm_b_re[i], ssm_b_im[i],
                          ssm_c_re[i], ssm_c_im[i], ssm_d[i], w_glu[i], b_glu[i], conv_w[i],
                          a_log[i], dt_bias[i], dn_norm_w[i], w_proj_ssm[i], w_proj_dn[i], w_out[i])
        h = layer_norm(DEEPNORM_ALPHA * h + mix, ln1_g[i], ln1_b[i])
        ffn = moe_ffn(h, w_router[i], b_router[i], w_gate_up[i], b_gate_up[i], w_down[i], b_down[i])
        h = layer_norm(DEEPNORM_ALPHA * h + ffn, ln2_g[i], ln2_b[i])
    return h
```

```python
import numpy as np
from contextlib import ExitStack
import concourse.bass as bass
import concourse.mybir as mybir
from concourse.bass_utils import run_bass_kernel_spmd

F32 = mybir.dt.float32
F32R = mybir.dt.float32r
BF16 = mybir.dt.bfloat16
I32 = mybir.dt.int32
ALU = mybir.AluOpType
AF = mybir.ActivationFunctionType
AX = mybir.AxisListType

D = 2048
ALPHA = 2.0 ** 0.25
LN_EPS = 1e-5
NORM_EPS = 1e-6
MAGIC = 12582912.0
TWO_PI = float(2 * np.pi)


class Buf:
    __slots__ = ("name", "w", "r")

    def __init__(self, name=""):
        self.name = name
        self.w = None
        self.r = {}


class Sched:
    def __init__(self, nc, es):
        self.nc = nc
        self.es = es
        self.eng = {"pe": nc.tensor, "act": nc.scalar, "dve": nc.vector, "pool": nc.gpsimd, "sp": nc.sync}
        self.sem, self.cnt, self.step = {}, {}, {}
        for n in self.eng:
            self.sem[n] = es.enter_context(nc.semaphore("sem_" + n))
            self.cnt[n] = 0
            self.step[n] = 1
        self.seen = {n: {} for n in self.eng}

    def stream(self, n, step=16):
        if n not in self.sem:
            self.sem[n] = self.es.enter_context(self.nc.semaphore("sem_" + n))
            self.cnt[n] = 0
            self.step[n] = step
        return n

    def _wait(self, e, tok):
        if tok is None:
            return
        s, c = tok
        if s == e and e == "pe":
            return
        if self.seen[e].get(s, 0) >= c:
            return
        self.eng[e].wait_ge(self.sem[s], c)
        self.seen[e][s] = c

    def deps(self, e, reads, writes, extra=()):
        best = {}

        def add(t):
            if t is not None and best.get(t[0], 0) < t[1]:
                best[t[0]] = t[1]
        for b in reads:
            add(b.w)
        for b in writes:
            add(b.w)
            for s, c in b.r.items():
                add((s, c))
        for t in extra:
            add(t)
        for s, c in best.items():
            self._wait(e, (s, c))

    def mark(self, tok, reads, writes):
        s, c = tok
        for b in reads:
            if b.r.get(s, 0) < c:
                b.r[s] = c
        for b in writes:
            b.w = tok
            b.r = {}

    def op(self, e, fn, reads=(), writes=(), extra=()):
        self.deps(e, reads, writes, extra)
        inst = fn(self.eng[e])
        self.cnt[e] += 1
        inst.then_inc(self.sem[e], 1)
        tok = (e, self.cnt[e])
        self.mark(tok, reads, writes)
        return tok

    def dma(self, q, stream, out, in_, reads=(), writes=(), extra=()):
        self.stream(stream)
        self.deps(q, reads, writes, extra)
        inst = self.eng[q].dma_start(out=out, in_=in_)
        self.cnt[stream] += 16
        inst.then_inc(self.sem[stream], 16)
        tok = (stream, self.cnt[stream])
        self.mark(tok, reads, writes)
        return tok

    def idma(self, stream, out, in_, idx_ap, reads=(), writes=()):
        self.stream(stream)
        self.deps("pool", reads, writes)
        inst = self.nc.gpsimd.indirect_dma_start(out=out, out_offset=None, in_=in_, in_offset=bass.IndirectOffsetOnAxis(ap=idx_ap, axis=0))
        self.cnt[stream] += 16
        inst.then_inc(self.sem[stream], 16)
        tok = (stream, self.cnt[stream])
        self.mark(tok, reads, writes)
        return tok

    def barrier(self):
        for e in self.eng:
            for s in list(self.sem):
                if self.cnt[s] > 0 and not (s == e and e == "pe"):
                    self._wait(e, (s, self.cnt[s]))


def build(SEQ=8192, stage=9):
    nc = bass.Bass("TRN2", target_bir_lowering=False)
    NBLK = SEQ // 512
    TPC = SEQ // 4
    NBB = TPC // 512
    NTOK = 4 * TPC
    TB = min(1024, TPC)
    NTB = NTOK // TB
    NEX = 8
    RPC = min(512, TPC)
    NRS = TPC // RPC

    def din(name, shape, dt=F32):
        return nc.dram_tensor(name, shape, dt, kind="ExternalInput").ap()

    x = din("x", [SEQ, D])
    lnin = din("lnin", [128, 2, 16])
    wA = din("wA", [D, 2304])
    wab = din("wab", [D, 8])
    s5p = din("s5p", [128, 8, 3])
    s5b = din("s5b", [128, 8, 2, 16])
    s5c = din("s5c", [128, 8, 2, 16])
    s5d = din("s5d", [128, 2])
    convw = din("convw", [128, 12, 4])
    hpar = din("hpar", [4, 2])
    dnw = din("dnw", [128, 1])
    ident_d = din("ident", [128, 128])
    masks_d = din("masks", [128, 3, 128])
    iota_d = din("iota", [128, 384])
    iotap_d = din("iotap", [128, 3])
    sel8_d = din("sel8", [8, 8, 128])
    sel4_d = din("sel4", [4, 4, 128])
    if stage >= 2 and stage != 15:
        xq = din("xq", [TPC, D])
        yidx_d = din("yidx", [128, NBB * 24], I32)
        wg = din("wg", [D, 4096])
        wglu = din("wglu", [1024, 1024])
        bglu = din("bglu", [128, 8])
        wps = din("wps", [1024, D])
        wpd = din("wpd", [D, D])
        wout = din("wout", [D, D])
        ln1 = din("ln1", [128, 2, 16])
        wr = din("wr", [128, 16, 32])
        br = din("br", [32, 1])
    if stage >= 9 and stage != 15:
        wgu = din("wgu", [NEX, D, 4096])
        bgu = din("bgu", [128, NEX, 32])
        wdn = din("wdn", [NEX, D, D])
        bdn = din("bdn", [NEX, D])
        ln2 = din("ln2", [2, D])
        esel_d = din("esel", [128, NEX, 32])

    if stage in (1, 15):
        out_d = nc.dram_tensor("out", [768, SEQ], BF16, kind="ExternalOutput").ap()
    else:
        out_d = nc.dram_tensor("out", [TPC, D], F32, kind="ExternalOutput").ap()

    yloc = nc.dram_tensor("yloc", [NBLK * 768, 512], BF16)
    yall = nc.dram_tensor("yall", [4 * NBLK * 768, 512], BF16)
    if stage >= 2 and stage != 15:
        HC = TPC // 256
        h1b_loc = nc.dram_tensor("h1b_loc", [TPC, D], BF16)
        h1b_all = nc.dram_tensor("h1b_all", [HC * 4 * 256, D], BF16)
        CAP = 384
        XTd = nc.dram_tensor("XTd", [NEX * D, 4 * CAP], BF16)
        Yd = nc.dram_tensor("Yd", [NEX * 4 * CAP, D], BF16)
        gate_loc = nc.dram_tensor("gate_loc", [TPC, 32], F32)
        gate_all = nc.dram_tensor("gate_all", [4 * TPC, 32], F32)
        h1tok = nc.dram_tensor("h1tok", [TPC, D], F32)
        dense = nc.dram_tensor("dense", [NRS * 4 * RPC, D], F32)
        ffn = nc.dram_tensor("ffn", [TPC, D], F32)

    with ExitStack() as es:
        S = Sched(nc, es)
        cc_sem = es.enter_context(nc.semaphore("cc_sem"))
        cc_cnt = [0]

        def sbt(stack, name, shape, dt=F32):
            return stack.enter_context(nc.sbuf_tensor("s_" + name, shape, dt))

        banks = [es.enter_context(nc.psum_tensor("pb%d" % i, [128, 512], F32)) for i in range(8)]
        bbuf = [Buf("pb%d" % i) for i in range(8)]
        brr = [0]

        def bank():
            i = brr[0]
            brr[0] = (i + 1) % 8
            return banks[i], bbuf[i]

        ident = sbt(es, "ident", [128, 128])
        ones = sbt(es, "ones", [128, 128])
        masks = sbt(es, "masks", [128, 3, 128])
        epsln = sbt(es, "epsln", [128, 1])
        epsnm = sbt(es, "epsnm", [128, 1])
        bC = Buf("consts")
        S.dma("sp", "cst", ident[:], ident_d[:, :], writes=[bC])
        S.dma("sp", "cst", masks[:], masks_d[:, :, :], writes=[bC])
        S.op("dve", lambda e: e.memset(ones[:], 1.0), writes=[bC])
        S.op("dve", lambda e: e.memset(epsln[:], LN_EPS), writes=[bC])
        S.op("dve", lambda e: e.memset(epsnm[:], NORM_EPS), writes=[bC])

        wbuf = [sbt(es, "wbuf%d" % i, [128, 16, 256], BF16) for i in range(2)]
        wbb = [Buf("wbuf%d" % i) for i in range(2)]
        wrr = [0]

        def load_w(src_ap, kc, ncols):
            i = wrr[0] % len(wbuf)
            wrr[0] = (i + 1) % len(wbuf)
            S.dma("pool", "wl%d" % i, wbuf[i][:, 0:kc, 0:ncols], src_ap.rearrange("(kc p) m -> p kc m", p=128), writes=[wbb[i]])
            return wbuf[i], wbb[i]

        def linear(wsrc, kc, mcols, rhs_fn, rhs_bufs, n, epilogue, group=256):
            mi = 0
            for c0 in range(0, mcols, group):
                gc = min(group, mcols - c0)
                wt, wb = load_w(wsrc[:, c0:c0 + gc], kc, gc)
                for m0 in range(0, gc, 128):
                    mw = min(128, gc - m0)
                    pb, pbb = bank()
                    for k in range(kc):
                        S.op("pe", lambda e, k=k, m0=m0, mw=mw, pb=pb: e.matmul(out=pb[0:mw, 0:n], lhsT=wt[:, k, m0:m0 + mw], rhs=rhs_fn(k), start=(k == 0), stop=(k == kc - 1)),
                             reads=[wb] + list(rhs_bufs), writes=[pbb])
                    epilogue(mi, pb, pbb)
                    mi += 1

        def layer_norm_T(pst, xsrc_rows, gb_tile, bgb, dstT, bdst, tcol0, xt, bxt, xn, bxn, st6, mv, rstd, bst):
            S.dma("sp", "xl", xt[:], xsrc_rows, writes=[bxt])
            for q in range(4):
                S.op("dve", lambda e, q=q: e.bn_stats(out=st6[:, q, :], in_=xt[:, q * 512:(q + 1) * 512]), reads=[bxt], writes=[bst])
            S.op("dve", lambda e: e.bn_aggr(out=mv[:], in_=st6[:].rearrange("p a b -> p (a b)")), reads=[bst], writes=[bst])
            S.op("act", lambda e: e.activation(out=rstd[:], in_=mv[:, 1:2], func=AF.Sqrt, bias=epsln[:, 0:1]), reads=[bst, bC], writes=[bst])
            S.op("dve", lambda e: e.reciprocal(out=rstd[:], in_=rstd[:]), reads=[bst], writes=[bst])
            S.op("dve", lambda e: e.tensor_scalar(out=xn[:], in0=xt[:], scalar1=mv[:, 0:1], scalar2=rstd[:, 0:1], op0=ALU.subtract, op1=ALU.mult), reads=[bxt, bst], writes=[bxn])
            for kq in range(4):
                pb, pbb = bank()
                for r in range(4):
                    k = kq * 4 + r
                    S.op("pe", lambda e, k=k, r=r, pb=pb: e.transpose(out=pb[:, r * 128:(r + 1) * 128], in_=xn[:, k * 128:(k + 1) * 128], identity=ident[:]), reads=[bxn, bC], writes=[pbb])
                for r in range(4):
                    k = kq * 4 + r
                    for (dt_, bd_) in zip(dstT, bdst):
                        S.op("act", lambda e, k=k, r=r, pb=pb, dt_=dt_: e.activation(out=dt_[:, k, tcol0:tcol0 + 128], in_=pb[:, r * 128:(r + 1) * 128], func=AF.Identity, scale=gb_tile[:, 0, k:k + 1], bias=gb_tile[:, 1, k:k + 1]),
                             reads=[pbb, bgb], writes=[bd_])

        with ExitStack() as pa:
            lnin_t = sbt(pa, "lnin_t", [128, 2, 16]); blnin = Buf()
            S.dma("sp", "cst", lnin_t[:], lnin[:, :, :], writes=[blnin])
            xt = sbt(pa, "xt", [128, D]); bxt = Buf()
            xn = sbt(pa, "xn", [128, D]); bxn = Buf()
            st6 = sbt(pa, "st6", [128, 4, 6]); mv = sbt(pa, "mv", [128, 2]); rstd = sbt(pa, "rstd", [128, 1]); bst = Buf()
            hT = sbt(pa, "hT", [128, 16, 512], BF16); bhT = Buf()
            iota = sbt(pa, "iota", [128, 132]); biota = Buf()
            S.dma("sp", "cst", iota[:], iota_d[:, 0:132], writes=[biota])

            p5 = sbt(pa, "p5", [128, 8, 3]); b5 = sbt(pa, "b5", [128, 8, 2, 16]); c5 = sbt(pa, "c5", [128, 8, 2, 16]); d5 = sbt(pa, "d5", [128, 2])
            bp5 = Buf()
            S.dma("sp", "cst", p5[:], s5p[:, :, :], writes=[bp5])
            S.dma("sp", "cst", b5[:], s5b[:, :, :, :], writes=[bp5])
            S.dma("sp", "cst", c5[:], s5c[:, :, :, :], writes=[bp5])
            S.dma("sp", "cst", d5[:], s5d[:, :], writes=[bp5])
            sc = sbt(pa, "s5sc", [128, 16, 8]); bsc = Buf()
            LR, LI, STEP, MAG, TH, RHO, COS1, SIN1, ARE, AIM, DEN, FRE, FIM, T0, T1, T2 = [sc[:, i, :] for i in range(16)]
            V = "dve"

            def ts(out, in0, s1, s2, o0, o1=None):
                if o1 is None:
                    S.op(V, lambda e: e.tensor_scalar(out=out, in0=in0, scalar1=s1, scalar2=None, op0=o0), reads=[bsc, bp5], writes=[bsc])
                else:
                    S.op(V, lambda e: e.tensor_scalar(out=out, in0=in0, scalar1=s1, scalar2=s2, op0=o0, op1=o1), reads=[bsc, bp5], writes=[bsc])

            def tt(out, a, b, op):
                S.op(V, lambda e: e.tensor_tensor(out=out, in0=a, in1=b, op=op), reads=[bsc, bp5], writes=[bsc])

            def act(out, in_, func, scale=1.0, bias=0.0):
                S.op("act", lambda e: e.activation(out=out, in_=in_, func=func, scale=scale, bias=bias), reads=[bsc, bp5, bC], writes=[bsc])

            def sincos(out_s, out_c, ang, t0, t1):
                for (o, sh) in ((out_s, 0.0), (out_c, float(np.pi / 2))):
                    ts(t0, ang, sh, None, ALU.add)
                    ts(t1, t0, float(1 / TWO_PI), MAGIC, ALU.mult, ALU.add)
                    ts(t1, t1, -MAGIC, None, ALU.add)
                    S.op(V, lambda e, t0=t0, t1=t1: e.scalar_tensor_tensor(out=t0, in0=t1, scalar=-TWO_PI, in1=t0, op0=ALU.mult, op1=ALU.add), reads=[bsc], writes=[bsc])
                    act(o, t0, AF.Sin)

            act(STEP, p5[:, :, 2], AF.Exp)
            tt(T0, p5[:, :, 0], STEP, ALU.mult)
            act(RHO, T0, AF.Exp)
            tt(TH, p5[:, :, 1], STEP, ALU.mult)
            sincos(SIN1, COS1, TH, T0, T1)
            tt(ARE, RHO, COS1, ALU.mult)
            tt(AIM, RHO, SIN1, ALU.mult)
            tt(T0, p5[:, :, 0], p5[:, :, 0], ALU.mult)
            tt(T1, p5[:, :, 1], p5[:, :, 1], ALU.mult)
            tt(DEN, T0, T1, ALU.add)
            S.op(V, lambda e: e.reciprocal(out=DEN, in_=DEN), reads=[bsc], writes=[bsc])
            ts(T2, ARE, -1.0, None, ALU.add)
            tt(T0, T2, p5[:, :, 0], ALU.mult)
            tt(T1, AIM, p5[:, :, 1], ALU.mult)
            tt(T0, T0, T1, ALU.add)
            tt(FRE, T0, DEN, ALU.mult)
            tt(T0, AIM, p5[:, :, 0], ALU.mult)
            tt(T1, T2, p5[:, :, 1], ALU.mult)
            tt(T0, T0, T1, ALU.subtract)
            tt(FIM, T0, DEN, ALU.mult)
            bb = sbt(pa, "bb", [128, 8, 2, 16]); tb = sbt(pa, "tbb", [128, 8, 16])
            freB = sc[:, 11, :].unsqueeze(2).to_broadcast([128, 8, 16])
            fimB = sc[:, 12, :].unsqueeze(2).to_broadcast([128, 8, 16])
            tt(bb[:, :, 0, :], b5[:, :, 0, :], freB, ALU.mult)
            tt(tb[:], b5[:, :, 1, :], fimB, ALU.mult)
            tt(bb[:, :, 0, :], bb[:, :, 0, :], tb[:], ALU.subtract)
            tt(bb[:, :, 1, :], b5[:, :, 1, :], freB, ALU.mult)
            tt(tb[:], b5[:, :, 0, :], fimB, ALU.mult)
            tt(bb[:, :, 1, :], bb[:, :, 1, :], tb[:], ALU.add)
            BbTz = sbt(pa, "BbTz", [128, 8, 2, 128]); CTz = sbt(pa, "CTz", [128, 8, 2, 128]); bdp = sbt(pa, "bdp", [128, 128])
            btab = Buf()
            S.op(V, lambda e: e.memset(CTz[:].rearrange("p a b c -> p (a b c)"), 0.0), writes=[btab])
            for gp in range(8):
                gl = gp % 4
                for ri in range(2):
                    S.op(V, lambda e: e.memset(bdp[:], 0.0), reads=[bsc], writes=[bsc])
                    for g2 in range(2):
                        c0 = 32 * gl + 16 * g2
                        S.op(V, lambda e, g2=g2, c0=c0, gp=gp, ri=ri: e.tensor_copy(out=bdp[64 * g2:64 * g2 + 64, c0:c0 + 16], in_=bb[64 * g2:64 * g2 + 64, gp, ri, :]), reads=[bsc], writes=[bsc])
                        if ri == 0:
                            S.op(V, lambda e, g2=g2, c0=c0, gp=gp: e.tensor_copy(out=CTz[64 * g2:64 * g2 + 64, gp, 0, c0:c0 + 16], in_=c5[64 * g2:64 * g2 + 64, gp, 0, :]), reads=[bp5], writes=[btab])
                        else:
                            S.op(V, lambda e, g2=g2, c0=c0, gp=gp: e.tensor_scalar(out=CTz[64 * g2:64 * g2 + 64, gp, 1, c0:c0 + 16], in0=c5[64 * g2:64 * g2 + 64, gp, 1, :], scalar1=-1.0, scalar2=None, op0=ALU.mult), reads=[bp5], writes=[btab])
                    pb, pbb = bank()
                    S.op("pe", lambda e, pb=pb: e.transpose(out=pb[:, 0:128], in_=bdp[:], identity=ident[:]), reads=[bsc, bC], writes=[pbb])
                    S.op("act", lambda e, pb=pb, gp=gp, ri=ri: e.copy(out=BbTz[:, gp, ri, :], in_=pb[:, 0:128]), reads=[pbb], writes=[btab])
            cst = sbt(pa, "cstab", [128, 8, 2, 132]); rhoT = sbt(pa, "rhoT", [128, 8, 128]); ph = sbt(pa, "ph", [128, 132]); pt0 = sbt(pa, "pt0", [128, 132]); pt1 = sbt(pa, "pt1", [128, 132])
            for gp in range(8):
                S.op(V, lambda e, gp=gp: e.tensor_scalar(out=ph[:], in0=iota[:], scalar1=sc[:, 4, gp:gp + 1], scalar2=None, op0=ALU.mult), reads=[bsc, biota], writes=[bsc])
                sincos(cst[:, gp, 1, :], cst[:, gp, 0, :], ph[:], pt0[:], pt1[:])
                S.op(V, lambda e, gp=gp: e.tensor_scalar(out=rhoT[:, gp, :], in0=ones[:], scalar1=sc[:, 5, gp:gp + 1], scalar2=None, op0=ALU.mult), reads=[bsc, bC], writes=[bsc])
            S.op(V, lambda e: e.tensor_copy(out=ph[:, 0:1], in_=ph[:, 0:1]), reads=[bsc], writes=[btab])
            carry = sbt(pa, "carry", [128, 8, 2]); bcar = Buf()
            S.op(V, lambda e: e.memset(carry[:].rearrange("p a b -> p (a b)"), 0.0), writes=[bcar])

            cw = sbt(pa, "cw", [128, 12, 4]); hp = sbt(pa, "hp", [4, 2]); nw = sbt(pa, "nw", [128, 1]); sel4 = sbt(pa, "sel4", [4, 4, 128])
            bgp = Buf()
            S.dma("sp", "cst", cw[:], convw[:, :, :], writes=[bgp])
            S.dma("sp", "cst", hp[:], hpar[:, :], writes=[bgp])
            S.dma("sp", "cst", nw[:], dnw[:, :], writes=[bgp])
            S.dma("sp", "cst", sel4[:], sel4_d[:, :, :], writes=[bgp])
            nexpA = sbt(pa, "nexpA", [4, 1])
            S.op("act", lambda e: e.activation(out=nexpA[:], in_=hp[:, 0:1], func=AF.Exp), reads=[bgp], writes=[bgp])
            S.op(V, lambda e: e.tensor_scalar(out=nexpA[:], in0=nexpA[:], scalar1=-1.0, scalar2=None, op0=ALU.mult), reads=[bgp], writes=[bgp])
            ones4 = sbt(pa, "ones4", [4, 128])
            S.op(V, lambda e: e.memset(ones4[:], 1.0), writes=[bgp])
            halo = sbt(pa, "halo", [128, 12, 3]); bhalo = Buf()
            S.op(V, lambda e: e.memset(halo[:].rearrange("p a b -> p (a b)"), 0.0), writes=[bhalo])
            Sst = sbt(pa, "Sst", [128, 4, 128], F32R); bS = Buf()
            S.op(V, lambda e: e.tensor_scalar(out=Sst[:], in0=ones[:, :].unsqueeze(1).to_broadcast([128, 4, 128]), scalar1=0.0, scalar2=None, op0=ALU.mult), reads=[bC], writes=[bS])

            uT = sbt(pa, "uT", [128, 2, 512]); buT = Buf()
            rtmp = [sbt(pa, "rtmp%d" % i, [128, 515]) for i in range(2)]; brtmp = [Buf(), Buf()]
            qkv = sbt(pa, "qkv", [128, 12, 512]); bqkv = [Buf() for _ in range(12)]
            qe = sbt(pa, "qe", [128, 4, 512], F32R); bqe = Buf()
            zs = sbt(pa, "zs", [128, 4, 512], BF16); bzs = Buf()
            abr = sbt(pa, "abr", [4, 2, 512]); babr = Buf()
            ysb = sbt(pa, "ysb", [128, 2, 512], BF16); bysb = Buf()
            ydn = sbt(pa, "ydn", [128, 4, 512], BF16); bydn = Buf()
            W = [sbt(pa, "wk%d" % i, [128, 512]) for i in range(25)]
            bW = [Buf("wk%d" % i) for i in range(25)]
            gsm = sbt(pa, "gsm", [128, 8, 16]); bgsm = Buf()
            grow = sbt(pa, "grow", [4, 3, 512]); bgrow = Buf()

            for blk in range(NBLK):
                t0 = blk * 512
                for tti in range(4):
                    layer_norm_T(pa, x[t0 + tti * 128:t0 + (tti + 1) * 128, :], lnin_t, blnin, [hT], [bhT], tti * 128, xt, bxt, xn, bxn, st6, mv, rstd, bst)

                def ep_A(mi, pb, pbb):
                    if mi < 2:
                        S.op("act", lambda e: e.copy(out=uT[:, mi, :], in_=pb[:, :]), reads=[pbb], writes=[buT])
                    elif mi < 14:
                        ct = mi - 2
                        r = rtmp[ct % 2]; br_ = brtmp[ct % 2]
                        S.op("act", lambda e: e.copy(out=r[:, 3:515], in_=pb[:, :]), reads=[pbb], writes=[br_])
                        S.op("act", lambda e: e.copy(out=r[:, 0:3], in_=halo[:, ct, :]), reads=[bhalo], writes=[br_])
                        S.op("act", lambda e: e.copy(out=halo[:, ct, :], in_=r[:, 512:515]), reads=[br_], writes=[bhalo])
                        acc = W[0]; bacc = bW[0]
                        S.op(V, lambda e: e.tensor_scalar(out=acc[:], in0=r[:, 3:515], scalar1=cw[:, ct, 3:4], scalar2=None, op0=ALU.mult), reads=[br_, bgp], writes=[bacc])
                        for jj in (2, 1, 0):
                            S.op(V, lambda e, jj=jj: e.scalar_tensor_tensor(out=acc[:], in0=r[:, jj:jj + 512], scalar=cw[:, ct, jj:jj + 1], in1=acc[:], op0=ALU.mult, op1=ALU.add), reads=[br_, bgp, bacc], writes=[bacc])
                        qo = qkv[:, ct, :].bitcast(F32R)
                        S.op("act", lambda e: e.activation(out=qo, in_=acc[:], func=AF.Silu), reads=[bacc], writes=[bqkv[ct]])
                    else:
                        hh = mi - 14
                        S.op("act", lambda e: e.activation(out=zs[:, hh, :], in_=pb[:, :], func=AF.Silu), reads=[pbb], writes=[bzs])

                linear(wA, 16, 2304, lambda k: hT[:, k, :], [bhT], 512, ep_A)

                def ep_ab(mi, pb, pbb):
                    S.op("act", lambda e: e.copy(out=abr[:, mi, :], in_=pb[0:4, :]), reads=[pbb], writes=[babr])
                for which in range(2):
                    i = wrr[0] % len(wbuf); wrr[0] = (i + 1) % len(wbuf)
                    S.dma("pool", "wl%d" % i, wbuf[i][:, 0:16, 0:4], wab[:, which * 4:which * 4 + 4].rearrange("(kc p) m -> p kc m", p=128), writes=[wbb[i]])
                    pb, pbb = bank()
                    for k in range(16):
                        S.op("pe", lambda e, k=k, i=i, pb=pb: e.matmul(out=pb[0:4, :], lhsT=wbuf[i][:, k, 0:4], rhs=hT[:, k, :], start=(k == 0), stop=(k == 15)), reads=[wbb[i], bhT], writes=[pbb])
                    ep_ab(which, pb, pbb)

                for hf in range(2):
                    ypb, ypbb = bank()
                    xs = []
                    for gl in range(4):
                        gp = hf * 4 + gl
                        bure, bbre = bank(); buim, bbim = bank()
                        S.op("pe", lambda e, gp=gp, bure=bure: e.matmul(out=bure[:, :], lhsT=BbTz[:, gp, 0, :], rhs=uT[:, hf, :], start=True, stop=True), reads=[btab, buT], writes=[bbre])
                        S.op("pe", lambda e, gp=gp, buim=buim: e.matmul(out=buim[:, :], lhsT=BbTz[:, gp, 1, :], rhs=uT[:, hf, :], start=True, stop=True), reads=[btab, buT], writes=[bbim])
                        cB = cst[:, gp, 0, 0:128].unsqueeze(1).to_broadcast([128, 4, 128])
                        sB = cst[:, gp, 1, 0:128].unsqueeze(1).to_broadcast([128, 4, 128])
                        t1, t2, btr, bti = W[0], W[1], W[2], W[3]
                        xre, xim = W[4 + 2 * gl], W[5 + 2 * gl]
                        wl = [bW[0], bW[1], bW[2], bW[3]]

                        def v3(t):
                            return t[:].rearrange("p (a b) -> p a b", a=4)

                        def p3(t):
                            return t[:, :].rearrange("p (a b) -> p a b", a=4)
                        S.op(V, lambda e: e.tensor_tensor(out=v3(t1), in0=p3(bure), in1=cB, op=ALU.mult), reads=[bbre, btab], writes=[bW[0]])
                        S.op(V, lambda e: e.tensor_tensor(out=v3(t2), in0=p3(buim), in1=sB, op=ALU.mult), reads=[bbim, btab], writes=[bW[1]])
                        S.op(V, lambda e: e.tensor_tensor(out=btr[:], in0=t1[:], in1=t2[:], op=ALU.add), reads=[bW[0], bW[1]], writes=[bW[2]])
                        S.op(V, lambda e: e.tensor_tensor(out=v3(t1), in0=p3(buim), in1=cB, op=ALU.mult), reads=[bbim, btab], writes=[bW[0]])
                        S.op(V, lambda e: e.tensor_tensor(out=v3(t2), in0=p3(bure), in1=sB, op=ALU.mult), reads=[bbre, btab], writes=[bW[1]])
                        S.op(V, lambda e: e.tensor_tensor(out=bti[:], in0=t1[:], in1=t2[:], op=ALU.subtract), reads=[bW[0], bW[1]], writes=[bW[3]])
                        for sb_ in range(4):
                            cs_ = slice(sb_ * 128, (sb_ + 1) * 128)
                            S.op(V, lambda e, cs_=cs_: e.tensor_tensor_scan(out=t1[:, cs_], data0=rhoT[:, gp, :], data1=btr[:, cs_], initial=carry[:, gp, 0:1], op0=ALU.mult, op1=ALU.add), reads=[bW[2], btab, bcar], writes=[bW[0]])
                            S.op(V, lambda e, cs_=cs_: e.tensor_tensor_scan(out=t2[:, cs_], data0=rhoT[:, gp, :], data1=bti[:, cs_], initial=carry[:, gp, 1:2], op0=ALU.mult, op1=ALU.add), reads=[bW[3], btab, bcar], writes=[bW[1]])
                            e1 = sb_ * 128 + 127
                            cr, ci = cst[:, gp, 0, 128:129], cst[:, gp, 1, 128:129]
                            tmpc = gsm[:, 7, 0:2]
                            S.op(V, lambda e: e.tensor_scalar(out=tmpc[:, 0:1], in0=t2[:, e1:e1 + 1], scalar1=ci, scalar2=-1.0, op0=ALU.mult, op1=ALU.mult), reads=[bW[1], btab], writes=[bgsm])
                            S.op(V, lambda e: e.tensor_scalar(out=tmpc[:, 1:2], in0=t1[:, e1:e1 + 1], scalar1=ci, scalar2=None, op0=ALU.mult), reads=[bW[0], btab], writes=[bgsm])
                            S.op(V, lambda e: e.scalar_tensor_tensor(out=carry[:, gp, 0:1], in0=t1[:, e1:e1 + 1], scalar=cr, in1=tmpc[:, 0:1], op0=ALU.mult, op1=ALU.add), reads=[bW[0], bgsm, btab], writes=[bcar])
                            S.op(V, lambda e: e.scalar_tensor_tensor(out=carry[:, gp, 1:2], in0=t2[:, e1:e1 + 1], scalar=cr, in1=tmpc[:, 1:2], op0=ALU.mult, op1=ALU.add), reads=[bW[1], bgsm, btab], writes=[bcar])
                        S.op(V, lambda e: e.tensor_tensor(out=v3(btr), in0=v3(t1), in1=cB, op=ALU.mult), reads=[bW[0], btab], writes=[bW[2]])
                        S.op(V, lambda e: e.tensor_tensor(out=v3(bti), in0=v3(t2), in1=sB, op=ALU.mult), reads=[bW[1], btab], writes=[bW[3]])
                        S.op(V, lambda e: e.tensor_tensor(out=xre[:], in0=btr[:], in1=bti[:], op=ALU.subtract), reads=[bW[2], bW[3]], writes=[bW[4 + 2 * gl]])
                        S.op(V, lambda e: e.tensor_tensor(out=v3(btr), in0=v3(t2), in1=cB, op=ALU.mult), reads=[bW[1], btab], writes=[bW[2]])
                        S.op(V, lambda e: e.tensor_tensor(out=v3(bti), in0=v3(t1), in1=sB, op=ALU.mult), reads=[bW[0], btab], writes=[bW[3]])
                        S.op(V, lambda e: e.tensor_tensor(out=xim[:], in0=btr[:], in1=bti[:], op=ALU.add), reads=[bW[2], bW[3]], writes=[bW[5 + 2 * gl]])
                        xs.append((gp, xre, xim, bW[4 + 2 * gl], bW[5 + 2 * gl]))
                    for n_, (gp, xre, xim, b1_, b2_) in enumerate(xs):
                        S.op("pe", lambda e, gp=gp, xre=xre: e.matmul(out=ypb[:, :], lhsT=CTz[:, gp, 0, :], rhs=xre[:], start=(n_ == 0), stop=False), reads=[btab, b1_], writes=[ypbb])
                        S.op("pe", lambda e, gp=gp, xim=xim: e.matmul(out=ypb[:, :], lhsT=CTz[:, gp, 1, :], rhs=xim[:], start=False, stop=(n_ == 3)), reads=[btab, b2_], writes=[ypbb])
                    yt = W[12]
                    S.op(V, lambda e: e.scalar_tensor_tensor(out=yt[:], in0=uT[:, hf, :], scalar=d5[:, hf:hf + 1], in1=ypb[:, :], op0=ALU.mult, op1=ALU.add), reads=[buT, bp5, ypbb], writes=[bW[12]])
                    S.op("act", lambda e: e.activation(out=ysb[:, hf, :], in_=yt[:], func=AF.Gelu), reads=[bW[12]], writes=[bysb])
                S.dma("sp", "st", yloc[blk * 768:blk * 768 + 256, :].rearrange("(a p) t -> p a t", p=128), ysb[:], reads=[bysb])

                for ct in range(8):
                    i0_ = 2 * (ct % 4); i1_ = i0_ + 1
                    sq = W[i0_]
                    S.op("act", lambda e: e.activation(out=sq[:], in_=qkv[:, ct, :], func=AF.Square), reads=[bqkv[ct]], writes=[bW[i0_]])
                    pb, pbb = bank()
                    S.op("pe", lambda e, pb=pb: e.matmul(out=pb[:, :], lhsT=ones[:], rhs=sq[:], start=True, stop=True), reads=[bC, bW[i0_]], writes=[pbb])
                    rs = W[i1_]
                    S.op("act", lambda e, pb=pb: e.activation(out=rs[:], in_=pb[:, :], func=AF.Sqrt, bias=epsnm[:, 0:1]), reads=[pbb, bC], writes=[bW[i1_]])
                    S.op(V, lambda e: e.reciprocal(out=rs[:], in_=rs[:]), reads=[bW[i1_]], writes=[bW[i1_]])
                    S.op(V, lambda e, ct=ct: e.scalar_tensor_tensor(out=qkv[:, ct, :].bitcast(F32R), in0=qkv[:, ct, :], scalar=(128.0 ** -0.5 if ct < 4 else 1.0), in1=rs[:], op0=ALU.mult, op1=ALU.mult), reads=[bqkv[ct], bW[i1_]], writes=[bqkv[ct]])
                S.op("act", lambda e: e.activation(out=grow[:, 2, :], in_=abr[:, 1, :], func=AF.Sigmoid), reads=[babr], writes=[bgrow])
                S.op("act", lambda e: e.activation(out=grow[:, 0, :], in_=abr[:, 0, :], func=AF.Exp, bias=hp[:, 1:2]), reads=[babr, bgp], writes=[bgrow])
                S.op("act", lambda e: e.activation(out=grow[:, 0, :], in_=grow[:, 0, :], func=AF.Ln, bias=1.0), reads=[bgrow], writes=[bgrow])
                S.op(V, lambda e: e.tensor_scalar(out=grow[:, 0, :], in0=grow[:, 0, :], scalar1=nexpA[:, 0:1], scalar2=None, op0=ALU.mult), reads=[bgrow, bgp], writes=[bgrow])
                for ch in range(4):
                    cs_ = slice(ch * 128, (ch + 1) * 128)
                    S.op(V, lambda e, cs_=cs_: e.tensor_tensor_scan(out=grow[:, 1, cs_], data0=ones4[:, :], data1=grow[:, 0, cs_], initial=0.0, op0=ALU.mult, op1=ALU.add), reads=[bgrow, bgp], writes=[bgrow])
                pbc, pbcb = bank()
                for ch in range(4):
                    for kind, row in ((0, 1), (1, 2)):
                        S.op("pe", lambda e, ch=ch, kind=kind, row=row: e.transpose(out=pbc[:, kind * 16 + ch * 4:kind * 16 + ch * 4 + 4], in_=grow[:, row, ch * 128:(ch + 1) * 128], identity=ident[0:4, 0:4]), reads=[bgrow, bC], writes=[pbcb])
                S.op("act", lambda e: e.copy(out=gsm[:, 0:2, :], in_=pbc[:, 0:32].rearrange("p (a b) -> p a b", a=2)), reads=[pbcb], writes=[bgsm])

                for ch in range(4):
                    cs_ = slice(ch * 128, (ch + 1) * 128)
                    hc = slice(ch * 4, ch * 4 + 4)
                    pg, pgb = bank(); pbt, pbtb = bank()
                    for h in range(4):
                        S.op("pe", lambda e, h=h, pg=pg: e.matmul(out=pg[:, h * 128:(h + 1) * 128], lhsT=sel4[:, h, :], rhs=grow[:, 1, cs_], start=True, stop=True), reads=[bgp, bgrow], writes=[pgb])
                        S.op("pe", lambda e, h=h, pbt=pbt: e.matmul(out=pbt[:, h * 128:(h + 1) * 128], lhsT=sel4[:, h, :], rhs=grow[:, 2, cs_], start=True, stop=True), reads=[bgp, bgrow], writes=[pbtb])
                    gcRB, beRB, egRB = W[0], W[1], W[2]
                    S.op("act", lambda e, pg=pg: e.copy(out=gcRB[:], in_=pg[:, :]), reads=[pgb], writes=[bW[0]])
                    S.op("act", lambda e, pbt=pbt: e.copy(out=beRB[:], in_=pbt[:, :]), reads=[pbtb], writes=[bW[1]])
                    S.op("act", lambda e, pg=pg: e.activation(out=egRB[:], in_=pg[:, :], func=AF.Exp), reads=[pgb], writes=[bW[2]])

                    def h3(t):
                        return t[:].rearrange("p (a b) -> p a b", a=4)

                    def colB(kind):
                        return gsm[:, kind, hc].unsqueeze(2).to_broadcast([128, 4, 128])
                    S.op("act", lambda e: e.activation(out=gsm[:, 2, hc], in_=gsm[:, 0, hc], func=AF.Exp), reads=[bgsm], writes=[bgsm])
                    S.op(V, lambda e: e.tensor_tensor(out=gsm[:, 3, hc], in0=gsm[:, 1, hc], in1=gsm[:, 2, hc], op=ALU.mult), reads=[bgsm], writes=[bgsm])
                    S.op(V, lambda e: e.tensor_copy(out=gsm[:, 4, hc], in_=h3(gcRB)[:, :, 127]), reads=[bW[0]], writes=[bgsm])
                    S.op(V, lambda e: e.tensor_tensor(out=gsm[:, 5, hc], in0=gsm[:, 4, hc], in1=gsm[:, 0, hc], op=ALU.subtract), reads=[bgsm], writes=[bgsm])
                    S.op("act", lambda e: e.activation(out=gsm[:, 5, hc], in_=gsm[:, 5, hc], func=AF.Exp), reads=[bgsm], writes=[bgsm])
                    S.op("act", lambda e: e.activation(out=gsm[:, 6, hc], in_=gsm[:, 4, hc], func=AF.Exp), reads=[bgsm], writes=[bgsm])
                    for h in range(4):
                        S.op(V, lambda e, h=h: e.tensor_tensor(out=qe[:, h, cs_], in0=qkv[:, h, cs_], in1=egRB[:, h * 128:(h + 1) * 128], op=ALU.mult), reads=[bqkv[h], bW[2]], writes=[bqe])
                    A1, E1, Dm, E2, DqT, DmT = W[3], W[4], W[5], W[6], W[7], W[8]
                    S.op(V, lambda e: e.tensor_tensor(out=h3(A1), in0=h3(gcRB), in1=colB(0), op=ALU.subtract), reads=[bW[0], bgsm], writes=[bW[3]])
                    S.op(V, lambda e: e.tensor_scalar(out=E2[:], in0=A1[:], scalar1=0.0, scalar2=None, op0=ALU.min), reads=[bW[3]], writes=[bW[6]])
                    S.op(V, lambda e: e.tensor_scalar(out=A1[:], in0=A1[:], scalar1=0.0, scalar2=None, op0=ALU.max), reads=[bW[3]], writes=[bW[3]])
                    S.op("act", lambda e: e.activation(out=E1[:], in_=A1[:], func=AF.Exp, scale=-1.0), reads=[bW[3]], writes=[bW[4]])
                    S.op("act", lambda e: e.activation(out=E2[:], in_=E2[:], func=AF.Exp), reads=[bW[6]], writes=[bW[6]])
                    mS = masks[:, 0, :].unsqueeze(1).to_broadcast([128, 4, 128])
                    mIT = masks[:, 1, :].unsqueeze(1).to_broadcast([128, 4, 128])
                    mST = masks[:, 2, :].unsqueeze(1).to_broadcast([128, 4, 128])
                    S.op(V, lambda e: e.tensor_tensor(out=h3(Dm), in0=h3(E1), in1=colB(1), op=ALU.mult), reads=[bW[4], bgsm], writes=[bW[5]])
                    S.op(V, lambda e: e.tensor_tensor(out=h3(Dm), in0=h3(Dm), in1=mS, op=ALU.mult), reads=[bW[5], bC], writes=[bW[5]])
                    S.op(V, lambda e: e.tensor_tensor(out=h3(DqT), in0=h3(E2), in1=mIT, op=ALU.mult), reads=[bW[6], bC], writes=[bW[7]])
                    S.op(V, lambda e: e.tensor_tensor(out=DmT[:], in0=E2[:], in1=beRB[:], op=ALU.mult), reads=[bW[6], bW[1]], writes=[bW[8]])
                    S.op(V, lambda e: e.tensor_tensor(out=h3(DmT), in0=h3(DmT), in1=mST, op=ALU.mult), reads=[bW[8], bC], writes=[bW[8]])
                    pkk, pkkb = bank(); pqk, pqkb = bank()
                    for h in range(4):
                        kT = qkv[:, 4 + h, cs_].bitcast(F32R)
                        qT = qkv[:, h, cs_].bitcast(F32R)
                        S.op("pe", lambda e, h=h, kT=kT: e.matmul(out=pkk[:, h * 128:(h + 1) * 128], lhsT=kT, rhs=kT, start=True, stop=True), reads=[bqkv[4 + h]], writes=[pkkb])
                        S.op("pe", lambda e, h=h, kT=kT, qT=qT: e.matmul(out=pqk[:, h * 128:(h + 1) * 128], lhsT=kT, rhs=qT, start=True, stop=True), reads=[bqkv[4 + h], bqkv[h]], writes=[pqkb])
                    Nn = [W[13], W[14]]; NT = [W[15], W[16]]; RT = [W[17], W[18]]; qkT = W[19]
                    iN, iT_, iR = 13, 15, 17

                    def r32(t):
                        return t[:].bitcast(F32R)
                    S.op(V, lambda e: e.tensor_tensor(out=r32(Nn[0]), in0=pkk[:, :], in1=Dm[:], op=ALU.mult), reads=[pkkb, bW[5]], writes=[bW[13]])
                    S.op(V, lambda e: e.tensor_tensor(out=r32(NT[0]), in0=pkk[:, :], in1=DmT[:], op=ALU.mult), reads=[pkkb, bW[8]], writes=[bW[15]])
                    idB = ident[:, :].unsqueeze(1).to_broadcast([128, 4, 128])
                    S.op(V, lambda e: e.tensor_tensor(out=h3(RT[0]).bitcast(F32R), in0=idB, in1=h3(NT[0]), op=ALU.subtract), reads=[bC, bW[15]], writes=[bW[17]])
                    S.op(V, lambda e: e.tensor_tensor(out=r32(qkT), in0=pqk[:, :], in1=DqT[:], op=ALU.mult), reads=[pqkb, bW[7]], writes=[bW[19]])
                    cur = 0
                    for lev in range(1, 7):
                        nxt = 1 - cur
                        pn, pnb = bank()
                        for h in range(4):
                            hs = slice(h * 128, (h + 1) * 128)
                            S.op("pe", lambda e, hs=hs, pn=pn, cur=cur: e.matmul(out=pn[:, hs], lhsT=r32(NT[cur])[:, hs], rhs=r32(Nn[cur])[:, hs], start=True, stop=True), reads=[bW[iN + cur], bW[iT_ + cur]], writes=[pnb])
                        if lev < 6:
                            pt_, ptb = bank()
                            for h in range(4):
                                hs = slice(h * 128, (h + 1) * 128)
                                S.op("pe", lambda e, hs=hs, pt_=pt_, cur=cur: e.matmul(out=pt_[:, hs], lhsT=r32(Nn[cur])[:, hs], rhs=r32(NT[cur])[:, hs], start=True, stop=True), reads=[bW[iN + cur], bW[iT_ + cur]], writes=[ptb])
                        S.op("act", lambda e, pn=pn, nxt=nxt: e.copy(out=r32(Nn[nxt]), in_=pn[:, :]), reads=[pnb], writes=[bW[iN + nxt]])
                        if lev < 6:
                            S.op("act", lambda e, pt_=pt_, nxt=nxt: e.copy(out=r32(NT[nxt]), in_=pt_[:, :]), reads=[ptb], writes=[bW[iT_ + nxt]])
                        pr, prb = bank()
                        for h in range(4):
                            hs = slice(h * 128, (h + 1) * 128)
                            S.op("pe", lambda e, hs=hs, pr=pr, nxt=nxt, cur=cur: e.matmul(out=pr[:, hs], lhsT=r32(Nn[nxt])[:, hs], rhs=r32(RT[cur])[:, hs], start=True, stop=True), reads=[bW[iN + nxt], bW[iR + cur]], writes=[prb])
                        S.op(V, lambda e, pr=pr, nxt=nxt, cur=cur: e.tensor_tensor(out=r32(RT[nxt]), in0=pr[:, :], in1=RT[cur][:], op=ALU.add), reads=[prb, bW[iR + cur]], writes=[bW[iR + nxt]])
                        cur = nxt
                    TT_ = RT[cur]; bTT = bW[iR + cur]
                    pk, pkb = bank(); pv, pvb = bank()
                    for h in range(4):
                        hs = slice(h * 128, (h + 1) * 128)
                        S.op("pe", lambda e, h=h, hs=hs: e.transpose(out=pk[:, hs], in_=qkv[:, 4 + h, cs_], identity=ident[:]), reads=[bqkv[4 + h], bC], writes=[pkb])
                        S.op("pe", lambda e, h=h, hs=hs: e.transpose(out=pv[:, hs], in_=qkv[:, 8 + h, cs_], identity=ident[:]), reads=[bqkv[8 + h], bC], writes=[pvb])
                    kbg, kdec, vb, usb, wT, vnew = W[20], W[21], W[22], W[9], W[23], W[24]
                    iKBG, iKDEC, iVB, iUSB, iWT, iVN = 20, 21, 22, 9, 23, 24
                    S.op(V, lambda e: e.tensor_tensor(out=h3(kbg).bitcast(F32R), in0=pk[:, :].rearrange("p (a b) -> p a b", a=4), in1=colB(3), op=ALU.mult), reads=[pkb, bgsm], writes=[bW[20]])
                    S.op(V, lambda e: e.tensor_tensor(out=h3(kdec).bitcast(F32R), in0=pk[:, :].rearrange("p (a b) -> p a b", a=4), in1=colB(5), op=ALU.mult), reads=[pkb, bgsm], writes=[bW[21]])
                    S.op(V, lambda e: e.tensor_tensor(out=h3(vb).bitcast(F32R), in0=pv[:, :].rearrange("p (a b) -> p a b", a=4), in1=colB(1), op=ALU.mult), reads=[pvb, bgsm], writes=[bW[22]])
                    pu, pub = bank(); pw_, pwb = bank()
                    for h in range(4):
                        hs = slice(h * 128, (h + 1) * 128)
                        S.op("pe", lambda e, hs=hs: e.matmul(out=pu[:, hs], lhsT=r32(TT_)[:, hs], rhs=r32(vb)[:, hs], start=True, stop=True), reads=[bTT, bW[22]], writes=[pub])
                        S.op("pe", lambda e, hs=hs: e.matmul(out=pw_[:, hs], lhsT=r32(kbg)[:, hs], rhs=r32(TT_)[:, hs], start=True, stop=True), reads=[bTT, bW[20]], writes=[pwb])
                    S.op("act", lambda e: e.copy(out=usb[:], in_=pu[:, :]), reads=[pub], writes=[bW[9]])
                    S.op("act", lambda e: e.copy(out=r32(wT), in_=pw_[:, :]), reads=[pwb], writes=[bW[23]])
                    p1, p1b = bank()
                    for h in range(4):
                        hs = slice(h * 128, (h + 1) * 128)
                        S.op("pe", lambda e, h=h, hs=hs: e.matmul(out=p1[:, hs], lhsT=r32(wT)[:, hs], rhs=Sst[:, h, :], start=True, stop=True), reads=[bW[23], bS], writes=[p1b])
                    S.op(V, lambda e: e.tensor_tensor(out=r32(vnew), in0=usb[:], in1=p1[:, :], op=ALU.subtract), reads=[bW[9], p1b], writes=[bW[24]])
                    po, pob = bank(); p3_, p3b = bank()
                    for h in range(4):
                        hs = slice(h * 128, (h + 1) * 128)
                        S.op("pe", lambda e, h=h, hs=hs: e.matmul(out=po[:, hs], lhsT=Sst[:, h, :], rhs=qe[:, h, cs_], start=True, stop=False), reads=[bS, bqe], writes=[pob])
                        S.op("pe", lambda e, h=h, hs=hs: e.matmul(out=po[:, hs], lhsT=r32(vnew)[:, hs], rhs=r32(qkT)[:, hs], start=False, stop=True), reads=[bW[24], bW[19]], writes=[pob])
                        S.op("pe", lambda e, h=h, hs=hs: e.matmul(out=p3_[:, hs], lhsT=r32(kdec)[:, hs], rhs=r32(vnew)[:, hs], start=True, stop=True), reads=[bW[21], bW[24]], writes=[p3b])
                    S3 = Sst[:].bitcast(F32)
                    S.op(V, lambda e: e.tensor_tensor(out=Sst[:], in0=S3, in1=colB(6), op=ALU.mult), reads=[bS, bgsm], writes=[bS])
                    S.op(V, lambda e: e.tensor_tensor(out=Sst[:], in0=S3, in1=p3_[:, :].rearrange("p (a b) -> p a b", a=4), op=ALU.add), reads=[bS, p3b], writes=[bS])
                    osb, osq, ors = W[10], W[11], W[12]
                    S.op("act", lambda e: e.copy(out=osb[:], in_=po[:, :]), reads=[pob], writes=[bW[10]])
                    S.op("act", lambda e: e.activation(out=osq[:], in_=po[:, :], func=AF.Square), reads=[pob], writes=[bW[11]])
                    ps_, psb = bank()
                    S.op("pe", lambda e: e.matmul(out=ps_[:, :], lhsT=ones[:], rhs=osq[:], start=True, stop=True), reads=[bC, bW[11]], writes=[psb])
                    S.op("act", lambda e: e.activation(out=ors[:], in_=ps_[:, :], func=AF.Sqrt, scale=1.0 / 128.0, bias=epsnm[:, 0:1]), reads=[psb, bC], writes=[bW[12]])
                    S.op(V, lambda e: e.reciprocal(out=ors[:], in_=ors[:]), reads=[bW[12]], writes=[bW[12]])
                    S.op(V, lambda e: e.scalar_tensor_tensor(out=osb[:], in0=osb[:], scalar=nw[:, 0:1], in1=ors[:], op0=ALU.mult, op1=ALU.mult), reads=[bW[10], bW[12], bgp], writes=[bW[10]])
                    S.op(V, lambda e: e.tensor_tensor(out=ydn[:, :, cs_], in0=h3(osb), in1=zs[:, :, cs_], op=ALU.mult), reads=[bW[10], bzs], writes=[bydn])
                S.dma("sp", "st", yloc[blk * 768 + 256:blk * 768 + 768, :].rearrange("(a p) t -> p a t", p=128), ydn[:], reads=[bydn])
                if stage != 1:
                    nc.gpsimd.wait_ge(S.sem["st"], S.cnt["st"])
                    nc.gpsimd.collective_compute("AllGather", ALU.bypass, replica_groups=[[0, 1, 2, 3], [4, 5, 6, 7]], ins=[yloc[blk * 768:(blk + 1) * 768, :].opt()], outs=[yall[blk * 3072:(blk + 1) * 3072, :].opt()]).then_inc(cc_sem)
                    cc_cnt[0] += 1
                    nc.gpsimd.wait_ge(cc_sem, cc_cnt[0])

        S.barrier()
        if stage == 1:
            S.eng["pool"].wait_ge(S.sem["st"], S.cnt["st"])
            i1 = nc.gpsimd.dma_start(out=out_d[:, :].rearrange("(f p) (b t) -> b f p t", p=128, t=512), in_=yloc[:, :].rearrange("(b f p) t -> b f p t", f=6, p=128))
            S.stream("fin"); S.cnt["fin"] += 16; i1.then_inc(S.sem["fin"], 16)
            nc.gpsimd.wait_ge(S.sem["fin"], S.cnt["fin"])
            return nc

        def all_wait_cc():
            for e in ("pool", "sp", "act", "dve", "pe"):
                S.eng[e].wait_ge(cc_sem, cc_cnt[0])
        all_wait_cc()
        if stage == 15:
            i1 = nc.gpsimd.dma_start(out=out_d[:, :].rearrange("(f p) (b t) -> b f p t", p=128, t=512), in_=yall[:, :].rearrange("(b r f p) t -> b r f p t", r=4, f=6, p=128)[:, 1])
            S.stream("fin"); S.cnt["fin"] += 16; i1.then_inc(S.sem["fin"], 16)
            nc.gpsimd.wait_ge(S.sem["fin"], S.cnt["fin"])
            return nc
        core_i = None
        V = "dve"

        with ExitStack() as pb_:
            for i_ in range(2, 4):
                wbuf.append(sbt(pb_, "b_wbuf%d" % i_, [128, 16, 256], BF16)); wbb.append(Buf("wbufB%d" % i_))
            lnin_t = sbt(pb_, "b_lnin", [128, 2, 16]); blnin = Buf()
            S.dma("sp", "cst", lnin_t[:], lnin[:, :, :], writes=[blnin])
            ln1_t = sbt(pb_, "b_ln1", [128, 2, 16]); wr_t = sbt(pb_, "b_wr", [128, 16, 32]); br_t = sbt(pb_, "b_br", [32, 1]); bglu_t = sbt(pb_, "b_bglu", [128, 8])
            bpar = Buf()
            S.dma("sp", "cst", ln1_t[:], ln1[:, :, :], writes=[bpar])
            S.dma("sp", "cst", wr_t[:], wr[:, :, :], writes=[bpar])
            S.dma("sp", "cst", br_t[:], br[:, :], writes=[bpar])
            S.dma("sp", "cst", bglu_t[:], bglu[:, :], writes=[bpar])
            yidx = sbt(pb_, "b_yidx", [128, NBB * 24], I32); byidx = Buf()
            S.dma("sp", "cst", yidx[:], yidx_d[:, :], writes=[byidx])
            xt = sbt(pb_, "b_xt", [128, D]); bxt = Buf()
            xn = sbt(pb_, "b_xn", [128, D]); bxn = Buf()
            st6 = sbt(pb_, "b_st6", [128, 4, 6]); mv = sbt(pb_, "b_mv", [128, 2]); rstd = sbt(pb_, "b_rstd", [128, 1]); bst = Buf()
            hT = sbt(pb_, "b_hT", [128, 16, 512], BF16); bhT = Buf()
            hTf = sbt(pb_, "b_hTf", [128, 16, 512]); bhTf = Buf()
            ysT = sbt(pb_, "b_ysT", [128, 8, 512], BF16); bysT = Buf()
            ydT = sbt(pb_, "b_ydT", [128, 16, 512], BF16); bydT = Buf()
            ygT = sbt(pb_, "b_ygT", [128, 8, 512], BF16); bygT = Buf()
            sgs = sbt(pb_, "b_sgs", [128, 16, 512], BF16); bsgs = Buf()
            sgd = sbt(pb_, "b_sgd", [128, 16, 512], BF16); bsgd = Buf()
            h1b = sbt(pb_, "b_h1b", [128, D], BF16); bh1b = Buf()
            Wb = [sbt(pb_, "b_wk%d" % i, [128, 512]) for i in range(5)]; bWb = [Buf() for _ in range(5)]
            lgT = sbt(pb_, "b_lgT", [32, 512]); blgT = Buf()
            lg = sbt(pb_, "b_lg", [128, 4, 32]); blg = Buf()
            m8 = sbt(pb_, "b_m8", [128, 8]); gt = sbt(pb_, "b_gt", [128, 4, 32]); sm = sbt(pb_, "b_sm", [128, 4]); bsm = Buf(); bgt = Buf()
            for blk in range(NBB):
                c0 = blk * 512
                for tti in range(4):
                    layer_norm_T(pb_, xq[c0 + tti * 128:c0 + (tti + 1) * 128, :], lnin_t, blnin, [hT, hTf], [bhT, bhTf], tti * 128, xt, bxt, xn, bxn, st6, mv, rstd, bst)
                for r in range(4):
                    for ft in range(6):
                        col = blk * 24 + r * 6 + ft
                        if ft < 2:
                            S.idma("yl", ysT[:, 2 * r + ft, :], yall[:, :], yidx[:, col:col + 1], reads=[byidx], writes=[bysT])
                        else:
                            S.idma("yl", ydT[:, 4 * r + ft - 2, :], yall[:, :], yidx[:, col:col + 1], reads=[byidx], writes=[bydT])

                def ep_glu(mi, pb, pbb):
                    S.op("act", lambda e: e.activation(out=Wb[0][:], in_=pb[:, :], func=AF.Sigmoid, bias=bglu_t[:, mi:mi + 1]), reads=[pbb, bpar], writes=[bWb[0]])
                    S.op(V, lambda e: e.tensor_tensor(out=ygT[:, mi, :], in0=ysT[:, mi, :], in1=Wb[0][:], op=ALU.mult), reads=[bysT, bWb[0]], writes=[bygT])
                linear(wglu, 8, 1024, lambda k: ysT[:, k, :], [bysT], 512, ep_glu)

                def ep_gs(mi, pb, pbb):
                    S.op("act", lambda e: e.activation(out=sgs[:, mi, :], in_=pb[:, :], func=AF.Sigmoid), reads=[pbb], writes=[bsgs])

                def ep_gd(mi, pb, pbb):
                    S.op("act", lambda e: e.activation(out=sgd[:, mi, :], in_=pb[:, :], func=AF.Sigmoid), reads=[pbb], writes=[bsgd])
                linear(wg[:, 0:2048], 16, 2048, lambda k: hT[:, k, :], [bhT], 512, ep_gs)
                linear(wg[:, 2048:4096], 16, 2048, lambda k: hT[:, k, :], [bhT], 512, ep_gd)

                def ep_ps(mi, pb, pbb):
                    S.op(V, lambda e: e.tensor_tensor(out=sgs[:, mi, :], in0=sgs[:, mi, :], in1=pb[:, :], op=ALU.mult), reads=[pbb, bsgs], writes=[bsgs])

                def ep_pd(mi, pb, pbb):
                    S.op(V, lambda e: e.tensor_tensor(out=Wb[1][:], in0=sgd[:, mi, :], in1=pb[:, :], op=ALU.mult), reads=[pbb, bsgd], writes=[bWb[1]])
                    S.op(V, lambda e: e.tensor_tensor(out=sgs[:, mi, :], in0=sgs[:, mi, :], in1=Wb[1][:], op=ALU.add), reads=[bsgs, bWb[1]], writes=[bsgs])
                linear(wps, 8, 2048, lambda k: ygT[:, k, :], [bygT], 512, ep_ps)
                linear(wpd, 16, 2048, lambda k: ydT[:, k, :], [bydT], 512, ep_pd)

                def ep_out(mi, pb, pbb):
                    S.op(V, lambda e: e.scalar_tensor_tensor(out=hTf[:, mi, :], in0=hTf[:, mi, :], scalar=ALPHA, in1=pb[:, :], op0=ALU.mult, op1=ALU.add), reads=[pbb, bhTf], writes=[bhTf])
                linear(wout, 16, 2048, lambda k: sgs[:, k, :], [bsgs], 512, ep_out)

                pm, pmb = bank(); pq, pqb = bank()
                for k in range(16):
                    S.op("pe", lambda e, k=k: e.matmul(out=pm[:, :], lhsT=ones[:], rhs=hTf[:, k, :], start=(k == 0), stop=(k == 15)), reads=[bC, bhTf], writes=[pmb])
                for k in range(16):
                    sq = Wb[k % 2]
                    S.op("act", lambda e, k=k, sq=sq: e.activation(out=sq[:], in_=hTf[:, k, :], func=AF.Square), reads=[bhTf], writes=[bWb[k % 2]])
                    S.op("pe", lambda e, k=k, sq=sq: e.matmul(out=pq[:, :], lhsT=ones[:], rhs=sq[:], start=(k == 0), stop=(k == 15)), reads=[bC, bWb[k % 2]], writes=[pqb])
                mu, rs1, tq = Wb[2], Wb[3], Wb[4]
                S.op("act", lambda e: e.activation(out=mu[:], in_=pm[:, :], func=AF.Identity, scale=1.0 / D), reads=[pmb], writes=[bWb[2]])
                S.op(V, lambda e: e.tensor_tensor(out=tq[:], in0=mu[:], in1=mu[:], op=ALU.mult), reads=[bWb[2]], writes=[bWb[4]])
                S.op(V, lambda e: e.scalar_tensor_tensor(out=rs1[:], in0=pq[:, :], scalar=1.0 / D, in1=tq[:], op0=ALU.mult, op1=ALU.subtract), reads=[pqb, bWb[4]], writes=[bWb[3]])
                S.op("act", lambda e: e.activation(out=rs1[:], in_=rs1[:], func=AF.Sqrt, bias=epsln[:, 0:1]), reads=[bWb[3], bC], writes=[bWb[3]])
                S.op(V, lambda e: e.reciprocal(out=rs1[:], in_=rs1[:]), reads=[bWb[3]], writes=[bWb[3]])
                for k in range(16):
                    S.op(V, lambda e, k=k: e.tensor_tensor(out=hTf[:, k, :], in0=hTf[:, k, :], in1=mu[:], op=ALU.subtract), reads=[bhTf, bWb[2]], writes=[bhTf])
                    S.op(V, lambda e, k=k: e.tensor_tensor(out=hTf[:, k, :], in0=hTf[:, k, :], in1=rs1[:], op=ALU.mult), reads=[bhTf, bWb[3]], writes=[bhTf])
                    S.op("act", lambda e, k=k: e.activation(out=hTf[:, k, :], in_=hTf[:, k, :], func=AF.Identity, scale=ln1_t[:, 0, k:k + 1], bias=ln1_t[:, 1, k:k + 1]), reads=[bhTf, bpar], writes=[bhTf])

                pl, plb = bank()
                for k in range(16):
                    S.op("pe", lambda e, k=k: e.matmul(out=pl[0:32, :], lhsT=wr_t[:, k, :], rhs=hTf[:, k, :], start=(k == 0), stop=(k == 15)), reads=[bpar, bhTf], writes=[plb])
                S.op("act", lambda e: e.activation(out=lgT[:], in_=pl[0:32, :], func=AF.Identity, bias=br_t[:, 0:1]), reads=[plb, bpar], writes=[blgT])
                pt_, ptb = bank()
                for tti in range(4):
                    S.op("pe", lambda e, tti=tti: e.transpose(out=pt_[:, tti * 32:(tti + 1) * 32], in_=lgT[:, tti * 128:(tti + 1) * 128], identity=ident[0:32, 0:32]), reads=[blgT, bC], writes=[ptb])
                S.op("act", lambda e: e.copy(out=lg[:], in_=pt_[:, 0:128].rearrange("p (a b) -> p a b", a=4)), reads=[ptb], writes=[blg])
                for tti in range(4):
                    S.op(V, lambda e, tti=tti: e.max(out=m8[:], in_=lg[:, tti, :]), reads=[blg], writes=[bsm])
                    S.op(V, lambda e, tti=tti: e.tensor_scalar(out=gt[:, tti, :], in0=lg[:, tti, :], scalar1=m8[:, 3:4], scalar2=None, op0=ALU.is_ge), reads=[blg, bsm], writes=[bgt])
                    S.op(V, lambda e: e.tensor_scalar(out=sm[:, 0:1], in0=m8[:, 0:1], scalar1=-1.0, scalar2=None, op0=ALU.mult), reads=[bsm], writes=[bsm])
                    S.op("act", lambda e, tti=tti: e.activation(out=lg[:, tti, :], in_=lg[:, tti, :], func=AF.Exp, bias=sm[:, 0:1]), reads=[blg, bsm], writes=[blg])
                    S.op(V, lambda e, tti=tti: e.tensor_tensor(out=gt[:, tti, :], in0=gt[:, tti, :], in1=lg[:, tti, :], op=ALU.mult), reads=[blg, bgt], writes=[bgt])
                    S.op(V, lambda e, tti=tti: e.reduce_sum(out=sm[:, 1:2], in_=gt[:, tti, :], axis=AX.X), reads=[bgt], writes=[bsm])
                    S.op(V, lambda e: e.reciprocal(out=sm[:, 1:2], in_=sm[:, 1:2]), reads=[bsm], writes=[bsm])
                    S.op(V, lambda e, tti=tti: e.tensor_scalar(out=gt[:, tti, :], in0=gt[:, tti, :], scalar1=sm[:, 1:2], scalar2=None, op0=ALU.mult), reads=[bgt, bsm], writes=[bgt])
                S.dma("sp", "st", gate_loc[c0:c0 + 512, :].rearrange("(a p) e -> p a e", p=128), gt[:], reads=[bgt])

                for tti in range(4):
                    for kq in range(4):
                        pb, pbb = bank()
                        for r in range(4):
                            k = kq * 4 + r
                            S.op("pe", lambda e, k=k, r=r, pb=pb, tti=tti: e.transpose(out=pb[:, r * 128:(r + 1) * 128], in_=hTf[:, k, tti * 128:(tti + 1) * 128], identity=ident[:]), reads=[bhTf, bC], writes=[pbb])
                        S.op("act", lambda e, pb=pb, kq=kq: e.copy(out=xn[:, kq * 512:(kq + 1) * 512], in_=pb[:, :]), reads=[pbb], writes=[bxn])
                        S.op(V, lambda e, pb=pb, kq=kq: e.tensor_copy(out=h1b[:, kq * 512:(kq + 1) * 512], in_=pb[:, :]), reads=[pbb], writes=[bh1b])
                    S.dma("sp", "st", h1tok[c0 + tti * 128:c0 + (tti + 1) * 128, :], xn[:], reads=[bxn])
                    S.dma("sp", "st", h1b_loc[c0 + tti * 128:c0 + (tti + 1) * 128, :], h1b[:], reads=[bh1b])
        del wbuf[2:], wbb[2:]
        wrr[0] = 0
        S.barrier()
        if stage == 2:
            nc.gpsimd.wait_ge(S.sem["st"], S.cnt["st"])
            i1 = nc.gpsimd.dma_start(out=out_d[:, :], in_=h1tok[:, :])
            S.stream("fin"); S.cnt["fin"] += 16; i1.then_inc(S.sem["fin"], 16)
            nc.gpsimd.wait_ge(S.sem["fin"], S.cnt["fin"])
            return nc

        nc.gpsimd.wait_ge(S.sem["st"], S.cnt["st"])
        G4 = [[0, 1, 2, 3], [4, 5, 6, 7]]
        for k in range(HC):
            nc.gpsimd.collective_compute("AllGather", ALU.bypass, replica_groups=G4, ins=[h1b_loc[256 * k:256 * (k + 1), :].opt()], outs=[h1b_all[1024 * k:1024 * (k + 1), :].opt()]).then_inc(cc_sem)
            cc_cnt[0] += 1
            nc.gpsimd.wait_ge(cc_sem, cc_cnt[0])
        nc.gpsimd.collective_compute("AllGather", ALU.bypass, replica_groups=G4, ins=[gate_loc.ap().opt()], outs=[gate_all.ap().opt()]).then_inc(cc_sem)
        cc_cnt[0] += 1
        all_wait_cc()

        NTt = TPC // 128
        SL = 4 * CAP
        with ExitStack() as pr_:
            posm = sbt(pr_, "r_posm", [128, 4, NTt, NEX]); Gr = sbt(pr_, "r_G", [128, 4, NTt, NEX]); brt = Buf()
            iotaC = sbt(pr_, "r_iotaC", [128, CAP]); iotap = sbt(pr_, "r_iotap", [128, 3]); sel8 = sbt(pr_, "r_sel8", [8, 8, 128]); brc = Buf()
            S.dma("sp", "cst", iotaC[:], iota_d[:, 0:CAP], writes=[brc])
            S.dma("sp", "cst", iotap[:], iotap_d[:, :], writes=[brc])
            S.dma("sp", "cst", sel8[:], sel8_d[:, :, :], writes=[brc])
            with ExitStack() as p1_:
                Gf = sbt(p1_, "r_Gf", [128, NTt, 32]); bGf = Buf()
                Gp = sbt(p1_, "r_Gp", [128, NTt, NEX, 32]); bGp = Buf()
                esel = sbt(p1_, "r_esel", [128, NEX, 32]); besel = Buf()
                mk = sbt(p1_, "r_mk", [128, NTt, NEX]); cum = sbt(p1_, "r_cum", [128, NTt, NEX]); bmk = Buf()
                S.dma("sp", "cst", esel[:], esel_d[:, :, :], writes=[besel])
                for r in range(4):
                    S.dma("sp", "xl", Gf[:], gate_all[r * TPC:(r + 1) * TPC, :].rearrange("(a p) e -> p a e", p=128), writes=[bGf])
                    S.op(V, lambda e: e.tensor_tensor(out=Gp[:], in0=Gf[:].unsqueeze(2).to_broadcast([128, NTt, NEX, 32]), in1=esel[:].unsqueeze(1).to_broadcast([128, NTt, NEX, 32]), op=ALU.mult), reads=[bGf, besel], writes=[bGp])
                    S.op(V, lambda e, r=r: e.reduce_sum(out=Gr[:, r].rearrange("p a b -> p (a b)"), in_=Gp[:].rearrange("p a b c -> p (a b) c"), axis=AX.X), reads=[bGp], writes=[brt])
                    S.op(V, lambda e, r=r: e.tensor_scalar(out=mk[:], in0=Gr[:, r], scalar1=0.0, scalar2=None, op0=ALU.is_gt), reads=[brt], writes=[bmk])
                    S.op(V, lambda e: e.memset(cum[:, 0, :], 0.0), reads=[bmk], writes=[bmk])
                    for i in range(1, NTt):
                        S.op(V, lambda e, i=i: e.tensor_tensor(out=cum[:, i, :], in0=cum[:, i - 1, :], in1=mk[:, i - 1, :], op=ALU.add), reads=[bmk], writes=[bmk])
                    for i0 in range(0, NTt, 32):
                        pb, pbb = bank()
                        for i in range(i0, min(NTt, i0 + 32)):
                            oc = (i - i0) * NEX
                            S.op("pe", lambda e, i=i, oc=oc, pb=pb: e.matmul(out=pb[:, oc:oc + NEX], lhsT=masks[:, 2, :], rhs=mk[:, i, :], start=True, stop=False), reads=[bC, bmk], writes=[pbb])
                            S.op("pe", lambda e, i=i, oc=oc, pb=pb: e.matmul(out=pb[:, oc:oc + NEX], lhsT=ones[:], rhs=cum[:, i, :], start=False, stop=True), reads=[bC, bmk], writes=[pbb])
                        n_ = min(NTt, i0 + 32) - i0
                        S.op(V, lambda e, i0=i0, n_=n_, pb=pb, r=r: e.scalar_tensor_tensor(out=posm[:, r, i0:i0 + n_, :], in0=pb[:, 0:n_ * NEX].rearrange("p (a b) -> p a b", b=NEX), scalar=1.0, in1=mk[:, i0:i0 + n_, :], op0=ALU.add, op1=ALU.mult), reads=[pbb, bmk], writes=[brt])
                        S.op(V, lambda e, i0=i0, n_=n_, r=r: e.tensor_scalar(out=posm[:, r, i0:i0 + n_, :], in0=posm[:, r, i0:i0 + n_, :], scalar1=-1.0, scalar2=None, op0=ALU.add), reads=[brt], writes=[brt])
            S.barrier()

            with ExitStack() as p2_:
                H = sbt(p2_, "g_H", [128, NTt, D], BF16); bH = Buf()
                Sels = [sbt(p2_, "g_Sel%d" % i_, [128, NTt, CAP], BF16) for i_ in range(2)]; bSels = [Buf(), Buf()]
                XTss = [sbt(p2_, "g_XTs%d" % i_, [128, 16, CAP], BF16) for i_ in range(2)]; bXTss = [Buf(), Buf()]
                g_rr = 0
                for r in range(4):
                    for k in range(HC):
                        S.dma("sp", "xl", H[:, 2 * k:2 * k + 2, :], h1b_all[(k * 4 + r) * 256:(k * 4 + r + 1) * 256, :].rearrange("(a p) d -> p a d", p=128), writes=[bH])
                    for ex in range(NEX):
                        Sel, bSel, XTs, bXTs = Sels[g_rr % 2], bSels[g_rr % 2], XTss[g_rr % 2], bXTss[g_rr % 2]
                        g_rr += 1
                        for i in range(NTt):
                            S.op(V, lambda e, i=i: e.tensor_scalar(out=Sel[:, i, :], in0=iotaC[:], scalar1=posm[:, r, i, ex:ex + 1], scalar2=None, op0=ALU.is_equal), reads=[brc, brt], writes=[bSel])
                        for kc in range(16):
                            pb, pbb = bank()
                            for i in range(NTt):
                                S.op("pe", lambda e, i=i, kc=kc, pb=pb: e.matmul(out=pb[:, 0:CAP], lhsT=H[:, i, kc * 128:(kc + 1) * 128], rhs=Sel[:, i, :], start=(i == 0), stop=(i == NTt - 1)), reads=[bH, bSel], writes=[pbb])
                            S.op("act", lambda e, kc=kc, pb=pb: e.copy(out=XTs[:, kc, :], in_=pb[:, 0:CAP]), reads=[pbb], writes=[bXTs])
                        S.dma("sp", "st", XTd[ex * D:(ex + 1) * D, r * CAP:(r + 1) * CAP].rearrange("(k p) c -> p k c", p=128), XTs[:], reads=[bXTs])
            S.barrier()
            nc.sync.wait_ge(S.sem["st"], S.cnt["st"])

            with ExitStack() as pm_:
                for i_ in range(2, 4):
                    wbuf.append(sbt(pm_, "m_wbuf%d" % i_, [128, 16, 256], BF16)); wbb.append(Buf("wbufM%d" % i_))
                XT = sbt(pm_, "m_XT", [128, 16, SL], BF16); bXT = Buf()
                AT = sbt(pm_, "m_AT", [128, 16, SL], BF16); bAT = Buf()
                w2 = [sbt(pm_, "m_w2%d" % i, [128, 16, 512], BF16) for i in range(2)]; bw2 = [Buf(), Buf()]
                w2rr = [0]
                Wm = [sbt(pm_, "m_wk%d" % i, [128, 512]) for i in range(3)]; bWm = [Buf() for _ in range(3)]
                Yst = [sbt(pm_, "m_Yst%d" % i, [128, 512], BF16) for i in range(2)]; bYst = [Buf(), Buf()]
                bdB = sbt(pm_, "m_bdB", [128, D]); bbdB = Buf()
                bgu_t = sbt(pm_, "m_bgu", [128, NEX, 32]); bbgu = Buf()
                S.dma("sp", "cst", bgu_t[:], bgu[:, :, :], writes=[bbgu])
                yrr = 0
                for ex in range(NEX):
                    S.dma("sp", "xl", XT[:], XTd[ex * D:(ex + 1) * D, :].rearrange("(k p) c -> p k c", p=128), writes=[bXT])
                    S.dma("sp", "bl", bdB[:], bdn[ex:ex + 1, :].to_broadcast([128, D]), writes=[bbdB])
                    for ft in range(16):
                        i = wrr[0] % len(wbuf); wrr[0] = (i + 1) % len(wbuf)
                        S.dma("pool", "wl%d" % i, wbuf[i][:, :, 0:128], wgu[ex, :, ft * 128:(ft + 1) * 128].rearrange("(kc p) m -> p kc m", p=128), writes=[wbb[i]])
                        S.dma("pool", "wl%d" % i, wbuf[i][:, :, 128:256], wgu[ex, :, 2048 + ft * 128:2048 + (ft + 1) * 128].rearrange("(kc p) m -> p kc m", p=128), writes=[wbb[i]])
                        for nh in range(SL // 512):
                            ns = slice(nh * 512, (nh + 1) * 512)
                            pg, pgb = bank(); pu, pub = bank()
                            for k in range(16):
                                S.op("pe", lambda e, k=k, i=i, pg=pg: e.matmul(out=pg[:, :], lhsT=wbuf[i][:, k, 0:128], rhs=XT[:, k, ns], start=(k == 0), stop=(k == 15)), reads=[wbb[i], bXT], writes=[pgb])
                            for k in range(16):
                                S.op("pe", lambda e, k=k, i=i, pu=pu: e.matmul(out=pu[:, :], lhsT=wbuf[i][:, k, 128:256], rhs=XT[:, k, ns], start=(k == 0), stop=(k == 15)), reads=[wbb[i], bXT], writes=[pub])
                            g_, s_, u_ = Wm
                            S.op(V, lambda e, pg=pg: e.tensor_scalar(out=g_[:], in0=pg[:, :], scalar1=bgu_t[:, ex, ft:ft + 1], scalar2=7.0, op0=ALU.add, op1=ALU.min), reads=[pgb, bbgu], writes=[bWm[0]])
                            S.op("act", lambda e: e.activation(out=s_[:], in_=g_[:], func=AF.Sigmoid, scale=1.702), reads=[bWm[0]], writes=[bWm[1]])
                            S.op(V, lambda e, pu=pu: e.tensor_scalar(out=u_[:], in0=pu[:, :], scalar1=bgu_t[:, ex, 16 + ft:17 + ft], scalar2=7.0, op0=ALU.add, op1=ALU.min), reads=[pub, bbgu], writes=[bWm[2]])
                            S.op(V, lambda e: e.tensor_scalar(out=u_[:], in0=u_[:], scalar1=-7.0, scalar2=1.0, op0=ALU.max, op1=ALU.add), reads=[bWm[2]], writes=[bWm[2]])
                            S.op(V, lambda e: e.tensor_tensor(out=g_[:], in0=g_[:], in1=s_[:], op=ALU.mult), reads=[bWm[0], bWm[1]], writes=[bWm[0]])
                            S.op(V, lambda e: e.tensor_tensor(out=AT[:, ft, ns], in0=g_[:], in1=u_[:], op=ALU.mult), reads=[bWm[0], bWm[2]], writes=[bAT])
                    for dq in range(4):
                        j = w2rr[0]; w2rr[0] = 1 - j
                        S.dma("pool", "w2l%d" % j, w2[j][:], wdn[ex, :, dq * 512:(dq + 1) * 512].rearrange("(kc p) m -> p kc m", p=128), writes=[bw2[j]])
                        ds_ = slice(dq * 512, (dq + 1) * 512)
                        for sti in range(SL // 128):
                            pd, pdb = bank()
                            for k in range(16):
                                S.op("pe", lambda e, k=k, j=j, pd=pd, sti=sti: e.matmul(out=pd[:, :], lhsT=AT[:, k, sti * 128:(sti + 1) * 128], rhs=w2[j][:, k, :], start=(k == 0), stop=(k == 15)), reads=[bw2[j], bAT], writes=[pdb])
                            yb = yrr % 2; yrr += 1
                            S.op(V, lambda e, pd=pd, yb=yb: e.tensor_tensor(out=Yst[yb][:], in0=pd[:, :], in1=bdB[:, ds_], op=ALU.add), reads=[pdb, bbdB], writes=[bYst[yb]])
                            S.dma("sp", "st", Yd[ex * SL + sti * 128:ex * SL + (sti + 1) * 128, ds_], Yst[yb][:], reads=[bYst[yb]])
            del wbuf[2:], wbb[2:]
            wrr[0] = 0
            S.barrier()
            nc.sync.wait_ge(S.sem["st"], S.cnt["st"])

            with ExitStack() as p4_:
                NTc = RPC // 128
                SGs = [sbt(p4_, "s_SG%d" % i_, [128, NEX, 3, RPC], BF16) for i_ in range(2)]; bSGs = [Buf(), Buf()]
                Yts = [sbt(p4_, "s_Yt%d" % i_, [128, NEX, 3, 512], BF16) for i_ in range(2)]; bYts = [Buf(), Buf()]
                posTs = [sbt(p4_, "s_posT%d" % i_, [8, RPC]) for i_ in range(2)]; gTs = [sbt(p4_, "s_gT%d" % i_, [8, RPC]) for i_ in range(2)]; bpTs = [Buf(), Buf()]
                gsbs = [sbt(p4_, "s_gsb%d" % i_, [128, 512]) for i_ in range(2)]; bgsbs = [Buf(), Buf()]
                ost = [sbt(p4_, "s_ost%d" % i, [128, 512]) for i in range(2)]; bost = [Buf(), Buf()]
                orr = 0; y_rr = 0; s_rr = 0; g_rr2 = 0
                for kch in range(NRS):
                    for r in range(4):
                        SG, bSG = SGs[s_rr % 2], bSGs[s_rr % 2]
                        posT, gT, bpT = posTs[s_rr % 2], gTs[s_rr % 2], bpTs[s_rr % 2]
                        s_rr += 1
                        pp_, ppb = bank(); pg_, pgb_ = bank()
                        for ii in range(NTc):
                            i = kch * NTc + ii
                            oc = ii * 128
                            S.op("pe", lambda e, i=i, oc=oc, pp_=pp_: e.transpose(out=pp_[0:8, oc:oc + 128], in_=posm[:, r, i, :], identity=ident[:]), reads=[brt, bC], writes=[ppb])
                            S.op("pe", lambda e, i=i, oc=oc, pg_=pg_: e.transpose(out=pg_[0:8, oc:oc + 128], in_=Gr[:, r, i, :], identity=ident[:]), reads=[brt, bC], writes=[pgb_])
                        S.op("act", lambda e, pp_=pp_: e.copy(out=posT[:, :], in_=pp_[0:8, 0:RPC]), reads=[ppb], writes=[bpT])
                        S.op("act", lambda e, pg_=pg_: e.copy(out=gT[:, :], in_=pg_[0:8, 0:RPC]), reads=[pgb_], writes=[bpT])
                        for ex in range(NEX):
                            pp_, ppb = bank(); pg_, pgb_ = bank()
                            gsb, bgsb = gsbs[g_rr2 % 2], bgsbs[g_rr2 % 2]
                            g_rr2 += 1
                            S.op("pe", lambda e, pp_=pp_: e.matmul(out=pp_[:, 0:RPC], lhsT=sel8[:, ex, :], rhs=posT[:, :], start=True, stop=True), reads=[brc, bpT], writes=[ppb])
                            S.op("pe", lambda e, pg_=pg_: e.matmul(out=pg_[:, 0:RPC], lhsT=sel8[:, ex, :], rhs=gT[:, :], start=True, stop=True), reads=[brc, bpT], writes=[pgb_])
                            S.op("act", lambda e, pg_=pg_: e.copy(out=gsb[:, 0:RPC], in_=pg_[:, 0:RPC]), reads=[pgb_], writes=[bgsb])
                            for cc in range(3):
                                S.op(V, lambda e, cc=cc, pp_=pp_: e.scalar_tensor_tensor(out=SG[:, ex, cc, :], in0=pp_[:, 0:RPC], scalar=iotap[:, cc:cc + 1], in1=gsb[:, 0:RPC], op0=ALU.is_equal, op1=ALU.mult), reads=[ppb, brc, bgsb], writes=[bSG])
                        for dq in range(4):
                            ds_ = slice(dq * 512, (dq + 1) * 512)
                            Yt, bYt = Yts[y_rr % 2], bYts[y_rr % 2]
                            y_rr += 1
                            for ex in range(NEX):
                                S.dma("sp", "yt%d" % (y_rr % 2), Yt[:, ex, :, :], Yd[ex * SL + r * CAP:ex * SL + (r + 1) * CAP, ds_].rearrange("(a p) d -> p a d", p=128), writes=[bYt])
                            for ii in range(NTc):
                                po_, pob_ = bank()
                                n = 0
                                for ex in range(NEX):
                                    for cc in range(3):
                                        S.op("pe", lambda e, ex=ex, cc=cc, ii=ii, n=n, po_=po_: e.matmul(out=po_[:, :], lhsT=SG[:, ex, cc, ii * 128:(ii + 1) * 128], rhs=Yt[:, ex, cc, :], start=(n == 0), stop=(n == NEX * 3 - 1)), reads=[bSG, bYt], writes=[pob_])
                                        n += 1
                                ob = orr % 2; orr += 1
                                S.op("act", lambda e, po_=po_, ob=ob: e.copy(out=ost[ob][:], in_=po_[:, :]), reads=[pob_], writes=[bost[ob]])
                                row = (kch * 4 + r) * RPC + ii * 128
                                S.dma("sp", "st", dense[row:row + 128, ds_], ost[ob][:], reads=[bost[ob]])
                    nc.gpsimd.wait_ge(S.sem["st"], S.cnt["st"])
                    nc.gpsimd.collective_compute("ReduceScatter", ALU.add, replica_groups=[[0, 1, 2, 3], [4, 5, 6, 7]], ins=[dense[kch * 4 * RPC:(kch + 1) * 4 * RPC, :].opt()], outs=[ffn[kch * RPC:(kch + 1) * RPC, :].opt()]).then_inc(cc_sem)
                    cc_cnt[0] += 1
                    nc.gpsimd.wait_ge(cc_sem, cc_cnt[0])
        S.barrier()
        all_wait_cc()

        with ExitStack() as pf:
            g2 = sbt(pf, "f_g2", [128, D]); b2 = sbt(pf, "f_b2", [128, D]); bg2 = Buf()
            S.dma("sp", "cst", g2[:], ln2[0:1, :].to_broadcast([128, D]), writes=[bg2])
            S.dma("sp", "cst", b2[:], ln2[1:2, :].to_broadcast([128, D]), writes=[bg2])
            ha = sbt(pf, "f_ha", [128, D]); fa = sbt(pf, "f_fa", [128, D]); bha, bfa = Buf(), Buf()
            st6 = sbt(pf, "f_st6", [128, 4, 6]); mv = sbt(pf, "f_mv", [128, 2]); rstd = sbt(pf, "f_rstd", [128, 1]); bst = Buf()
            fin_toks = []
            for tti in range(TPC // 128):
                rows = slice(tti * 128, (tti + 1) * 128)
                S.dma("sp", "xl", ha[:], h1tok[rows, :], writes=[bha])
                S.dma("sp", "xl", fa[:], ffn[rows, :], writes=[bfa])
                S.op(V, lambda e: e.scalar_tensor_tensor(out=ha[:], in0=ha[:], scalar=ALPHA, in1=fa[:], op0=ALU.mult, op1=ALU.add), reads=[bha, bfa], writes=[bha])
                for q in range(4):
                    S.op(V, lambda e, q=q: e.bn_stats(out=st6[:, q, :], in_=ha[:, q * 512:(q + 1) * 512]), reads=[bha], writes=[bst])
                S.op(V, lambda e: e.bn_aggr(out=mv[:], in_=st6[:].rearrange("p a b -> p (a b)")), reads=[bst], writes=[bst])
                S.op("act", lambda e: e.activation(out=rstd[:], in_=mv[:, 1:2], func=AF.Sqrt, bias=epsln[:, 0:1]), reads=[bst, bC], writes=[bst])
                S.op(V, lambda e: e.reciprocal(out=rstd[:], in_=rstd[:]), reads=[bst], writes=[bst])
                S.op(V, lambda e: e.tensor_scalar(out=fa[:], in0=ha[:], scalar1=mv[:, 0:1], scalar2=rstd[:, 0:1], op0=ALU.subtract, op1=ALU.mult), reads=[bha, bst], writes=[bfa])
                S.op(V, lambda e: e.tensor_tensor(out=fa[:], in0=fa[:], in1=g2[:], op=ALU.mult), reads=[bfa, bg2], writes=[bfa])
                S.op(V, lambda e: e.tensor_tensor(out=fa[:], in0=fa[:], in1=b2[:], op=ALU.add), reads=[bfa, bg2], writes=[bfa])
                fin_toks.append(S.dma("sp", "fin", out_d[rows, :], fa[:], reads=[bfa]))
            S.eng["sp"].wait_ge(S.sem["fin"], S.cnt["fin"])
    return nc


def _consts():
    c = np.arange(128)
    ident = np.eye(128, dtype=np.float32)
    m0 = (c[:, None] > c[None, :]).astype(np.float32)
    m1 = (c[None, :] >= c[:, None]).astype(np.float32)
    m2 = (c[None, :] > c[:, None]).astype(np.float32)
    masks = np.stack([m0, m1, m2], axis=1)
    iota = np.broadcast_to(np.arange(384, dtype=np.float32)[None, :], (128, 384)).copy()
    iotap = (np.arange(128, dtype=np.float32)[:, None] + 128.0 * np.arange(3, dtype=np.float32)[None, :]).copy()
    sel8 = np.zeros((8, 8, 128), np.float32)
    for h in range(8):
        sel8[h, h, :] = 1.0
    sel4 = np.zeros((4, 4, 128), np.float32)
    for h in range(4):
        sel4[h, h, :] = 1.0
    return dict(ident=ident, masks=masks, iota=iota, sel4=sel4, iotap=iotap, sel8=sel8)


def _core_inputs(inp, core, SEQ, stage):
    b, j = core // 4, core % 4
    f = np.float32
    m = {}
    m["x"] = np.ascontiguousarray(inp["x"][b, :SEQ])
    m["lnin"] = np.ascontiguousarray(np.stack([inp["ln_in_g"].reshape(16, 128).T, inp["ln_in_b"].reshape(16, 128).T], axis=1)).astype(f)
    w_in = inp["w_in"][0]
    o = 0
    u_c = w_in[:, 256 * j:256 * (j + 1)]
    base = 1024
    q_c = w_in[:, base + 512 * j: base + 512 * (j + 1)]
    k_c = w_in[:, base + 2048 + 512 * j: base + 2048 + 512 * (j + 1)]
    v_c = w_in[:, base + 4096 + 512 * j: base + 4096 + 512 * (j + 1)]
    z_c = w_in[:, base + 6144 + 512 * j: base + 6144 + 512 * (j + 1)]
    a_c = w_in[:, base + 8192 + 4 * j: base + 8192 + 4 * (j + 1)]
    b_c = w_in[:, base + 8192 + 16 + 4 * j: base + 8192 + 16 + 4 * (j + 1)]
    m["wA"] = np.ascontiguousarray(np.concatenate([u_c, q_c, k_c, v_c, z_c], axis=1))
    m["wab"] = np.ascontiguousarray(np.concatenate([a_c, b_c], axis=1))
    g0 = 16 * j

    def pairlay(a):
        sh = a.shape[2:]
        a = a.reshape((8, 2, 64) + sh)
        a = np.moveaxis(a, 0, 2)
        return np.ascontiguousarray(a.reshape((128, 8) + sh))
    lam_re = inp["lam_re"][0][g0:g0 + 16]
    lam_im = inp["lam_im"][0][g0:g0 + 16]
    lstep = np.broadcast_to(inp["log_step"][0][g0:g0 + 16][:, None], (16, 64))
    m["s5p"] = pairlay(np.stack([lam_re, lam_im, lstep], axis=-1)).astype(f)
    m["s5b"] = pairlay(np.stack([inp["ssm_b_re"][0][g0:g0 + 16], inp["ssm_b_im"][0][g0:g0 + 16]], axis=2)).astype(f)
    cre = np.transpose(inp["ssm_c_re"][0][g0:g0 + 16], (0, 2, 1))
    cim = np.transpose(inp["ssm_c_im"][0][g0:g0 + 16], (0, 2, 1))
    m["s5c"] = pairlay(np.stack([cre, cim], axis=2)).astype(f)
    m["s5d"] = np.ascontiguousarray(inp["ssm_d"][0][g0:g0 + 16].reshape(2, 128).T).astype(f)
    cwf = inp["conv_w"][0][:, 0, :]
    tiles = []
    for part in range(3):
        for h in range(4):
            c0 = part * 2048 + (4 * j + h) * 128
            tiles.append(cwf[:, c0:c0 + 128].T)
    m["convw"] = np.ascontiguousarray(np.stack(tiles, axis=1)).astype(f)
    m["hpar"] = np.ascontiguousarray(np.stack([inp["a_log"][0][4 * j:4 * j + 4], inp["dt_bias"][0][4 * j:4 * j + 4]], axis=1)).astype(f)
    m["dnw"] = np.ascontiguousarray(inp["dn_norm_w"][0].reshape(128, 1)).astype(f)
    m.update(_consts())
    if stage == 15:
        return m
    if stage >= 2:
        NBLK = SEQ // 512
        TPC = SEQ // 4
        NBB = TPC // 512
        i = j
        m["xq"] = np.ascontiguousarray(inp["x"][b, i * TPC:(i + 1) * TPC])
        yidx = np.zeros((128, NBB * 24), np.int32)
        p = np.arange(128, dtype=np.int64)
        for blk in range(NBB):
            gb = i * NBB + blk
            for r in range(4):
                for ft in range(6):
                    yidx[:, blk * 24 + r * 6 + ft] = ((gb * 4 + r) * 6 + ft) * 128 + p
        m["yidx"] = yidx
        m["wg"] = np.ascontiguousarray(w_in[:, 9248:13344])
        m["wglu"] = np.ascontiguousarray(inp["w_glu"][0])
        m["bglu"] = np.ascontiguousarray(inp["b_glu"][0].reshape(8, 128).T).astype(f)
        m["wps"] = np.ascontiguousarray(inp["w_proj_ssm"][0])
        m["wpd"] = np.ascontiguousarray(inp["w_proj_dn"][0])
        m["wout"] = np.ascontiguousarray(inp["w_out"][0])
        m["ln1"] = np.ascontiguousarray(np.stack([inp["ln1_g"][0].reshape(16, 128).T, inp["ln1_b"][0].reshape(16, 128).T], axis=1)).astype(f)
        m["wr"] = np.ascontiguousarray(np.transpose(inp["w_router"][0].reshape(16, 128, 32), (1, 0, 2))).astype(f)
        m["br"] = np.ascontiguousarray(inp["b_router"][0].reshape(32, 1)).astype(f)
    if stage >= 9:
        e0 = 8 * j
        m["wgu"] = np.ascontiguousarray(inp["w_gate_up"][0][e0:e0 + 8])
        m["bgu"] = np.ascontiguousarray(np.transpose(inp["b_gate_up"][0][e0:e0 + 8].reshape(8, 32, 128), (2, 0, 1))).astype(f)
        m["wdn"] = np.ascontiguousarray(inp["w_down"][0][e0:e0 + 8])
        m["bdn"] = np.ascontiguousarray(inp["b_down"][0][e0:e0 + 8]).astype(f)
        m["ln2"] = np.ascontiguousarray(np.stack([inp["ln2_g"][0], inp["ln2_b"][0]], axis=0)).astype(f)
        es = np.zeros((128, 8, 32), f)
        for jj in range(8):
            es[:, jj, e0 + jj] = 1.0
        m["esel"] = es
    return m


def _run(inputs, SEQ, stage):
    nc = build(SEQ, stage)
    in_maps = [_core_inputs(inputs, c, SEQ, stage) for c in range(8)]
    res = run_bass_kernel_spmd(nc, in_maps, core_ids=list(range(8)))
    return [r["out"] for r in res.results]


def kernel(**inputs):
    SEQ = inputs["x"].shape[1]
    outs = _run(inputs, SEQ, 9)
    TPC = SEQ // 4
    out = np.zeros((2, SEQ, D), np.float32)
    for c in range(8):
        b, i = c // 4, c % 4
        out[b, i * TPC:(i + 1) * TPC] = outs[c]
    return out
```

```python
import numpy as np
from contextlib import ExitStack
import concourse.bass as bass
import concourse.mybir as mybir
from concourse.bass_utils import run_bass_kernel_spmd

F32 = mybir.dt.float32
F32R = mybir.dt.float32r
BF16 = mybir.dt.bfloat16
I32 = mybir.dt.int32
ALU = mybir.AluOpType
AF = mybir.ActivationFunctionType
AX = mybir.AxisListType

D = 2048
ALPHA = 2.0 ** 0.25
LN_EPS = 1e-5
NORM_EPS = 1e-6
MAGIC = 12582912.0
TWO_PI = float(2 * np.pi)


class Buf:
    __slots__ = ("name", "w", "r")

    def __init__(self, name=""):
        self.name = name
        self.w = None
        self.r = {}


class Sched:
    def __init__(self, nc, es):
        self.nc = nc
        self.es = es
        self.eng = {"pe": nc.tensor, "act": nc.scalar, "dve": nc.vector, "pool": nc.gpsimd, "sp": nc.sync}
        self.sem, self.cnt, self.step = {}, {}, {}
        for n in self.eng:
            self.sem[n] = es.enter_context(nc.semaphore("sem_" + n))
            self.cnt[n] = 0
            self.step[n] = 1
        self.seen = {n: {} for n in self.eng}

    def stream(self, n, step=16):
        if n not in self.sem:
            self.sem[n] = self.es.enter_context(self.nc.semaphore("sem_" + n))
            self.cnt[n] = 0
            self.step[n] = step
        return n

    def _wait(self, e, tok):
        if tok is None:
            return
        s, c = tok
        if s == e and e == "pe":
            return
        if self.seen[e].get(s, 0) >= c:
            return
        self.eng[e].wait_ge(self.sem[s], c)
        self.seen[e][s] = c

    def deps(self, e, reads, writes, extra=()):
        best = {}

        def add(t):
            if t is not None and best.get(t[0], 0) < t[1]:
                best[t[0]] = t[1]
        for b in reads:
            add(b.w)
        for b in writes:
            add(b.w)
            for s, c in b.r.items():
                add((s, c))
        for t in extra:
            add(t)
        for s, c in best.items():
            self._wait(e, (s, c))

    def mark(self, tok, reads, writes):
        s, c = tok
        for b in reads:
            if b.r.get(s, 0) < c:
                b.r[s] = c
        for b in writes:
            b.w = tok
            b.r = {}

    def op(self, e, fn, reads=(), writes=(), extra=()):
        self.deps(e, reads, writes, extra)
        inst = fn(self.eng[e])
        self.cnt[e] += 1
        inst.then_inc(self.sem[e], 1)
        tok = (e, self.cnt[e])
        self.mark(tok, reads, writes)
        return tok

    def dma(self, q, stream, out, in_, reads=(), writes=(), extra=()):
        self.stream(stream)
        self.deps(q, reads, writes, extra)
        inst = self.eng[q].dma_start(out=out, in_=in_)
        self.cnt[stream] += 16
        inst.then_inc(self.sem[stream], 16)
        tok = (stream, self.cnt[stream])
        self.mark(tok, reads, writes)
        return tok

    def idma(self, stream, out, in_, idx_ap, reads=(), writes=()):
        self.stream(stream)
        self.deps("pool", reads, writes)
        inst = self.nc.gpsimd.indirect_dma_start(out=out, out_offset=None, in_=in_, in_offset=bass.IndirectOffsetOnAxis(ap=idx_ap, axis=0))
        self.cnt[stream] += 16
        inst.then_inc(self.sem[stream], 16)
        tok = (stream, self.cnt[stream])
        self.mark(tok, reads, writes)
        return tok

    def barrier(self):
        for e in self.eng:
            for s in list(self.sem):
                if self.cnt[s] > 0 and not (s == e and e == "pe"):
                    self._wait(e, (s, self.cnt[s]))


def build(SEQ=8192, stage=9):
    nc = bass.Bass("TRN2", target_bir_lowering=False)
    NBLK = SEQ // 512
    TPC = SEQ // 4
    NBB = TPC // 512
    NTOK = 4 * TPC
    TB = min(1024, TPC)
    NTB = NTOK // TB
    NEX = 8
    RPC = min(512, TPC)
    NRS = TPC // RPC

    def din(name, shape, dt=F32):
        return nc.dram_tensor(name, shape, dt, kind="ExternalInput").ap()

    x = din("x", [SEQ, D])
    lnin = din("lnin", [128, 2, 16])
    wA = din("wA", [D, 2304])
    wab = din("wab", [D, 8])
    s5p = din("s5p", [128, 8, 3])
    s5b = din("s5b", [128, 8, 2, 16])
    s5c = din("s5c", [128, 8, 2, 16])
    s5d = din("s5d", [128, 2])
    convw = din("convw", [128, 12, 4])
    hpar = din("hpar", [4, 2])
    dnw = din("dnw", [128, 1])
    ident_d = din("ident", [128, 128])
    masks_d = din("masks", [128, 3, 128])
    iota_d = din("iota", [128, 384])
    iotap_d = din("iotap", [128, 3])
    sel8_d = din("sel8", [8, 8, 128])
    sel4_d = din("sel4", [4, 4, 128])
    if stage >= 2 and stage != 15:
        xq = din("xq", [TPC, D])
        yidx_d = din("yidx", [128, NBB * 24], I32)
        wg = din("wg", [D, 4096])
        wglu = din("wglu", [1024, 1024])
        bglu = din("bglu", [128, 8])
        wps = din("wps", [1024, D])
        wpd = din("wpd", [D, D])
        wout = din("wout", [D, D])
        ln1 = din("ln1", [128, 2, 16])
        wr = din("wr", [128, 16, 32])
        br = din("br", [32, 1])
    if stage >= 9 and stage != 15:
        wgu = din("wgu", [NEX, D, 4096])
        bgu = din("bgu", [128, NEX, 32])
        wdn = din("wdn", [NEX, D, D])
        bdn = din("bdn", [NEX, D])
        ln2 = din("ln2", [2, D])
        esel_d = din("esel", [128, NEX, 32])

    if stage in (1, 15):
        out_d = nc.dram_tensor("out", [768, SEQ], BF16, kind="ExternalOutput").ap()
    else:
        out_d = nc.dram_tensor("out", [TPC, D], F32, kind="ExternalOutput").ap()

    yloc = nc.dram_tensor("yloc", [NBLK * 768, 512], BF16)
    yall = nc.dram_tensor("yall", [4 * NBLK * 768, 512], BF16)
    if stage >= 2 and stage != 15:
        HC = TPC // 256
        h1b_loc = nc.dram_tensor("h1b_loc", [TPC, D], BF16)
        h1b_all = nc.dram_tensor("h1b_all", [HC * 4 * 256, D], BF16)
        CAP = 384
        XTd = nc.dram_tensor("XTd", [NEX * D, 4 * CAP], BF16)
        Yd = nc.dram_tensor("Yd", [NEX * 4 * CAP, D], BF16)
        gate_loc = nc.dram_tensor("gate_loc", [TPC, 32], F32)
        gate_all = nc.dram_tensor("gate_all", [4 * TPC, 32], F32)
        h1tok = nc.dram_tensor("h1tok", [TPC, D], F32)
        dense = nc.dram_tensor("dense", [NRS * 4 * RPC, D], F32)
        ffn = nc.dram_tensor("ffn", [TPC, D], F32)

    with ExitStack() as es:
        S = Sched(nc, es)
        cc_sem = es.enter_context(nc.semaphore("cc_sem"))
        cc_cnt = [0]

        def sbt(stack, name, shape, dt=F32):
            return stack.enter_context(nc.sbuf_tensor("s_" + name, shape, dt))

        banks = [es.enter_context(nc.psum_tensor("pb%d" % i, [128, 512], F32)) for i in range(8)]
        bbuf = [Buf("pb%d" % i) for i in range(8)]
        brr = [0]

        def bank():
            i = brr[0]
            brr[0] = (i + 1) % 8
            return banks[i], bbuf[i]

        ident = sbt(es, "ident", [128, 128])
        ones = sbt(es, "ones", [128, 128])
        masks = sbt(es, "masks", [128, 3, 128])
        epsln = sbt(es, "epsln", [128, 1])
        epsnm = sbt(es, "epsnm", [128, 1])
        bC = Buf("consts")
        S.dma("sp", "cst", ident[:], ident_d[:, :], writes=[bC])
        S.dma("sp", "cst", masks[:], masks_d[:, :, :], writes=[bC])
        S.op("dve", lambda e: e.memset(ones[:], 1.0), writes=[bC])
        S.op("dve", lambda e: e.memset(epsln[:], LN_EPS), writes=[bC])
        S.op("dve", lambda e: e.memset(epsnm[:], NORM_EPS), writes=[bC])

        wbuf = [sbt(es, "wbuf%d" % i, [128, 16, 256], BF16) for i in range(2)]
        wbb = [Buf("wbuf%d" % i) for i in range(2)]
        wrr = [0]

        def load_w(src_ap, kc, ncols):
            i = wrr[0] % len(wbuf)
            wrr[0] = (i + 1) % len(wbuf)
            S.dma("pool", "wl%d" % i, wbuf[i][:, 0:kc, 0:ncols], src_ap.rearrange("(kc p) m -> p kc m", p=128), writes=[wbb[i]])
            return wbuf[i], wbb[i]

        def linear(wsrc, kc, mcols, rhs_fn, rhs_bufs, n, epilogue, group=256):
            mi = 0
            for c0 in range(0, mcols, group):
                gc = min(group, mcols - c0)
                wt, wb = load_w(wsrc[:, c0:c0 + gc], kc, gc)
                for m0 in range(0, gc, 128):
                    mw = min(128, gc - m0)
                    pb, pbb = bank()
                    for k in range(kc):
                        S.op("pe", lambda e, k=k, m0=m0, mw=mw, pb=pb: e.matmul(out=pb[0:mw, 0:n], lhsT=wt[:, k, m0:m0 + mw], rhs=rhs_fn(k), start=(k == 0), stop=(k == kc - 1)),
                             reads=[wb] + list(rhs_bufs), writes=[pbb])
                    epilogue(mi, pb, pbb)
                    mi += 1

        def layer_norm_T(pst, xsrc_rows, gb_tile, bgb, dstT, bdst, tcol0, xt, bxt, xn, bxn, st6, mv, rstd, bst):
            S.dma("sp", "xl", xt[:], xsrc_rows, writes=[bxt])
            for q in range(4):
                S.op("dve", lambda e, q=q: e.bn_stats(out=st6[:, q, :], in_=xt[:, q * 512:(q + 1) * 512]), reads=[bxt], writes=[bst])
            S.op("dve", lambda e: e.bn_aggr(out=mv[:], in_=st6[:].rearrange("p a b -> p (a b)")), reads=[bst], writes=[bst])
            S.op("act", lambda e: e.activation(out=rstd[:], in_=mv[:, 1:2], func=AF.Sqrt, bias=epsln[:, 0:1]), reads=[bst, bC], writes=[bst])
            S.op("dve", lambda e: e.reciprocal(out=rstd[:], in_=rstd[:]), reads=[bst], writes=[bst])
            S.op("dve", lambda e: e.tensor_scalar(out=xn[:], in0=xt[:], scalar1=mv[:, 0:1], scalar2=rstd[:, 0:1], op0=ALU.subtract, op1=ALU.mult), reads=[bxt, bst], writes=[bxn])
            for kq in range(4):
                pb, pbb = bank()
                for r in range(4):
                    k = kq * 4 + r
                    S.op("pe", lambda e, k=k, r=r, pb=pb: e.transpose(out=pb[:, r * 128:(r + 1) * 128], in_=xn[:, k * 128:(k + 1) * 128], identity=ident[:]), reads=[bxn, bC], writes=[pbb])
                for r in range(4):
                    k = kq * 4 + r
                    for (dt_, bd_) in zip(dstT, bdst):
                        S.op("act", lambda e, k=k, r=r, pb=pb, dt_=dt_: e.activation(out=dt_[:, k, tcol0:tcol0 + 128], in_=pb[:, r * 128:(r + 1) * 128], func=AF.Identity, scale=gb_tile[:, 0, k:k + 1], bias=gb_tile[:, 1, k:k + 1]),
                             reads=[pbb, bgb], writes=[bd_])

        with ExitStack() as pa:
            lnin_t = sbt(pa, "lnin_t", [128, 2, 16]); blnin = Buf()
            S.dma("sp", "cst", lnin_t[:], lnin[:, :, :], writes=[blnin])
            xt = sbt(pa, "xt", [128, D]); bxt = Buf()
            xn = sbt(pa, "xn", [128, D]); bxn = Buf()
            st6 = sbt(pa, "st6", [128, 4, 6]); mv = sbt(pa, "mv", [128, 2]); rstd = sbt(pa, "rstd", [128, 1]); bst = Buf()
            hT = sbt(pa, "hT", [128, 16, 512], BF16); bhT = Buf()
            iota = sbt(pa, "iota", [128, 132]); biota = Buf()
            S.dma("sp", "cst", iota[:], iota_d[:, 0:132], writes=[biota])

            p5 = sbt(pa, "p5", [128, 8, 3]); b5 = sbt(pa, "b5", [128, 8, 2, 16]); c5 = sbt(pa, "c5", [128, 8, 2, 16]); d5 = sbt(pa, "d5", [128, 2])
            bp5 = Buf()
            S.dma("sp", "cst", p5[:], s5p[:, :, :], writes=[bp5])
            S.dma("sp", "cst", b5[:], s5b[:, :, :, :], writes=[bp5])
            S.dma("sp", "cst", c5[:], s5c[:, :, :, :], writes=[bp5])
            S.dma("sp", "cst", d5[:], s5d[:, :], writes=[bp5])
            sc = sbt(pa, "s5sc", [128, 16, 8]); bsc = Buf()
            LR, LI, STEP, MAG, TH, RHO, COS1, SIN1, ARE, AIM, DEN, FRE, FIM, T0, T1, T2 = [sc[:, i, :] for i in range(16)]
            V = "dve"

            def ts(out, in0, s1, s2, o0, o1=None):
                if o1 is None:
                    S.op(V, lambda e: e.tensor_scalar(out=out, in0=in0, scalar1=s1, scalar2=None, op0=o0), reads=[bsc, bp5], writes=[bsc])
                else:
                    S.op(V, lambda e: e.tensor_scalar(out=out, in0=in0, scalar1=s1, scalar2=s2, op0=o0, op1=o1), reads=[bsc, bp5], writes=[bsc])

            def tt(out, a, b, op):
                S.op(V, lambda e: e.tensor_tensor(out=out, in0=a, in1=b, op=op), reads=[bsc, bp5], writes=[bsc])

            def act(out, in_, func, scale=1.0, bias=0.0):
                S.op("act", lambda e: e.activation(out=out, in_=in_, func=func, scale=scale, bias=bias), reads=[bsc, bp5, bC], writes=[bsc])

            def sincos(out_s, out_c, ang, t0, t1):
                for (o, sh) in ((out_s, 0.0), (out_c, float(np.pi / 2))):
                    ts(t0, ang, sh, None, ALU.add)
                    ts(t1, t0, float(1 / TWO_PI), MAGIC, ALU.mult, ALU.add)
                    ts(t1, t1, -MAGIC, None, ALU.add)
                    S.op(V, lambda e, t0=t0, t1=t1: e.scalar_tensor_tensor(out=t0, in0=t1, scalar=-TWO_PI, in1=t0, op0=ALU.mult, op1=ALU.add), reads=[bsc], writes=[bsc])
                    act(o, t0, AF.Sin)

            act(STEP, p5[:, :, 2], AF.Exp)
            tt(T0, p5[:, :, 0], STEP, ALU.mult)
            act(RHO, T0, AF.Exp)
            tt(TH, p5[:, :, 1], STEP, ALU.mult)
            sincos(SIN1, COS1, TH, T0, T1)
            tt(ARE, RHO, COS1, ALU.mult)
            tt(AIM, RHO, SIN1, ALU.mult)
            tt(T0, p5[:, :, 0], p5[:, :, 0], ALU.mult)
            tt(T1, p5[:, :, 1], p5[:, :, 1], ALU.mult)
            tt(DEN, T0, T1, ALU.add)
            S.op(V, lambda e: e.reciprocal(out=DEN, in_=DEN), reads=[bsc], writes=[bsc])
            ts(T2, ARE, -1.0, None, ALU.add)
            tt(T0, T2, p5[:, :, 0], ALU.mult)
            tt(T1, AIM, p5[:, :, 1], ALU.mult)
            tt(T0, T0, T1, ALU.add)
            tt(FRE, T0, DEN, ALU.mult)
            tt(T0, AIM, p5[:, :, 0], ALU.mult)
            tt(T1, T2, p5[:, :, 1], ALU.mult)
            tt(T0, T0, T1, ALU.subtract)
            tt(FIM, T0, DEN, ALU.mult)
            bb = sbt(pa, "bb", [128, 8, 2, 16]); tb = sbt(pa, "tbb", [128, 8, 16])
            freB = sc[:, 11, :].unsqueeze(2).to_broadcast([128, 8, 16])
            fimB = sc[:, 12, :].unsqueeze(2).to_broadcast([128, 8, 16])
            tt(bb[:, :, 0, :], b5[:, :, 0, :], freB, ALU.mult)
            tt(tb[:], b5[:, :, 1, :], fimB, ALU.mult)
            tt(bb[:, :, 0, :], bb[:, :, 0, :], tb[:], ALU.subtract)
            tt(bb[:, :, 1, :], b5[:, :, 1, :], freB, ALU.mult)
            tt(tb[:], b5[:, :, 0, :], fimB, ALU.mult)
            tt(bb[:, :, 1, :], bb[:, :, 1, :], tb[:], ALU.add)
            BbTz = sbt(pa, "BbTz", [128, 8, 2, 128]); CTz = sbt(pa, "CTz", [128, 8, 2, 128]); bdp = sbt(pa, "bdp", [128, 128])
            btab = Buf()
            S.op(V, lambda e: e.memset(CTz[:].rearrange("p a b c -> p (a b c)"), 0.0), writes=[btab])
            for gp in range(8):
                gl = gp % 4
                for ri in range(2):
                    S.op(V, lambda e: e.memset(bdp[:], 0.0), reads=[bsc], writes=[bsc])
                    for g2 in range(2):
                        c0 = 32 * gl + 16 * g2
                        S.op(V, lambda e, g2=g2, c0=c0, gp=gp, ri=ri: e.tensor_copy(out=bdp[64 * g2:64 * g2 + 64, c0:c0 + 16], in_=bb[64 * g2:64 * g2 + 64, gp, ri, :]), reads=[bsc], writes=[bsc])
                        if ri == 0:
                            S.op(V, lambda e, g2=g2, c0=c0, gp=gp: e.tensor_copy(out=CTz[64 * g2:64 * g2 + 64, gp, 0, c0:c0 + 16], in_=c5[64 * g2:64 * g2 + 64, gp, 0, :]), reads=[bp5], writes=[btab])
                        else:
                            S.op(V, lambda e, g2=g2, c0=c0, gp=gp: e.tensor_scalar(out=CTz[64 * g2:64 * g2 + 64, gp, 1, c0:c0 + 16], in0=c5[64 * g2:64 * g2 + 64, gp, 1, :], scalar1=-1.0, scalar2=None, op0=ALU.mult), reads=[bp5], writes=[btab])
                    pb, pbb = bank()
                    S.op("pe", lambda e, pb=pb: e.transpose(out=pb[:, 0:128], in_=bdp[:], identity=ident[:]), reads=[bsc, bC], writes=[pbb])
                    S.op("act", lambda e, pb=pb, gp=gp, ri=ri: e.copy(out=BbTz[:, gp, ri, :].bitcast(F32R), in_=pb[:, 0:128]), reads=[pbb], writes=[btab])
            cst = sbt(pa, "cstab", [128, 8, 2, 132]); rhoT = sbt(pa, "rhoT", [128, 8, 128]); ph = sbt(pa, "ph", [128, 132]); pt0 = sbt(pa, "pt0", [128, 132]); pt1 = sbt(pa, "pt1", [128, 132])
            for gp in range(8):
                S.op(V, lambda e, gp=gp: e.tensor_scalar(out=ph[:], in0=iota[:], scalar1=sc[:, 4, gp:gp + 1], scalar2=None, op0=ALU.mult), reads=[bsc, biota], writes=[bsc])
                sincos(cst[:, gp, 1, :], cst[:, gp, 0, :], ph[:], pt0[:], pt1[:])
                S.op(V, lambda e, gp=gp: e.tensor_scalar(out=rhoT[:, gp, :], in0=ones[:], scalar1=sc[:, 5, gp:gp + 1], scalar2=None, op0=ALU.mult), reads=[bsc, bC], writes=[bsc])
            S.op(V, lambda e: e.tensor_copy(out=ph[:, 0:1], in_=ph[:, 0:1]), reads=[bsc], writes=[btab])
            carry = sbt(pa, "carry", [128, 8, 2]); bcar = Buf()
            S.op(V, lambda e: e.memset(carry[:].rearrange("p a b -> p (a b)"), 0.0), writes=[bcar])

            cw = sbt(pa, "cw", [128, 12, 4]); hp = sbt(pa, "hp", [4, 2]); nw = sbt(pa, "nw", [128, 1]); sel4 = sbt(pa, "sel4", [4, 4, 128])
            bgp = Buf()
            S.dma("sp", "cst", cw[:], convw[:, :, :], writes=[bgp])
            S.dma("sp", "cst", hp[:], hpar[:, :], writes=[bgp])
            S.dma("sp", "cst", nw[:], dnw[:, :], writes=[bgp])
            S.dma("sp", "cst", sel4[:], sel4_d[:, :, :], writes=[bgp])
            nexpA = sbt(pa, "nexpA", [4, 1])
            S.op("act", lambda e: e.activation(out=nexpA[:], in_=hp[:, 0:1], func=AF.Exp), reads=[bgp], writes=[bgp])
            S.op(V, lambda e: e.tensor_scalar(out=nexpA[:], in0=nexpA[:], scalar1=-1.0, scalar2=None, op0=ALU.mult), reads=[bgp], writes=[bgp])
            ones4 = sbt(pa, "ones4", [4, 128])
            S.op(V, lambda e: e.memset(ones4[:], 1.0), writes=[bgp])
            halo = sbt(pa, "halo", [128, 12, 3]); bhalo = Buf()
            S.op(V, lambda e: e.memset(halo[:].rearrange("p a b -> p (a b)"), 0.0), writes=[bhalo])
            Sst = sbt(pa, "Sst", [128, 4, 128], F32R); bS = Buf()
            S.op(V, lambda e: e.tensor_scalar(out=Sst[:], in0=ones[:, :].unsqueeze(1).to_broadcast([128, 4, 128]), scalar1=0.0, scalar2=None, op0=ALU.mult), reads=[bC], writes=[bS])

            uT = sbt(pa, "uT", [128, 2, 512]); buT = Buf()
            rtmp = [sbt(pa, "rtmp%d" % i, [128, 515]) for i in range(2)]; brtmp = [Buf(), Buf()]
            qkv = sbt(pa, "qkv", [128, 12, 512]); bqkv = [Buf() for _ in range(12)]
            qe = sbt(pa, "qe", [128, 4, 512], F32R); bqe = Buf()
            zs = sbt(pa, "zs", [128, 4, 512], BF16); bzs = Buf()
            abr = sbt(pa, "abr", [4, 2, 512]); babr = Buf()
            ysb = sbt(pa, "ysb", [128, 2, 512], BF16); bysb = Buf()
            ydn = sbt(pa, "ydn", [128, 4, 512], BF16); bydn = Buf()
            W = [sbt(pa, "wk%d" % i, [128, 512]) for i in range(25)]
            bW = [Buf("wk%d" % i) for i in range(25)]
            gsm = sbt(pa, "gsm", [128, 8, 16]); bgsm = Buf()
            grow = sbt(pa, "grow", [4, 3, 512]); bgrow = Buf()

            for blk in range(NBLK):
                t0 = blk * 512
                for tti in range(4):
                    layer_norm_T(pa, x[t0 + tti * 128:t0 + (tti + 1) * 128, :], lnin_t, blnin, [hT], [bhT], tti * 128, xt, bxt, xn, bxn, st6, mv, rstd, bst)

                def ep_A(mi, pb, pbb):
                    if mi < 2:
                        S.op("act", lambda e: e.copy(out=uT[:, mi, :].bitcast(F32R), in_=pb[:, :]), reads=[pbb], writes=[buT])
                    elif mi < 14:
                        ct = mi - 2
                        r = rtmp[ct % 2]; br_ = brtmp[ct % 2]
                        S.op("act", lambda e: e.copy(out=r[:, 3:515], in_=pb[:, :]), reads=[pbb], writes=[br_])
                        S.op("act", lambda e: e.copy(out=r[:, 0:3], in_=halo[:, ct, :]), reads=[bhalo], writes=[br_])
                        S.op("act", lambda e: e.copy(out=halo[:, ct, :], in_=r[:, 512:515]), reads=[br_], writes=[bhalo])
                        acc = W[0]; bacc = bW[0]
                        S.op(V, lambda e: e.tensor_scalar(out=acc[:], in0=r[:, 3:515], scalar1=cw[:, ct, 3:4], scalar2=None, op0=ALU.mult), reads=[br_, bgp], writes=[bacc])
                        for jj in (2, 1, 0):
                            S.op(V, lambda e, jj=jj: e.scalar_tensor_tensor(out=acc[:], in0=r[:, jj:jj + 512], scalar=cw[:, ct, jj:jj + 1], in1=acc[:], op0=ALU.mult, op1=ALU.add), reads=[br_, bgp, bacc], writes=[bacc])
                        qo = qkv[:, ct, :].bitcast(F32R)
                        S.op("act", lambda e: e.activation(out=qo, in_=acc[:], func=AF.Silu), reads=[bacc], writes=[bqkv[ct]])
                    else:
                        hh = mi - 14
                        S.op("act", lambda e: e.activation(out=zs[:, hh, :], in_=pb[:, :], func=AF.Silu), reads=[pbb], writes=[bzs])

                linear(wA, 16, 2304, lambda k: hT[:, k, :], [bhT], 512, ep_A)

                def ep_ab(mi, pb, pbb):
                    S.op("act", lambda e: e.copy(out=abr[:, mi, :], in_=pb[0:4, :]), reads=[pbb], writes=[babr])
                for which in range(2):
                    i = wrr[0] % len(wbuf); wrr[0] = (i + 1) % len(wbuf)
                    S.dma("pool", "wl%d" % i, wbuf[i][:, 0:16, 0:4], wab[:, which * 4:which * 4 + 4].rearrange("(kc p) m -> p kc m", p=128), writes=[wbb[i]])
                    pb, pbb = bank()
                    for k in range(16):
                        S.op("pe", lambda e, k=k, i=i, pb=pb: e.matmul(out=pb[0:4, :], lhsT=wbuf[i][:, k, 0:4], rhs=hT[:, k, :], start=(k == 0), stop=(k == 15)), reads=[wbb[i], bhT], writes=[pbb])
                    ep_ab(which, pb, pbb)

                for hf in range(2):
                    ypb, ypbb = bank()
                    xs = []
                    for gl in range(4):
                        gp = hf * 4 + gl
                        bure, bbre = bank(); buim, bbim = bank()
                        S.op("pe", lambda e, gp=gp, bure=bure: e.matmul(out=bure[:, :], lhsT=BbTz[:, gp, 0, :].bitcast(F32R), rhs=uT[:, hf, :].bitcast(F32R), start=True, stop=True), reads=[btab, buT], writes=[bbre])
                        S.op("pe", lambda e, gp=gp, buim=buim: e.matmul(out=buim[:, :], lhsT=BbTz[:, gp, 1, :].bitcast(F32R), rhs=uT[:, hf, :].bitcast(F32R), start=True, stop=True), reads=[btab, buT], writes=[bbim])
                        cB = cst[:, gp, 0, 0:128].unsqueeze(1).to_broadcast([128, 4, 128])
                        sB = cst[:, gp, 1, 0:128].unsqueeze(1).to_broadcast([128, 4, 128])
                        t1, t2, btr, bti = W[0], W[1], W[2], W[3]
                        xre, xim = W[4 + 2 * gl], W[5 + 2 * gl]
                        wl = [bW[0], bW[1], bW[2], bW[3]]

                        def v3(t):
                            return t[:].rearrange("p (a b) -> p a b", a=4)

                        def p3(t):
                            return t[:, :].rearrange("p (a b) -> p a b", a=4)
                        S.op(V, lambda e: e.tensor_tensor(out=v3(t1), in0=p3(bure), in1=cB, op=ALU.mult), reads=[bbre, btab], writes=[bW[0]])
                        S.op(V, lambda e: e.tensor_tensor(out=v3(t2), in0=p3(buim), in1=sB, op=ALU.mult), reads=[bbim, btab], writes=[bW[1]])
                        S.op(V, lambda e: e.tensor_tensor(out=btr[:], in0=t1[:], in1=t2[:], op=ALU.add), reads=[bW[0], bW[1]], writes=[bW[2]])
                        S.op(V, lambda e: e.tensor_tensor(out=v3(t1), in0=p3(buim), in1=cB, op=ALU.mult), reads=[bbim, btab], writes=[bW[0]])
                        S.op(V, lambda e: e.tensor_tensor(out=v3(t2), in0=p3(bure), in1=sB, op=ALU.mult), reads=[bbre, btab], writes=[bW[1]])
                        S.op(V, lambda e: e.tensor_tensor(out=bti[:], in0=t1[:], in1=t2[:], op=ALU.subtract), reads=[bW[0], bW[1]], writes=[bW[3]])
                        for sb_ in range(4):
                            cs_ = slice(sb_ * 128, (sb_ + 1) * 128)
                            S.op(V, lambda e, cs_=cs_: e.tensor_tensor_scan(out=t1[:, cs_], data0=rhoT[:, gp, :], data1=btr[:, cs_], initial=carry[:, gp, 0:1], op0=ALU.mult, op1=ALU.add), reads=[bW[2], btab, bcar], writes=[bW[0]])
                            S.op(V, lambda e, cs_=cs_: e.tensor_tensor_scan(out=t2[:, cs_], data0=rhoT[:, gp, :], data1=bti[:, cs_], initial=carry[:, gp, 1:2], op0=ALU.mult, op1=ALU.add), reads=[bW[3], btab, bcar], writes=[bW[1]])
                            e1 = sb_ * 128 + 127
                            cr, ci = cst[:, gp, 0, 128:129], cst[:, gp, 1, 128:129]
                            tmpc = gsm[:, 7, 0:2]
                            S.op(V, lambda e: e.tensor_scalar(out=tmpc[:, 0:1], in0=t2[:, e1:e1 + 1], scalar1=ci, scalar2=-1.0, op0=ALU.mult, op1=ALU.mult), reads=[bW[1], btab], writes=[bgsm])
                            S.op(V, lambda e: e.tensor_scalar(out=tmpc[:, 1:2], in0=t1[:, e1:e1 + 1], scalar1=ci, scalar2=None, op0=ALU.mult), reads=[bW[0], btab], writes=[bgsm])
                            S.op(V, lambda e: e.scalar_tensor_tensor(out=carry[:, gp, 0:1], in0=t1[:, e1:e1 + 1], scalar=cr, in1=tmpc[:, 0:1], op0=ALU.mult, op1=ALU.add), reads=[bW[0], bgsm, btab], writes=[bcar])
                            S.op(V, lambda e: e.scalar_tensor_tensor(out=carry[:, gp, 1:2], in0=t2[:, e1:e1 + 1], scalar=cr, in1=tmpc[:, 1:2], op0=ALU.mult, op1=ALU.add), reads=[bW[1], bgsm, btab], writes=[bcar])
                        S.op(V, lambda e: e.tensor_tensor(out=v3(btr), in0=v3(t1), in1=cB, op=ALU.mult), reads=[bW[0], btab], writes=[bW[2]])
                        S.op(V, lambda e: e.tensor_tensor(out=v3(bti), in0=v3(t2), in1=sB, op=ALU.mult), reads=[bW[1], btab], writes=[bW[3]])
                        S.op(V, lambda e: e.tensor_tensor(out=xre[:], in0=btr[:], in1=bti[:], op=ALU.subtract), reads=[bW[2], bW[3]], writes=[bW[4 + 2 * gl]])
                        S.op(V, lambda e: e.tensor_tensor(out=v3(btr), in0=v3(t2), in1=cB, op=ALU.mult), reads=[bW[1], btab], writes=[bW[2]])
                        S.op(V, lambda e: e.tensor_tensor(out=v3(bti), in0=v3(t1), in1=sB, op=ALU.mult), reads=[bW[0], btab], writes=[bW[3]])
                        S.op(V, lambda e: e.tensor_tensor(out=xim[:], in0=btr[:], in1=bti[:], op=ALU.add), reads=[bW[2], bW[3]], writes=[bW[5 + 2 * gl]])
                        xs.append((gp, xre, xim, bW[4 + 2 * gl], bW[5 + 2 * gl]))
                    for n_, (gp, xre, xim, b1_, b2_) in enumerate(xs):
                        S.op("pe", lambda e, gp=gp, xre=xre: e.matmul(out=ypb[:, :], lhsT=CTz[:, gp, 0, :], rhs=xre[:], start=(n_ == 0), stop=False), reads=[btab, b1_], writes=[ypbb])
                        S.op("pe", lambda e, gp=gp, xim=xim: e.matmul(out=ypb[:, :], lhsT=CTz[:, gp, 1, :], rhs=xim[:], start=False, stop=(n_ == 3)), reads=[btab, b2_], writes=[ypbb])
                    yt = W[12]
                    S.op(V, lambda e: e.scalar_tensor_tensor(out=yt[:], in0=uT[:, hf, :], scalar=d5[:, hf:hf + 1], in1=ypb[:, :], op0=ALU.mult, op1=ALU.add), reads=[buT, bp5, ypbb], writes=[bW[12]])
                    S.op("act", lambda e: e.activation(out=ysb[:, hf, :], in_=yt[:], func=AF.Gelu), reads=[bW[12]], writes=[bysb])
                S.dma("sp", "st", yloc[blk * 768:blk * 768 + 256, :].rearrange("(a p) t -> p a t", p=128), ysb[:], reads=[bysb])

                for ct in range(8):
                    i0_ = 2 * (ct % 4); i1_ = i0_ + 1
                    sq = W[i0_]
                    S.op("act", lambda e: e.activation(out=sq[:], in_=qkv[:, ct, :], func=AF.Square), reads=[bqkv[ct]], writes=[bW[i0_]])
                    pb, pbb = bank()
                    S.op("pe", lambda e, pb=pb: e.matmul(out=pb[:, :], lhsT=ones[:], rhs=sq[:], start=True, stop=True), reads=[bC, bW[i0_]], writes=[pbb])
                    rs = W[i1_]
                    S.op("act", lambda e, pb=pb: e.activation(out=rs[:], in_=pb[:, :], func=AF.Sqrt, bias=epsnm[:, 0:1]), reads=[pbb, bC], writes=[bW[i1_]])
                    S.op(V, lambda e: e.reciprocal(out=rs[:], in_=rs[:]), reads=[bW[i1_]], writes=[bW[i1_]])
                    S.op(V, lambda e, ct=ct: e.scalar_tensor_tensor(out=qkv[:, ct, :].bitcast(F32R), in0=qkv[:, ct, :], scalar=(128.0 ** -0.5 if ct < 4 else 1.0), in1=rs[:], op0=ALU.mult, op1=ALU.mult), reads=[bqkv[ct], bW[i1_]], writes=[bqkv[ct]])
                S.op("act", lambda e: e.activation(out=grow[:, 2, :], in_=abr[:, 1, :], func=AF.Sigmoid), reads=[babr], writes=[bgrow])
                S.op("act", lambda e: e.activation(out=grow[:, 0, :], in_=abr[:, 0, :], func=AF.Exp, bias=hp[:, 1:2]), reads=[babr, bgp], writes=[bgrow])
                S.op("act", lambda e: e.activation(out=grow[:, 0, :], in_=grow[:, 0, :], func=AF.Ln, bias=1.0), reads=[bgrow], writes=[bgrow])
                S.op(V, lambda e: e.tensor_scalar(out=grow[:, 0, :], in0=grow[:, 0, :], scalar1=nexpA[:, 0:1], scalar2=None, op0=ALU.mult), reads=[bgrow, bgp], writes=[bgrow])
                for ch in range(4):
                    cs_ = slice(ch * 128, (ch + 1) * 128)
                    S.op(V, lambda e, cs_=cs_: e.tensor_tensor_scan(out=grow[:, 1, cs_], data0=ones4[:, :], data1=grow[:, 0, cs_], initial=0.0, op0=ALU.mult, op1=ALU.add), reads=[bgrow, bgp], writes=[bgrow])
                pbc, pbcb = bank()
                for ch in range(4):
                    for kind, row in ((0, 1), (1, 2)):
                        S.op("pe", lambda e, ch=ch, kind=kind, row=row: e.transpose(out=pbc[:, kind * 16 + ch * 4:kind * 16 + ch * 4 + 4], in_=grow[:, row, ch * 128:(ch + 1) * 128], identity=ident[0:4, 0:4]), reads=[bgrow, bC], writes=[pbcb])
                S.op("act", lambda e: e.copy(out=gsm[:, 0:2, :], in_=pbc[:, 0:32].rearrange("p (a b) -> p a b", a=2)), reads=[pbcb], writes=[bgsm])

                for ch in range(4):
                    cs_ = slice(ch * 128, (ch + 1) * 128)
                    hc = slice(ch * 4, ch * 4 + 4)
                    pg, pgb = bank(); pbt, pbtb = bank()
                    for h in range(4):
                        S.op("pe", lambda e, h=h, pg=pg: e.matmul(out=pg[:, h * 128:(h + 1) * 128], lhsT=sel4[:, h, :], rhs=grow[:, 1, cs_], start=True, stop=True), reads=[bgp, bgrow], writes=[pgb])
                        S.op("pe", lambda e, h=h, pbt=pbt: e.matmul(out=pbt[:, h * 128:(h + 1) * 128], lhsT=sel4[:, h, :], rhs=grow[:, 2, cs_], start=True, stop=True), reads=[bgp, bgrow], writes=[pbtb])
                    gcRB, beRB, egRB = W[0], W[1], W[2]
                    S.op("act", lambda e, pg=pg: e.copy(out=gcRB[:], in_=pg[:, :]), reads=[pgb], writes=[bW[0]])
                    S.op("act", lambda e, pbt=pbt: e.copy(out=beRB[:], in_=pbt[:, :]), reads=[pbtb], writes=[bW[1]])
                    S.op("act", lambda e, pg=pg: e.activation(out=egRB[:], in_=pg[:, :], func=AF.Exp), reads=[pgb], writes=[bW[2]])

                    def h3(t):
                        return t[:].rearrange("p (a b) -> p a b", a=4)

                    def colB(kind):
                        return gsm[:, kind, hc].unsqueeze(2).to_broadcast([128, 4, 128])
                    S.op("act", lambda e: e.activation(out=gsm[:, 2, hc], in_=gsm[:, 0, hc], func=AF.Exp), reads=[bgsm], writes=[bgsm])
                    S.op(V, lambda e: e.tensor_tensor(out=gsm[:, 3, hc], in0=gsm[:, 1, hc], in1=gsm[:, 2, hc], op=ALU.mult), reads=[bgsm], writes=[bgsm])
                    S.op(V, lambda e: e.tensor_copy(out=gsm[:, 4, hc], in_=h3(gcRB)[:, :, 127]), reads=[bW[0]], writes=[bgsm])
                    S.op(V, lambda e: e.tensor_tensor(out=gsm[:, 5, hc], in0=gsm[:, 4, hc], in1=gsm[:, 0, hc], op=ALU.subtract), reads=[bgsm], writes=[bgsm])
                    S.op("act", lambda e: e.activation(out=gsm[:, 5, hc], in_=gsm[:, 5, hc], func=AF.Exp), reads=[bgsm], writes=[bgsm])
                    S.op("act", lambda e: e.activation(out=gsm[:, 6, hc], in_=gsm[:, 4, hc], func=AF.Exp), reads=[bgsm], writes=[bgsm])
                    for h in range(4):
                        S.op(V, lambda e, h=h: e.tensor_tensor(out=qe[:, h, cs_], in0=qkv[:, h, cs_], in1=egRB[:, h * 128:(h + 1) * 128], op=ALU.mult), reads=[bqkv[h], bW[2]], writes=[bqe])
                    A1, E1, Dm, E2, DqT, DmT = W[3], W[4], W[5], W[6], W[7], W[8]
                    S.op(V, lambda e: e.tensor_tensor(out=h3(A1), in0=h3(gcRB), in1=colB(0), op=ALU.subtract), reads=[bW[0], bgsm], writes=[bW[3]])
                    S.op(V, lambda e: e.tensor_scalar(out=E2[:], in0=A1[:], scalar1=0.0, scalar2=None, op0=ALU.min), reads=[bW[3]], writes=[bW[6]])
                    S.op(V, lambda e: e.tensor_scalar(out=A1[:], in0=A1[:], scalar1=0.0, scalar2=None, op0=ALU.max), reads=[bW[3]], writes=[bW[3]])
                    S.op("act", lambda e: e.activation(out=E1[:], in_=A1[:], func=AF.Exp, scale=-1.0), reads=[bW[3]], writes=[bW[4]])
                    S.op("act", lambda e: e.activation(out=E2[:], in_=E2[:], func=AF.Exp), reads=[bW[6]], writes=[bW[6]])
                    mS = masks[:, 0, :].unsqueeze(1).to_broadcast([128, 4, 128])
                    mIT = masks[:, 1, :].unsqueeze(1).to_broadcast([128, 4, 128])
                    mST = masks[:, 2, :].unsqueeze(1).to_broadcast([128, 4, 128])
                    S.op(V, lambda e: e.tensor_tensor(out=h3(Dm), in0=h3(E1), in1=colB(1), op=ALU.mult), reads=[bW[4], bgsm], writes=[bW[5]])
                    S.op(V, lambda e: e.tensor_tensor(out=h3(Dm), in0=h3(Dm), in1=mS, op=ALU.mult), reads=[bW[5], bC], writes=[bW[5]])
                    S.op(V, lambda e: e.tensor_tensor(out=h3(DqT), in0=h3(E2), in1=mIT, op=ALU.mult), reads=[bW[6], bC], writes=[bW[7]])
                    S.op(V, lambda e: e.tensor_tensor(out=DmT[:], in0=E2[:], in1=beRB[:], op=ALU.mult), reads=[bW[6], bW[1]], writes=[bW[8]])
                    S.op(V, lambda e: e.tensor_tensor(out=h3(DmT), in0=h3(DmT), in1=mST, op=ALU.mult), reads=[bW[8], bC], writes=[bW[8]])
                    pkk, pkkb = bank(); pqk, pqkb = bank()
                    for h in range(4):
                        kT = qkv[:, 4 + h, cs_].bitcast(F32R)
                        qT = qkv[:, h, cs_].bitcast(F32R)
                        S.op("pe", lambda e, h=h, kT=kT: e.matmul(out=pkk[:, h * 128:(h + 1) * 128], lhsT=kT, rhs=kT, start=True, stop=True), reads=[bqkv[4 + h]], writes=[pkkb])
                        S.op("pe", lambda e, h=h, kT=kT, qT=qT: e.matmul(out=pqk[:, h * 128:(h + 1) * 128], lhsT=kT, rhs=qT, start=True, stop=True), reads=[bqkv[4 + h], bqkv[h]], writes=[pqkb])
                    Nn = [W[13], W[14]]; NT = [W[15], W[16]]; RT = [W[17], W[18]]; qkT = W[19]
                    iN, iT_, iR = 13, 15, 17

                    def r32(t):
                        return t[:].bitcast(F32R)
                    S.op(V, lambda e: e.tensor_tensor(out=r32(Nn[0]), in0=pkk[:, :], in1=Dm[:], op=ALU.mult), reads=[pkkb, bW[5]], writes=[bW[13]])
                    S.op(V, lambda e: e.tensor_tensor(out=r32(NT[0]), in0=pkk[:, :], in1=DmT[:], op=ALU.mult), reads=[pkkb, bW[8]], writes=[bW[15]])
                    idB = ident[:, :].unsqueeze(1).to_broadcast([128, 4, 128])
                    S.op(V, lambda e: e.tensor_tensor(out=h3(RT[0]).bitcast(F32R), in0=idB, in1=h3(NT[0]), op=ALU.subtract), reads=[bC, bW[15]], writes=[bW[17]])
                    S.op(V, lambda e: e.tensor_tensor(out=r32(qkT), in0=pqk[:, :], in1=DqT[:], op=ALU.mult), reads=[pqkb, bW[7]], writes=[bW[19]])
                    cur = 0
                    for lev in range(1, 7):
                        nxt = 1 - cur
                        pn, pnb = bank()
                        for h in range(4):
                            hs = slice(h * 128, (h + 1) * 128)
                            S.op("pe", lambda e, hs=hs, pn=pn, cur=cur: e.matmul(out=pn[:, hs], lhsT=r32(NT[cur])[:, hs], rhs=r32(Nn[cur])[:, hs], start=True, stop=True), reads=[bW[iN + cur], bW[iT_ + cur]], writes=[pnb])
                        if lev < 6:
                            pt_, ptb = bank()
                            for h in range(4):
                                hs = slice(h * 128, (h + 1) * 128)
                                S.op("pe", lambda e, hs=hs, pt_=pt_, cur=cur: e.matmul(out=pt_[:, hs], lhsT=r32(Nn[cur])[:, hs], rhs=r32(NT[cur])[:, hs], start=True, stop=True), reads=[bW[iN + cur], bW[iT_ + cur]], writes=[ptb])
                        S.op("act", lambda e, pn=pn, nxt=nxt: e.copy(out=r32(Nn[nxt]), in_=pn[:, :]), reads=[pnb], writes=[bW[iN + nxt]])
                        if lev < 6:
                            S.op("act", lambda e, pt_=pt_, nxt=nxt: e.copy(out=r32(NT[nxt]), in_=pt_[:, :]), reads=[ptb], writes=[bW[iT_ + nxt]])
                        pr, prb = bank()
                        for h in range(4):
                            hs = slice(h * 128, (h + 1) * 128)
                            S.op("pe", lambda e, hs=hs, pr=pr, nxt=nxt, cur=cur: e.matmul(out=pr[:, hs], lhsT=r32(Nn[nxt])[:, hs], rhs=r32(RT[cur])[:, hs], start=True, stop=True), reads=[bW[iN + nxt], bW[iR + cur]], writes=[prb])
                        S.op(V, lambda e, pr=pr, nxt=nxt, cur=cur: e.tensor_tensor(out=r32(RT[nxt]), in0=pr[:, :], in1=RT[cur][:], op=ALU.add), reads=[prb, bW[iR + cur]], writes=[bW[iR + nxt]])
                        cur = nxt
                    TT_ = RT[cur]; bTT = bW[iR + cur]
                    pk, pkb = bank(); pv, pvb = bank()
                    for h in range(4):
                        hs = slice(h * 128, (h + 1) * 128)
                        S.op("pe", lambda e, h=h, hs=hs: e.transpose(out=pk[:, hs], in_=qkv[:, 4 + h, cs_], identity=ident[:]), reads=[bqkv[4 + h], bC], writes=[pkb])
                        S.op("pe", lambda e, h=h, hs=hs: e.transpose(out=pv[:, hs], in_=qkv[:, 8 + h, cs_], identity=ident[:]), reads=[bqkv[8 + h], bC], writes=[pvb])
                    kbg, kdec, vb, usb, wT, vnew = W[20], W[21], W[22], W[9], W[23], W[24]
                    iKBG, iKDEC, iVB, iUSB, iWT, iVN = 20, 21, 22, 9, 23, 24
                    S.op(V, lambda e: e.tensor_tensor(out=h3(kbg).bitcast(F32R), in0=pk[:, :].rearrange("p (a b) -> p a b", a=4), in1=colB(3), op=ALU.mult), reads=[pkb, bgsm], writes=[bW[20]])
                    S.op(V, lambda e: e.tensor_tensor(out=h3(kdec).bitcast(F32R), in0=pk[:, :].rearrange("p (a b) -> p a b", a=4), in1=colB(5), op=ALU.mult), reads=[pkb, bgsm], writes=[bW[21]])
                    S.op(V, lambda e: e.tensor_tensor(out=h3(vb).bitcast(F32R), in0=pv[:, :].rearrange("p (a b) -> p a b", a=4), in1=colB(1), op=ALU.mult), reads=[pvb, bgsm], writes=[bW[22]])
                    pu, pub = bank(); pw_, pwb = bank()
                    for h in range(4):
                        hs = slice(h * 128, (h + 1) * 128)
                        S.op("pe", lambda e, hs=hs: e.matmul(out=pu[:, hs], lhsT=r32(TT_)[:, hs], rhs=r32(vb)[:, hs], start=True, stop=True), reads=[bTT, bW[22]], writes=[pub])
                        S.op("pe", lambda e, hs=hs: e.matmul(out=pw_[:, hs], lhsT=r32(kbg)[:, hs], rhs=r32(TT_)[:, hs], start=True, stop=True), reads=[bTT, bW[20]], writes=[pwb])
                    S.op("act", lambda e: e.copy(out=usb[:], in_=pu[:, :]), reads=[pub], writes=[bW[9]])
                    S.op("act", lambda e: e.copy(out=r32(wT), in_=pw_[:, :]), reads=[pwb], writes=[bW[23]])
                    p1, p1b = bank()
                    for h in range(4):
                        hs = slice(h * 128, (h + 1) * 128)
                        S.op("pe", lambda e, h=h, hs=hs: e.matmul(out=p1[:, hs], lhsT=r32(wT)[:, hs], rhs=Sst[:, h, :], start=True, stop=True), reads=[bW[23], bS], writes=[p1b])
                    S.op(V, lambda e: e.tensor_tensor(out=r32(vnew), in0=usb[:], in1=p1[:, :], op=ALU.subtract), reads=[bW[9], p1b], writes=[bW[24]])
                    po, pob = bank(); p3_, p3b = bank()
                    for h in range(4):
                        hs = slice(h * 128, (h + 1) * 128)
                        S.op("pe", lambda e, h=h, hs=hs: e.matmul(out=po[:, hs], lhsT=Sst[:, h, :], rhs=qe[:, h, cs_], start=True, stop=False), reads=[bS, bqe], writes=[pob])
                        S.op("pe", lambda e, h=h, hs=hs: e.matmul(out=po[:, hs], lhsT=r32(vnew)[:, hs], rhs=r32(qkT)[:, hs], start=False, stop=True), reads=[bW[24], bW[19]], writes=[pob])
                        S.op("pe", lambda e, h=h, hs=hs: e.matmul(out=p3_[:, hs], lhsT=r32(kdec)[:, hs], rhs=r32(vnew)[:, hs], start=True, stop=True), reads=[bW[21], bW[24]], writes=[p3b])
                    S3 = Sst[:].bitcast(F32)
                    S.op(V, lambda e: e.tensor_tensor(out=Sst[:], in0=S3, in1=colB(6), op=ALU.mult), reads=[bS, bgsm], writes=[bS])
                    S.op(V, lambda e: e.tensor_tensor(out=Sst[:], in0=S3, in1=p3_[:, :].rearrange("p (a b) -> p a b", a=4), op=ALU.add), reads=[bS, p3b], writes=[bS])
                    osb, osq, ors = W[10], W[11], W[12]
                    S.op("act", lambda e: e.copy(out=osb[:], in_=po[:, :]), reads=[pob], writes=[bW[10]])
                    S.op("act", lambda e: e.activation(out=osq[:], in_=po[:, :], func=AF.Square), reads=[pob], writes=[bW[11]])
                    ps_, psb = bank()
                    S.op("pe", lambda e: e.matmul(out=ps_[:, :], lhsT=ones[:], rhs=osq[:], start=True, stop=True), reads=[bC, bW[11]], writes=[psb])
                    S.op("act", lambda e: e.activation(out=ors[:], in_=ps_[:, :], func=AF.Sqrt, scale=1.0 / 128.0, bias=epsnm[:, 0:1]), reads=[psb, bC], writes=[bW[12]])
                    S.op(V, lambda e: e.reciprocal(out=ors[:], in_=ors[:]), reads=[bW[12]], writes=[bW[12]])
                    S.op(V, lambda e: e.scalar_tensor_tensor(out=osb[:], in0=osb[:], scalar=nw[:, 0:1], in1=ors[:], op0=ALU.mult, op1=ALU.mult), reads=[bW[10], bW[12], bgp], writes=[bW[10]])
                    S.op(V, lambda e: e.tensor_tensor(out=ydn[:, :, cs_], in0=h3(osb), in1=zs[:, :, cs_], op=ALU.mult), reads=[bW[10], bzs], writes=[bydn])
                S.dma("sp", "st", yloc[blk * 768 + 256:blk * 768 + 768, :].rearrange("(a p) t -> p a t", p=128), ydn[:], reads=[bydn])
                if stage != 1:
                    nc.gpsimd.wait_ge(S.sem["st"], S.cnt["st"])
                    nc.gpsimd.collective_compute("AllGather", ALU.bypass, replica_groups=[[0, 1, 2, 3], [4, 5, 6, 7]], ins=[yloc[blk * 768:(blk + 1) * 768, :].opt()], outs=[yall[blk * 3072:(blk + 1) * 3072, :].opt()]).then_inc(cc_sem)
                    cc_cnt[0] += 1
                    nc.gpsimd.wait_ge(cc_sem, cc_cnt[0])

        S.barrier()
        if stage == 1:
            S.eng["pool"].wait_ge(S.sem["st"], S.cnt["st"])
            i1 = nc.gpsimd.dma_start(out=out_d[:, :].rearrange("(f p) (b t) -> b f p t", p=128, t=512), in_=yloc[:, :].rearrange("(b f p) t -> b f p t", f=6, p=128))
            S.stream("fin"); S.cnt["fin"] += 16; i1.then_inc(S.sem["fin"], 16)
            nc.gpsimd.wait_ge(S.sem["fin"], S.cnt["fin"])
            return nc

        def all_wait_cc():
            for e in ("pool", "sp", "act", "dve", "pe"):
                S.eng[e].wait_ge(cc_sem, cc_cnt[0])
        all_wait_cc()
        if stage == 15:
            i1 = nc.gpsimd.dma_start(out=out_d[:, :].rearrange("(f p) (b t) -> b f p t", p=128, t=512), in_=yall[:, :].rearrange("(b r f p) t -> b r f p t", r=4, f=6, p=128)[:, 1])
            S.stream("fin"); S.cnt["fin"] += 16; i1.then_inc(S.sem["fin"], 16)
            nc.gpsimd.wait_ge(S.sem["fin"], S.cnt["fin"])
            return nc
        core_i = None
        V = "dve"

        with ExitStack() as pb_:
            for i_ in range(2, 4):
                wbuf.append(sbt(pb_, "b_wbuf%d" % i_, [128, 16, 256], BF16)); wbb.append(Buf("wbufB%d" % i_))
            lnin_t = sbt(pb_, "b_lnin", [128, 2, 16]); blnin = Buf()
            S.dma("sp", "cst", lnin_t[:], lnin[:, :, :], writes=[blnin])
            ln1_t = sbt(pb_, "b_ln1", [128, 2, 16]); wr_t = sbt(pb_, "b_wr", [128, 16, 32]); br_t = sbt(pb_, "b_br", [32, 1]); bglu_t = sbt(pb_, "b_bglu", [128, 8])
            bpar = Buf()
            S.dma("sp", "cst", ln1_t[:], ln1[:, :, :], writes=[bpar])
            S.dma("sp", "cst", wr_t[:], wr[:, :, :], writes=[bpar])
            S.dma("sp", "cst", br_t[:], br[:, :], writes=[bpar])
            S.dma("sp", "cst", bglu_t[:], bglu[:, :], writes=[bpar])
            yidx = sbt(pb_, "b_yidx", [128, NBB * 24], I32); byidx = Buf()
            S.dma("sp", "cst", yidx[:], yidx_d[:, :], writes=[byidx])
            xt = sbt(pb_, "b_xt", [128, D]); bxt = Buf()
            xn = sbt(pb_, "b_xn", [128, D]); bxn = Buf()
            st6 = sbt(pb_, "b_st6", [128, 4, 6]); mv = sbt(pb_, "b_mv", [128, 2]); rstd = sbt(pb_, "b_rstd", [128, 1]); bst = Buf()
            hT = sbt(pb_, "b_hT", [128, 16, 512], BF16); bhT = Buf()
            hTf = sbt(pb_, "b_hTf", [128, 16, 512]); bhTf = Buf()
            ysT = sbt(pb_, "b_ysT", [128, 8, 512], BF16); bysT = Buf()
            ydT = sbt(pb_, "b_ydT", [128, 16, 512], BF16); bydT = Buf()
            ygT = sbt(pb_, "b_ygT", [128, 8, 512], BF16); bygT = Buf()
            sgs = sbt(pb_, "b_sgs", [128, 16, 512], BF16); bsgs = Buf()
            sgd = sbt(pb_, "b_sgd", [128, 16, 512], BF16); bsgd = Buf()
            h1b = sbt(pb_, "b_h1b", [128, D], BF16); bh1b = Buf()
            Wb = [sbt(pb_, "b_wk%d" % i, [128, 512]) for i in range(5)]; bWb = [Buf() for _ in range(5)]
            lgT = sbt(pb_, "b_lgT", [32, 512]); blgT = Buf()
            lg = sbt(pb_, "b_lg", [128, 4, 32]); blg = Buf()
            m8 = sbt(pb_, "b_m8", [128, 8]); gt = sbt(pb_, "b_gt", [128, 4, 32]); sm = sbt(pb_, "b_sm", [128, 4]); bsm = Buf(); bgt = Buf()
            for blk in range(NBB):
                c0 = blk * 512
                for tti in range(4):
                    layer_norm_T(pb_, xq[c0 + tti * 128:c0 + (tti + 1) * 128, :], lnin_t, blnin, [hT, hTf], [bhT, bhTf], tti * 128, xt, bxt, xn, bxn, st6, mv, rstd, bst)
                for r in range(4):
                    for ft in range(6):
                        col = blk * 24 + r * 6 + ft
                        if ft < 2:
                            S.idma("yl", ysT[:, 2 * r + ft, :], yall[:, :], yidx[:, col:col + 1], reads=[byidx], writes=[bysT])
                        else:
                            S.idma("yl", ydT[:, 4 * r + ft - 2, :], yall[:, :], yidx[:, col:col + 1], reads=[byidx], writes=[bydT])

                def ep_glu(mi, pb, pbb):
                    S.op("act", lambda e: e.activation(out=Wb[0][:], in_=pb[:, :], func=AF.Sigmoid, bias=bglu_t[:, mi:mi + 1]), reads=[pbb, bpar], writes=[bWb[0]])
                    S.op(V, lambda e: e.tensor_tensor(out=ygT[:, mi, :], in0=ysT[:, mi, :], in1=Wb[0][:], op=ALU.mult), reads=[bysT, bWb[0]], writes=[bygT])
                linear(wglu, 8, 1024, lambda k: ysT[:, k, :], [bysT], 512, ep_glu)

                def ep_gs(mi, pb, pbb):
                    S.op("act", lambda e: e.activation(out=sgs[:, mi, :], in_=pb[:, :], func=AF.Sigmoid), reads=[pbb], writes=[bsgs])

                def ep_gd(mi, pb, pbb):
                    S.op("act", lambda e: e.activation(out=sgd[:, mi, :], in_=pb[:, :], func=AF.Sigmoid), reads=[pbb], writes=[bsgd])
                linear(wg[:, 0:2048], 16, 2048, lambda k: hT[:, k, :], [bhT], 512, ep_gs)
                linear(wg[:, 2048:4096], 16, 2048, lambda k: hT[:, k, :], [bhT], 512, ep_gd)

                def ep_ps(mi, pb, pbb):
                    S.op(V, lambda e: e.tensor_tensor(out=sgs[:, mi, :], in0=sgs[:, mi, :], in1=pb[:, :], op=ALU.mult), reads=[pbb, bsgs], writes=[bsgs])

                def ep_pd(mi, pb, pbb):
                    S.op(V, lambda e: e.tensor_tensor(out=Wb[1][:], in0=sgd[:, mi, :], in1=pb[:, :], op=ALU.mult), reads=[pbb, bsgd], writes=[bWb[1]])
                    S.op(V, lambda e: e.tensor_tensor(out=sgs[:, mi, :], in0=sgs[:, mi, :], in1=Wb[1][:], op=ALU.add), reads=[bsgs, bWb[1]], writes=[bsgs])
                linear(wps, 8, 2048, lambda k: ygT[:, k, :], [bygT], 512, ep_ps)
                linear(wpd, 16, 2048, lambda k: ydT[:, k, :], [bydT], 512, ep_pd)

                def ep_out(mi, pb, pbb):
                    S.op(V, lambda e: e.scalar_tensor_tensor(out=hTf[:, mi, :], in0=hTf[:, mi, :], scalar=ALPHA, in1=pb[:, :], op0=ALU.mult, op1=ALU.add), reads=[pbb, bhTf], writes=[bhTf])
                linear(wout, 16, 2048, lambda k: sgs[:, k, :], [bsgs], 512, ep_out)

                pm, pmb = bank(); pq, pqb = bank()
                for k in range(16):
                    S.op("pe", lambda e, k=k: e.matmul(out=pm[:, :], lhsT=ones[:], rhs=hTf[:, k, :], start=(k == 0), stop=(k == 15)), reads=[bC, bhTf], writes=[pmb])
                for k in range(16):
                    sq = Wb[k % 2]
                    S.op("act", lambda e, k=k, sq=sq: e.activation(out=sq[:], in_=hTf[:, k, :], func=AF.Square), reads=[bhTf], writes=[bWb[k % 2]])
                    S.op("pe", lambda e, k=k, sq=sq: e.matmul(out=pq[:, :], lhsT=ones[:], rhs=sq[:], start=(k == 0), stop=(k == 15)), reads=[bC, bWb[k % 2]], writes=[pqb])
                mu, rs1, tq = Wb[2], Wb[3], Wb[4]
                S.op("act", lambda e: e.activation(out=mu[:], in_=pm[:, :], func=AF.Identity, scale=1.0 / D), reads=[pmb], writes=[bWb[2]])
                S.op(V, lambda e: e.tensor_tensor(out=tq[:], in0=mu[:], in1=mu[:], op=ALU.mult), reads=[bWb[2]], writes=[bWb[4]])
                S.op(V, lambda e: e.scalar_tensor_tensor(out=rs1[:], in0=pq[:, :], scalar=1.0 / D, in1=tq[:], op0=ALU.mult, op1=ALU.subtract), reads=[pqb, bWb[4]], writes=[bWb[3]])
                S.op("act", lambda e: e.activation(out=rs1[:], in_=rs1[:], func=AF.Sqrt, bias=epsln[:, 0:1]), reads=[bWb[3], bC], writes=[bWb[3]])
                S.op(V, lambda e: e.reciprocal(out=rs1[:], in_=rs1[:]), reads=[bWb[3]], writes=[bWb[3]])
                for k in range(16):
                    S.op(V, lambda e, k=k: e.tensor_tensor(out=hTf[:, k, :], in0=hTf[:, k, :], in1=mu[:], op=ALU.subtract), reads=[bhTf, bWb[2]], writes=[bhTf])
                    S.op(V, lambda e, k=k: e.tensor_tensor(out=hTf[:, k, :], in0=hTf[:, k, :], in1=rs1[:], op=ALU.mult), reads=[bhTf, bWb[3]], writes=[bhTf])
                    S.op("act", lambda e, k=k: e.activation(out=hTf[:, k, :], in_=hTf[:, k, :], func=AF.Identity, scale=ln1_t[:, 0, k:k + 1], bias=ln1_t[:, 1, k:k + 1]), reads=[bhTf, bpar], writes=[bhTf])

                pl, plb = bank()
                for k in range(16):
                    S.op("pe", lambda e, k=k: e.matmul(out=pl[0:32, :], lhsT=wr_t[:, k, :], rhs=hTf[:, k, :], start=(k == 0), stop=(k == 15)), reads=[bpar, bhTf], writes=[plb])
                S.op("act", lambda e: e.activation(out=lgT[:], in_=pl[0:32, :], func=AF.Identity, bias=br_t[:, 0:1]), reads=[plb, bpar], writes=[blgT])
                pt_, ptb = bank()
                for tti in range(4):
                    S.op("pe", lambda e, tti=tti: e.transpose(out=pt_[:, tti * 32:(tti + 1) * 32], in_=lgT[:, tti * 128:(tti + 1) * 128], identity=ident[0:32, 0:32]), reads=[blgT, bC], writes=[ptb])
                S.op("act", lambda e: e.copy(out=lg[:], in_=pt_[:, 0:128].rearrange("p (a b) -> p a b", a=4)), reads=[ptb], writes=[blg])
                for tti in range(4):
                    S.op(V, lambda e, tti=tti: e.max(out=m8[:], in_=lg[:, tti, :]), reads=[blg], writes=[bsm])
                    S.op(V, lambda e, tti=tti: e.tensor_scalar(out=gt[:, tti, :], in0=lg[:, tti, :], scalar1=m8[:, 3:4], scalar2=None, op0=ALU.is_ge), reads=[blg, bsm], writes=[bgt])
                    S.op(V, lambda e: e.tensor_scalar(out=sm[:, 0:1], in0=m8[:, 0:1], scalar1=-1.0, scalar2=None, op0=ALU.mult), reads=[bsm], writes=[bsm])
                    S.op("act", lambda e, tti=tti: e.activation(out=lg[:, tti, :], in_=lg[:, tti, :], func=AF.Exp, bias=sm[:, 0:1]), reads=[blg, bsm], writes=[blg])
                    S.op(V, lambda e, tti=tti: e.tensor_tensor(out=gt[:, tti, :], in0=gt[:, tti, :], in1=lg[:, tti, :], op=ALU.mult), reads=[blg, bgt], writes=[bgt])
                    S.op(V, lambda e, tti=tti: e.reduce_sum(out=sm[:, 1:2], in_=gt[:, tti, :], axis=AX.X), reads=[bgt], writes=[bsm])
                    S.op(V, lambda e: e.reciprocal(out=sm[:, 1:2], in_=sm[:, 1:2]), reads=[bsm], writes=[bsm])
                    S.op(V, lambda e, tti=tti: e.tensor_scalar(out=gt[:, tti, :], in0=gt[:, tti, :], scalar1=sm[:, 1:2], scalar2=None, op0=ALU.mult), reads=[bgt, bsm], writes=[bgt])
                S.dma("sp", "st", gate_loc[c0:c0 + 512, :].rearrange("(a p) e -> p a e", p=128), gt[:], reads=[bgt])

                for tti in range(4):
                    for kq in range(4):
                        pb, pbb = bank()
                        for r in range(4):
                            k = kq * 4 + r
                            S.op("pe", lambda e, k=k, r=r, pb=pb, tti=tti: e.transpose(out=pb[:, r * 128:(r + 1) * 128], in_=hTf[:, k, tti * 128:(tti + 1) * 128], identity=ident[:]), reads=[bhTf, bC], writes=[pbb])
                        S.op("act", lambda e, pb=pb, kq=kq: e.copy(out=xn[:, kq * 512:(kq + 1) * 512], in_=pb[:, :]), reads=[pbb], writes=[bxn])
                        S.op(V, lambda e, pb=pb, kq=kq: e.tensor_copy(out=h1b[:, kq * 512:(kq + 1) * 512], in_=pb[:, :]), reads=[pbb], writes=[bh1b])
                    S.dma("sp", "st", h1tok[c0 + tti * 128:c0 + (tti + 1) * 128, :], xn[:], reads=[bxn])
                    S.dma("sp", "st", h1b_loc[c0 + tti * 128:c0 + (tti + 1) * 128, :], h1b[:], reads=[bh1b])
        del wbuf[2:], wbb[2:]
        wrr[0] = 0
        S.barrier()
        if stage == 2:
            nc.gpsimd.wait_ge(S.sem["st"], S.cnt["st"])
            i1 = nc.gpsimd.dma_start(out=out_d[:, :], in_=h1tok[:, :])
            S.stream("fin"); S.cnt["fin"] += 16; i1.then_inc(S.sem["fin"], 16)
            nc.gpsimd.wait_ge(S.sem["fin"], S.cnt["fin"])
            return nc

        nc.gpsimd.wait_ge(S.sem["st"], S.cnt["st"])
        G4 = [[0, 1, 2, 3], [4, 5, 6, 7]]
        for k in range(HC):
            nc.gpsimd.collective_compute("AllGather", ALU.bypass, replica_groups=G4, ins=[h1b_loc[256 * k:256 * (k + 1), :].opt()], outs=[h1b_all[1024 * k:1024 * (k + 1), :].opt()]).then_inc(cc_sem)
            cc_cnt[0] += 1
            nc.gpsimd.wait_ge(cc_sem, cc_cnt[0])
        nc.gpsimd.collective_compute("AllGather", ALU.bypass, replica_groups=G4, ins=[gate_loc.ap().opt()], outs=[gate_all.ap().opt()]).then_inc(cc_sem)
        cc_cnt[0] += 1
        all_wait_cc()

        NTt = TPC // 128
        SL = 4 * CAP
        with ExitStack() as pr_:
            posm = sbt(pr_, "r_posm", [128, 4, NTt, NEX]); Gr = sbt(pr_, "r_G", [128, 4, NTt, NEX]); brt = Buf()
            iotaC = sbt(pr_, "r_iotaC", [128, CAP]); iotap = sbt(pr_, "r_iotap", [128, 3]); sel8 = sbt(pr_, "r_sel8", [8, 8, 128]); brc = Buf()
            S.dma("sp", "cst", iotaC[:], iota_d[:, 0:CAP], writes=[brc])
            S.dma("sp", "cst", iotap[:], iotap_d[:, :], writes=[brc])
            S.dma("sp", "cst", sel8[:], sel8_d[:, :, :], writes=[brc])
            with ExitStack() as p1_:
                Gf = sbt(p1_, "r_Gf", [128, NTt, 32]); bGf = Buf()
                Gp = sbt(p1_, "r_Gp", [128, NTt, NEX, 32]); bGp = Buf()
                esel = sbt(p1_, "r_esel", [128, NEX, 32]); besel = Buf()
                mk = sbt(p1_, "r_mk", [128, NTt, NEX]); cum = sbt(p1_, "r_cum", [128, NTt, NEX]); bmk = Buf()
                S.dma("sp", "cst", esel[:], esel_d[:, :, :], writes=[besel])
                for r in range(4):
                    S.dma("sp", "xl", Gf[:], gate_all[r * TPC:(r + 1) * TPC, :].rearrange("(a p) e -> p a e", p=128), writes=[bGf])
                    S.op(V, lambda e: e.tensor_tensor(out=Gp[:], in0=Gf[:].unsqueeze(2).to_broadcast([128, NTt, NEX, 32]), in1=esel[:].unsqueeze(1).to_broadcast([128, NTt, NEX, 32]), op=ALU.mult), reads=[bGf, besel], writes=[bGp])
                    S.op(V, lambda e, r=r: e.reduce_sum(out=Gr[:, r].rearrange("p a b -> p (a b)"), in_=Gp[:].rearrange("p a b c -> p (a b) c"), axis=AX.X), reads=[bGp], writes=[brt])
                    S.op(V, lambda e, r=r: e.tensor_scalar(out=mk[:], in0=Gr[:, r], scalar1=0.0, scalar2=None, op0=ALU.is_gt), reads=[brt], writes=[bmk])
                    S.op(V, lambda e: e.memset(cum[:, 0, :], 0.0), reads=[bmk], writes=[bmk])
                    for i in range(1, NTt):
                        S.op(V, lambda e, i=i: e.tensor_tensor(out=cum[:, i, :], in0=cum[:, i - 1, :], in1=mk[:, i - 1, :], op=ALU.add), reads=[bmk], writes=[bmk])
                    for i0 in range(0, NTt, 32):
                        pb, pbb = bank()
                        for i in range(i0, min(NTt, i0 + 32)):
                            oc = (i - i0) * NEX
                            S.op("pe", lambda e, i=i, oc=oc, pb=pb: e.matmul(out=pb[:, oc:oc + NEX], lhsT=masks[:, 2, :], rhs=mk[:, i, :], start=True, stop=False), reads=[bC, bmk], writes=[pbb])
                            S.op("pe", lambda e, i=i, oc=oc, pb=pb: e.matmul(out=pb[:, oc:oc + NEX], lhsT=ones[:], rhs=cum[:, i, :], start=False, stop=True), reads=[bC, bmk], writes=[pbb])
                        n_ = min(NTt, i0 + 32) - i0
                        S.op(V, lambda e, i0=i0, n_=n_, pb=pb, r=r: e.scalar_tensor_tensor(out=posm[:, r, i0:i0 + n_, :], in0=pb[:, 0:n_ * NEX].rearrange("p (a b) -> p a b", b=NEX), scalar=1.0, in1=mk[:, i0:i0 + n_, :], op0=ALU.add, op1=ALU.mult), reads=[pbb, bmk], writes=[brt])
                        S.op(V, lambda e, i0=i0, n_=n_, r=r: e.tensor_scalar(out=posm[:, r, i0:i0 + n_, :], in0=posm[:, r, i0:i0 + n_, :], scalar1=-1.0, scalar2=None, op0=ALU.add), reads=[brt], writes=[brt])
            S.barrier()

            with ExitStack() as p2_:
                H = sbt(p2_, "g_H", [128, NTt, D], BF16); bH = Buf()
                Sels = [sbt(p2_, "g_Sel%d" % i_, [128, NTt, CAP], BF16) for i_ in range(2)]; bSels = [Buf(), Buf()]
                XTss = [sbt(p2_, "g_XTs%d" % i_, [128, 16, CAP], BF16) for i_ in range(2)]; bXTss = [Buf(), Buf()]
                g_rr = 0
                for r in range(4):
                    for k in range(HC):
                        S.dma("sp", "xl", H[:, 2 * k:2 * k + 2, :], h1b_all[(k * 4 + r) * 256:(k * 4 + r + 1) * 256, :].rearrange("(a p) d -> p a d", p=128), writes=[bH])
                    for ex in range(NEX):
                        Sel, bSel, XTs, bXTs = Sels[g_rr % 2], bSels[g_rr % 2], XTss[g_rr % 2], bXTss[g_rr % 2]
                        g_rr += 1
                        for i in range(NTt):
                            S.op(V, lambda e, i=i: e.tensor_scalar(out=Sel[:, i, :], in0=iotaC[:], scalar1=posm[:, r, i, ex:ex + 1], scalar2=None, op0=ALU.is_equal), reads=[brc, brt], writes=[bSel])
                        for kc in range(16):
                            pb, pbb = bank()
                            for i in range(NTt):
                                S.op("pe", lambda e, i=i, kc=kc, pb=pb: e.matmul(out=pb[:, 0:CAP], lhsT=H[:, i, kc * 128:(kc + 1) * 128], rhs=Sel[:, i, :], start=(i == 0), stop=(i == NTt - 1)), reads=[bH, bSel], writes=[pbb])
                            S.op("act", lambda e, kc=kc, pb=pb: e.copy(out=XTs[:, kc, :], in_=pb[:, 0:CAP]), reads=[pbb], writes=[bXTs])
                        S.dma("sp", "st", XTd[ex * D:(ex + 1) * D, r * CAP:(r + 1) * CAP].rearrange("(k p) c -> p k c", p=128), XTs[:], reads=[bXTs])
            S.barrier()
            nc.sync.wait_ge(S.sem["st"], S.cnt["st"])

            with ExitStack() as pm_:
                for i_ in range(2, 4):
                    wbuf.append(sbt(pm_, "m_wbuf%d" % i_, [128, 16, 256], BF16)); wbb.append(Buf("wbufM%d" % i_))
                XT = sbt(pm_, "m_XT", [128, 16, SL], BF16); bXT = Buf()
                AT = sbt(pm_, "m_AT", [128, 16, SL], BF16); bAT = Buf()
                w2 = [sbt(pm_, "m_w2%d" % i, [128, 16, 512], BF16) for i in range(2)]; bw2 = [Buf(), Buf()]
                w2rr = [0]
                Wm = [sbt(pm_, "m_wk%d" % i, [128, 512]) for i in range(3)]; bWm = [Buf() for _ in range(3)]
                Yst = [sbt(pm_, "m_Yst%d" % i, [128, 512], BF16) for i in range(2)]; bYst = [Buf(), Buf()]
                bdB = sbt(pm_, "m_bdB", [128, D]); bbdB = Buf()
                bgu_t = sbt(pm_, "m_bgu", [128, NEX, 32]); bbgu = Buf()
                S.dma("sp", "cst", bgu_t[:], bgu[:, :, :], writes=[bbgu])
                yrr = 0
                for ex in range(NEX):
                    S.dma("sp", "xl", XT[:], XTd[ex * D:(ex + 1) * D, :].rearrange("(k p) c -> p k c", p=128), writes=[bXT])
                    S.dma("sp", "bl", bdB[:], bdn[ex:ex + 1, :].to_broadcast([128, D]), writes=[bbdB])
                    for ft in range(16):
                        i = wrr[0] % len(wbuf); wrr[0] = (i + 1) % len(wbuf)
                        S.dma("pool", "wl%d" % i, wbuf[i][:, :, 0:128], wgu[ex, :, ft * 128:(ft + 1) * 128].rearrange("(kc p) m -> p kc m", p=128), writes=[wbb[i]])
                        S.dma("pool", "wl%d" % i, wbuf[i][:, :, 128:256], wgu[ex, :, 2048 + ft * 128:2048 + (ft + 1) * 128].rearrange("(kc p) m -> p kc m", p=128), writes=[wbb[i]])
                        for nh in range(SL // 512):
                            ns = slice(nh * 512, (nh + 1) * 512)
                            pg, pgb = bank(); pu, pub = bank()
                            for k in range(16):
                                S.op("pe", lambda e, k=k, i=i, pg=pg: e.matmul(out=pg[:, :], lhsT=wbuf[i][:, k, 0:128], rhs=XT[:, k, ns], start=(k == 0), stop=(k == 15)), reads=[wbb[i], bXT], writes=[pgb])
                            for k in range(16):
                                S.op("pe", lambda e, k=k, i=i, pu=pu: e.matmul(out=pu[:, :], lhsT=wbuf[i][:, k, 128:256], rhs=XT[:, k, ns], start=(k == 0), stop=(k == 15)), reads=[wbb[i], bXT], writes=[pub])
                            g_, s_, u_ = Wm
                            S.op(V, lambda e, pg=pg: e.tensor_scalar(out=g_[:], in0=pg[:, :], scalar1=bgu_t[:, ex, ft:ft + 1], scalar2=7.0, op0=ALU.add, op1=ALU.min), reads=[pgb, bbgu], writes=[bWm[0]])
                            S.op("act", lambda e: e.activation(out=s_[:], in_=g_[:], func=AF.Sigmoid, scale=1.702), reads=[bWm[0]], writes=[bWm[1]])
                            S.op(V, lambda e, pu=pu: e.tensor_scalar(out=u_[:], in0=pu[:, :], scalar1=bgu_t[:, ex, 16 + ft:17 + ft], scalar2=7.0, op0=ALU.add, op1=ALU.min), reads=[pub, bbgu], writes=[bWm[2]])
                            S.op(V, lambda e: e.tensor_scalar(out=u_[:], in0=u_[:], scalar1=-7.0, scalar2=1.0, op0=ALU.max, op1=ALU.add), reads=[bWm[2]], writes=[bWm[2]])
                            S.op(V, lambda e: e.tensor_tensor(out=g_[:], in0=g_[:], in1=s_[:], op=ALU.mult), reads=[bWm[0], bWm[1]], writes=[bWm[0]])
                            S.op(V, lambda e: e.tensor_tensor(out=AT[:, ft, ns], in0=g_[:], in1=u_[:], op=ALU.mult), reads=[bWm[0], bWm[2]], writes=[bAT])
                    for dq in range(4):
                        j = w2rr[0]; w2rr[0] = 1 - j
                        S.dma("pool", "w2l%d" % j, w2[j][:], wdn[ex, :, dq * 512:(dq + 1) * 512].rearrange("(kc p) m -> p kc m", p=128), writes=[bw2[j]])
                        ds_ = slice(dq * 512, (dq + 1) * 512)
                        for sti in range(SL // 128):
                            pd, pdb = bank()
                            for k in range(16):
                                S.op("pe", lambda e, k=k, j=j, pd=pd, sti=sti: e.matmul(out=pd[:, :], lhsT=AT[:, k, sti * 128:(sti + 1) * 128], rhs=w2[j][:, k, :], start=(k == 0), stop=(k == 15)), reads=[bw2[j], bAT], writes=[pdb])
                            yb = yrr % 2; yrr += 1
                            S.op(V, lambda e, pd=pd, yb=yb: e.tensor_tensor(out=Yst[yb][:], in0=pd[:, :], in1=bdB[:, ds_], op=ALU.add), reads=[pdb, bbdB], writes=[bYst[yb]])
                            S.dma("sp", "st", Yd[ex * SL + sti * 128:ex * SL + (sti + 1) * 128, ds_], Yst[yb][:], reads=[bYst[yb]])
            del wbuf[2:], wbb[2:]
            wrr[0] = 0
            S.barrier()
            nc.sync.wait_ge(S.sem["st"], S.cnt["st"])

            with ExitStack() as p4_:
                SG = sbt(p4_, "s_SG", [128, NEX, 3, TPC], BF16); bSG = Buf()
                Yts = [sbt(p4_, "s_Yt%d" % i_, [128, NEX, 3, 512], BF16) for i_ in range(2)]; bYts = [Buf(), Buf()]
                y_rr = 0
                posT = sbt(p4_, "s_posT", [8, TPC]); gT = sbt(p4_, "s_gT", [8, TPC]); bpT = Buf()
                gsb = sbt(p4_, "s_gsb", [128, 512]); bgsb = Buf()
                ost = [sbt(p4_, "s_ost%d" % i, [128, 512]) for i in range(2)]; bost = [Buf(), Buf()]
                orr = 0
                for r in range(4):
                    for i0 in range(0, NTt, 4):
                        pp_, ppb = bank(); pg_, pgb_ = bank()
                        for i in range(i0, min(NTt, i0 + 4)):
                            oc = (i - i0) * 128
                            S.op("pe", lambda e, i=i, oc=oc, pp_=pp_: e.transpose(out=pp_[0:8, oc:oc + 128], in_=posm[:, r, i, :], identity=ident[:]), reads=[brt, bC], writes=[ppb])
                            S.op("pe", lambda e, i=i, oc=oc, pg_=pg_: e.transpose(out=pg_[0:8, oc:oc + 128], in_=Gr[:, r, i, :], identity=ident[:]), reads=[brt, bC], writes=[pgb_])
                        n_ = (min(NTt, i0 + 4) - i0) * 128
                        S.op("act", lambda e, i0=i0, n_=n_, pp_=pp_: e.copy(out=posT[:, i0 * 128:i0 * 128 + n_], in_=pp_[0:8, 0:n_]), reads=[ppb], writes=[bpT])
                        S.op("act", lambda e, i0=i0, n_=n_, pg_=pg_: e.copy(out=gT[:, i0 * 128:i0 * 128 + n_], in_=pg_[0:8, 0:n_]), reads=[pgb_], writes=[bpT])
                    for ex in range(NEX):
                        for cb in range(TPC // 512):
                            cs_ = slice(cb * 512, (cb + 1) * 512)
                            pp_, ppb = bank(); pg_, pgb_ = bank()
                            S.op("pe", lambda e, pp_=pp_: e.matmul(out=pp_[:, :], lhsT=sel8[:, ex, :], rhs=posT[:, cs_], start=True, stop=True), reads=[brc, bpT], writes=[ppb])
                            S.op("pe", lambda e, pg_=pg_: e.matmul(out=pg_[:, :], lhsT=sel8[:, ex, :], rhs=gT[:, cs_], start=True, stop=True), reads=[brc, bpT], writes=[pgb_])
                            S.op("act", lambda e, pg_=pg_: e.copy(out=gsb[:], in_=pg_[:, :]), reads=[pgb_], writes=[bgsb])
                            for cc in range(3):
                                S.op(V, lambda e, cc=cc, pp_=pp_: e.scalar_tensor_tensor(out=SG[:, ex, cc, cs_], in0=pp_[:, :], scalar=iotap[:, cc:cc + 1], in1=gsb[:], op0=ALU.is_equal, op1=ALU.mult), reads=[ppb, brc, bgsb], writes=[bSG])
                    for dq in range(4):
                        ds_ = slice(dq * 512, (dq + 1) * 512)
                        Yt, bYt = Yts[y_rr % 2], bYts[y_rr % 2]
                        y_rr += 1
                        for ex in range(NEX):
                            S.dma("sp", "yt%d" % (y_rr % 2), Yt[:, ex, :, :], Yd[ex * SL + r * CAP:ex * SL + (r + 1) * CAP, ds_].rearrange("(a p) d -> p a d", p=128), writes=[bYt])
                        for i in range(NTt):
                            po_, pob_ = bank()
                            n = 0
                            for ex in range(NEX):
                                for cc in range(3):
                                    S.op("pe", lambda e, ex=ex, cc=cc, i=i, n=n, po_=po_: e.matmul(out=po_[:, :], lhsT=SG[:, ex, cc, i * 128:(i + 1) * 128], rhs=Yt[:, ex, cc, :], start=(n == 0), stop=(n == NEX * 3 - 1)), reads=[bSG, bYt], writes=[pob_])
                                    n += 1
                            ob = orr % 2; orr += 1
                            S.op("act", lambda e, po_=po_, ob=ob: e.copy(out=ost[ob][:], in_=po_[:, :]), reads=[pob_], writes=[bost[ob]])
                            l = i * 128
                            row = ((l // RPC) * 4 + r) * RPC + (l % RPC)
                            S.dma("sp", "st", dense[row:row + 128, ds_], ost[ob][:], reads=[bost[ob]])
        S.barrier()
        nc.gpsimd.wait_ge(S.sem["st"], S.cnt["st"])
        for k in range(NRS):
            nc.gpsimd.collective_compute("ReduceScatter", ALU.add, replica_groups=[[0, 1, 2, 3], [4, 5, 6, 7]], ins=[dense[k * 4 * RPC:(k + 1) * 4 * RPC, :].opt()], outs=[ffn[k * RPC:(k + 1) * RPC, :].opt()]).then_inc(cc_sem)
            cc_cnt[0] += 1
            nc.gpsimd.wait_ge(cc_sem, cc_cnt[0])
        all_wait_cc()

        with ExitStack() as pf:
            g2 = sbt(pf, "f_g2", [128, D]); b2 = sbt(pf, "f_b2", [128, D]); bg2 = Buf()
            S.dma("sp", "cst", g2[:], ln2[0:1, :].to_broadcast([128, D]), writes=[bg2])
            S.dma("sp", "cst", b2[:], ln2[1:2, :].to_broadcast([128, D]), writes=[bg2])
            ha = sbt(pf, "f_ha", [128, D]); fa = sbt(pf, "f_fa", [128, D]); bha, bfa = Buf(), Buf()
            st6 = sbt(pf, "f_st6", [128, 4, 6]); mv = sbt(pf, "f_mv", [128, 2]); rstd = sbt(pf, "f_rstd", [128, 1]); bst = Buf()
            fin_toks = []
            for tti in range(TPC // 128):
                rows = slice(tti * 128, (tti + 1) * 128)
                S.dma("sp", "xl", ha[:], h1tok[rows, :], writes=[bha])
                S.dma("sp", "xl", fa[:], ffn[rows, :], writes=[bfa])
                S.op(V, lambda e: e.scalar_tensor_tensor(out=ha[:], in0=ha[:], scalar=ALPHA, in1=fa[:], op0=ALU.mult, op1=ALU.add), reads=[bha, bfa], writes=[bha])
                for q in range(4):
                    S.op(V, lambda e, q=q: e.bn_stats(out=st6[:, q, :], in_=ha[:, q * 512:(q + 1) * 512]), reads=[bha], writes=[bst])
                S.op(V, lambda e: e.bn_aggr(out=mv[:], in_=st6[:].rearrange("p a b -> p (a b)")), reads=[bst], writes=[bst])
                S.op("act", lambda e: e.activation(out=rstd[:], in_=mv[:, 1:2], func=AF.Sqrt, bias=epsln[:, 0:1]), reads=[bst, bC], writes=[bst])
                S.op(V, lambda e: e.reciprocal(out=rstd[:], in_=rstd[:]), reads=[bst], writes=[bst])
                S.op(V, lambda e: e.tensor_scalar(out=fa[:], in0=ha[:], scalar1=mv[:, 0:1], scalar2=rstd[:, 0:1], op0=ALU.subtract, op1=ALU.mult), reads=[bha, bst], writes=[bfa])
                S.op(V, lambda e: e.tensor_tensor(out=fa[:], in0=fa[:], in1=g2[:], op=ALU.mult), reads=[bfa, bg2], writes=[bfa])
                S.op(V, lambda e: e.tensor_tensor(out=fa[:], in0=fa[:], in1=b2[:], op=ALU.add), reads=[bfa, bg2], writes=[bfa])
                fin_toks.append(S.dma("sp", "fin", out_d[rows, :], fa[:], reads=[bfa]))
            S.eng["sp"].wait_ge(S.sem["fin"], S.cnt["fin"])
    return nc


def _consts():
    c = np.arange(128)
    ident = np.eye(128, dtype=np.float32)
    m0 = (c[:, None] > c[None, :]).astype(np.float32)
    m1 = (c[None, :] >= c[:, None]).astype(np.float32)
    m2 = (c[None, :] > c[:, None]).astype(np.float32)
    masks = np.stack([m0, m1, m2], axis=1)
    iota = np.broadcast_to(np.arange(384, dtype=np.float32)[None, :], (128, 384)).copy()
    iotap = (np.arange(128, dtype=np.float32)[:, None] + 128.0 * np.arange(3, dtype=np.float32)[None, :]).copy()
    sel8 = np.zeros((8, 8, 128), np.float32)
    for h in range(8):
        sel8[h, h, :] = 1.0
    sel4 = np.zeros((4, 4, 128), np.float32)
    for h in range(4):
        sel4[h, h, :] = 1.0
    return dict(ident=ident, masks=masks, iota=iota, sel4=sel4, iotap=iotap, sel8=sel8)


def _core_inputs(inp, core, SEQ, stage):
    b, j = core // 4, core % 4
    f = np.float32
    m = {}
    m["x"] = np.ascontiguousarray(inp["x"][b, :SEQ])
    m["lnin"] = np.ascontiguousarray(np.stack([inp["ln_in_g"].reshape(16, 128).T, inp["ln_in_b"].reshape(16, 128).T], axis=1)).astype(f)
    w_in = inp["w_in"][0]
    o = 0
    u_c = w_in[:, 256 * j:256 * (j + 1)]
    base = 1024
    q_c = w_in[:, base + 512 * j: base + 512 * (j + 1)]
    k_c = w_in[:, base + 2048 + 512 * j: base + 2048 + 512 * (j + 1)]
    v_c = w_in[:, base + 4096 + 512 * j: base + 4096 + 512 * (j + 1)]
    z_c = w_in[:, base + 6144 + 512 * j: base + 6144 + 512 * (j + 1)]
    a_c = w_in[:, base + 8192 + 4 * j: base + 8192 + 4 * (j + 1)]
    b_c = w_in[:, base + 8192 + 16 + 4 * j: base + 8192 + 16 + 4 * (j + 1)]
    m["wA"] = np.ascontiguousarray(np.concatenate([u_c, q_c, k_c, v_c, z_c], axis=1))
    m["wab"] = np.ascontiguousarray(np.concatenate([a_c, b_c], axis=1))
    g0 = 16 * j

    def pairlay(a):
        sh = a.shape[2:]
        a = a.reshape((8, 2, 64) + sh)
        a = np.moveaxis(a, 0, 2)
        return np.ascontiguousarray(a.reshape((128, 8) + sh))
    lam_re = inp["lam_re"][0][g0:g0 + 16]
    lam_im = inp["lam_im"][0][g0:g0 + 16]
    lstep = np.broadcast_to(inp["log_step"][0][g0:g0 + 16][:, None], (16, 64))
    m["s5p"] = pairlay(np.stack([lam_re, lam_im, lstep], axis=-1)).astype(f)
    m["s5b"] = pairlay(np.stack([inp["ssm_b_re"][0][g0:g0 + 16], inp["ssm_b_im"][0][g0:g0 + 16]], axis=2)).astype(f)
    cre = np.transpose(inp["ssm_c_re"][0][g0:g0 + 16], (0, 2, 1))
    cim = np.transpose(inp["ssm_c_im"][0][g0:g0 + 16], (0, 2, 1))
    m["s5c"] = pairlay(np.stack([cre, cim], axis=2)).astype(f)
    m["s5d"] = np.ascontiguousarray(inp["ssm_d"][0][g0:g0 + 16].reshape(2, 128).T).astype(f)
    cwf = inp["conv_w"][0][:, 0, :]
    tiles = []
    for part in range(3):
        for h in range(4):
            c0 = part * 2048 + (4 * j + h) * 128
            tiles.append(cwf[:, c0:c0 + 128].T)
    m["convw"] = np.ascontiguousarray(np.stack(tiles, axis=1)).astype(f)
    m["hpar"] = np.ascontiguousarray(np.stack([inp["a_log"][0][4 * j:4 * j + 4], inp["dt_bias"][0][4 * j:4 * j + 4]], axis=1)).astype(f)
    m["dnw"] = np.ascontiguousarray(inp["dn_norm_w"][0].reshape(128, 1)).astype(f)
    m.update(_consts())
    if stage == 15:
        return m
    if stage >= 2:
        NBLK = SEQ // 512
        TPC = SEQ // 4
        NBB = TPC // 512
        i = j
        m["xq"] = np.ascontiguousarray(inp["x"][b, i * TPC:(i + 1) * TPC])
        yidx = np.zeros((128, NBB * 24), np.int32)
        p = np.arange(128, dtype=np.int64)
        for blk in range(NBB):
            gb = i * NBB + blk
            for r in range(4):
                for ft in range(6):
                    yidx[:, blk * 24 + r * 6 + ft] = ((gb * 4 + r) * 6 + ft) * 128 + p
        m["yidx"] = yidx
        m["wg"] = np.ascontiguousarray(w_in[:, 9248:13344])
        m["wglu"] = np.ascontiguousarray(inp["w_glu"][0])
        m["bglu"] = np.ascontiguousarray(inp["b_glu"][0].reshape(8, 128).T).astype(f)
        m["wps"] = np.ascontiguousarray(inp["w_proj_ssm"][0])
        m["wpd"] = np.ascontiguousarray(inp["w_proj_dn"][0])
        m["wout"] = np.ascontiguousarray(inp["w_out"][0])
        m["ln1"] = np.ascontiguousarray(np.stack([inp["ln1_g"][0].reshape(16, 128).T, inp["ln1_b"][0].reshape(16, 128).T], axis=1)).astype(f)
        m["wr"] = np.ascontiguousarray(np.transpose(inp["w_router"][0].reshape(16, 128, 32), (1, 0, 2))).astype(f)
        m["br"] = np.ascontiguousarray(inp["b_router"][0].reshape(32, 1)).astype(f)
    if stage >= 9:
        e0 = 8 * j
        m["wgu"] = np.ascontiguousarray(inp["w_gate_up"][0][e0:e0 + 8])
        m["bgu"] = np.ascontiguousarray(np.transpose(inp["b_gate_up"][0][e0:e0 + 8].reshape(8, 32, 128), (2, 0, 1))).astype(f)
        m["wdn"] = np.ascontiguousarray(inp["w_down"][0][e0:e0 + 8])
        m["bdn"] = np.ascontiguousarray(inp["b_down"][0][e0:e0 + 8]).astype(f)
        m["ln2"] = np.ascontiguousarray(np.stack([inp["ln2_g"][0], inp["ln2_b"][0]], axis=0)).astype(f)
        es = np.zeros((128, 8, 32), f)
        for jj in range(8):
            es[:, jj, e0 + jj] = 1.0
        m["esel"] = es
    return m


def _run(inputs, SEQ, stage):
    nc = build(SEQ, stage)
    in_maps = [_core_inputs(inputs, c, SEQ, stage) for c in range(8)]
    res = run_bass_kernel_spmd(nc, in_maps, core_ids=list(range(8)))
    return [r["out"] for r in res.results]


def kernel(**inputs):
    SEQ = inputs["x"].shape[1]
    outs = _run(inputs, SEQ, 9)
    TPC = SEQ // 4
    out = np.zeros((2, SEQ, D), np.float32)
    for c in range(8):
        b, i = c // 4, c % 4
        out[b, i * TPC:(i + 1) * TPC] = outs[c]
    return out
```

```python
import numpy as np
from contextlib import ExitStack
import concourse.bass as bass
import concourse.mybir as mybir
from concourse.bass_utils import run_bass_kernel_spmd

F32 = mybir.dt.float32
F32R = mybir.dt.float32r
BF16 = mybir.dt.bfloat16
I32 = mybir.dt.int32
ALU = mybir.AluOpType
AF = mybir.ActivationFunctionType
AX = mybir.AxisListType

D = 2048
ALPHA = 2.0 ** 0.25
LN_EPS = 1e-5
NORM_EPS = 1e-6
MAGIC = 12582912.0
TWO_PI = float(2 * np.pi)


class Buf:
    __slots__ = ("name", "w", "r")

    def __init__(self, name=""):
        self.name = name
        self.w = None
        self.r = {}


class Sched:
    def __init__(self, nc, es):
        self.nc = nc
        self.es = es
        self.eng = {"pe": nc.tensor, "act": nc.scalar, "dve": nc.vector, "pool": nc.gpsimd, "sp": nc.sync}
        self.sem, self.cnt, self.step = {}, {}, {}
        for n in self.eng:
            self.sem[n] = es.enter_context(nc.semaphore("sem_" + n))
            self.cnt[n] = 0
            self.step[n] = 1
        self.seen = {n: {} for n in self.eng}

    def stream(self, n, step=16):
        if n not in self.sem:
            self.sem[n] = self.es.enter_context(self.nc.semaphore("sem_" + n))
            self.cnt[n] = 0
            self.step[n] = step
        return n

    def _wait(self, e, tok):
        if tok is None:
            return
        s, c = tok
        if s == e and e == "pe":
            return
        if self.seen[e].get(s, 0) >= c:
            return
        self.eng[e].wait_ge(self.sem[s], c)
        self.seen[e][s] = c

    def deps(self, e, reads, writes, extra=()):
        best = {}

        def add(t):
            if t is not None and best.get(t[0], 0) < t[1]:
                best[t[0]] = t[1]
        for b in reads:
            add(b.w)
        for b in writes:
            add(b.w)
            for s, c in b.r.items():
                add((s, c))
        for t in extra:
            add(t)
        for s, c in best.items():
            self._wait(e, (s, c))

    def mark(self, tok, reads, writes):
        s, c = tok
        for b in reads:
            if b.r.get(s, 0) < c:
                b.r[s] = c
        for b in writes:
            b.w = tok
            b.r = {}

    def op(self, e, fn, reads=(), writes=(), extra=()):
        self.deps(e, reads, writes, extra)
        inst = fn(self.eng[e])
        self.cnt[e] += 1
        inst.then_inc(self.sem[e], 1)
        tok = (e, self.cnt[e])
        self.mark(tok, reads, writes)
        return tok

    def dma(self, q, stream, out, in_, reads=(), writes=(), extra=()):
        self.stream(stream)
        self.deps(q, reads, writes, extra)
        inst = self.eng[q].dma_start(out=out, in_=in_)
        self.cnt[stream] += 16
        inst.then_inc(self.sem[stream], 16)
        tok = (stream, self.cnt[stream])
        self.mark(tok, reads, writes)
        return tok

    def idma(self, stream, out, in_, idx_ap, reads=(), writes=()):
        self.stream(stream)
        self.deps("pool", reads, writes)
        inst = self.nc.gpsimd.indirect_dma_start(out=out, out_offset=None, in_=in_, in_offset=bass.IndirectOffsetOnAxis(ap=idx_ap, axis=0))
        self.cnt[stream] += 16
        inst.then_inc(self.sem[stream], 16)
        tok = (stream, self.cnt[stream])
        self.mark(tok, reads, writes)
        return tok

    def barrier(self):
        for e in self.eng:
            for s in list(self.sem):
                if self.cnt[s] > 0 and not (s == e and e == "pe"):
                    self._wait(e, (s, self.cnt[s]))


def build(SEQ=8192, stage=9):
    nc = bass.Bass("TRN2", target_bir_lowering=False)
    NBLK = SEQ // 512
    TPC = SEQ // 4
    NBB = TPC // 512
    NTOK = 4 * TPC
    TB = min(1024, TPC)
    NTB = NTOK // TB
    NEX = 8
    RPC = min(512, TPC)
    NRS = TPC // RPC

    def din(name, shape, dt=F32):
        return nc.dram_tensor(name, shape, dt, kind="ExternalInput").ap()

    x = din("x", [SEQ, D])
    lnin = din("lnin", [128, 2, 16])
    wA = din("wA", [D, 2304])
    wab = din("wab", [D, 8])
    s5p = din("s5p", [128, 8, 3])
    s5b = din("s5b", [128, 8, 2, 16])
    s5c = din("s5c", [128, 8, 2, 16])
    s5d = din("s5d", [128, 2])
    convw = din("convw", [128, 12, 4])
    hpar = din("hpar", [4, 2])
    dnw = din("dnw", [128, 1])
    ident_d = din("ident", [128, 128])
    masks_d = din("masks", [128, 3, 128])
    iota_d = din("iota", [128, 384])
    iotap_d = din("iotap", [128, 3])
    sel8_d = din("sel8", [8, 8, 128])
    sel4_d = din("sel4", [4, 4, 128])
    if stage >= 2 and stage != 15:
        xq = din("xq", [TPC, D])
        yidx_d = din("yidx", [128, NBB * 24], I32)
        wg = din("wg", [D, 4096])
        wglu = din("wglu", [1024, 1024])
        bglu = din("bglu", [128, 8])
        wps = din("wps", [1024, D])
        wpd = din("wpd", [D, D])
        wout = din("wout", [D, D])
        ln1 = din("ln1", [128, 2, 16])
        wr = din("wr", [128, 16, 32])
        br = din("br", [32, 1])
    if stage >= 9 and stage != 15:
        wgu = din("wgu", [NEX, D, 4096])
        bgu = din("bgu", [128, NEX, 32])
        wdn = din("wdn", [NEX, D, D])
        bdn = din("bdn", [NEX, D])
        ln2 = din("ln2", [2, D])
        esel_d = din("esel", [128, NEX, 32])

    if stage in (1, 15):
        out_d = nc.dram_tensor("out", [768, SEQ], BF16, kind="ExternalOutput").ap()
    else:
        out_d = nc.dram_tensor("out", [TPC, D], F32, kind="ExternalOutput").ap()

    yloc = nc.dram_tensor("yloc", [NBLK * 768, 512], BF16)
    yall = nc.dram_tensor("yall", [4 * NBLK * 768, 512], BF16)
    if stage >= 2 and stage != 15:
        HC = TPC // 256
        h1b_loc = nc.dram_tensor("h1b_loc", [TPC, D], BF16)
        h1b_all = nc.dram_tensor("h1b_all", [HC * 4 * 256, D], BF16)
        CAP = 384
        XTd = nc.dram_tensor("XTd", [NEX * D, 4 * CAP], BF16)
        Yd = nc.dram_tensor("Yd", [NEX * 4 * CAP, D], BF16)
        gate_loc = nc.dram_tensor("gate_loc", [TPC, 32], F32)
        gate_all = nc.dram_tensor("gate_all", [4 * TPC, 32], F32)
        h1tok = nc.dram_tensor("h1tok", [TPC, D], F32)
        dense = nc.dram_tensor("dense", [NRS * 4 * RPC, D], F32)
        ffn = nc.dram_tensor("ffn", [TPC, D], F32)

    with ExitStack() as es:
        S = Sched(nc, es)
        cc_sem = es.enter_context(nc.semaphore("cc_sem"))
        cc_cnt = [0]

        def sbt(stack, name, shape, dt=F32):
            return stack.enter_context(nc.sbuf_tensor("s_" + name, shape, dt))

        banks = [es.enter_context(nc.psum_tensor("pb%d" % i, [128, 512], F32)) for i in range(8)]
        bbuf = [Buf("pb%d" % i) for i in range(8)]
        brr = [0]

        def bank():
            i = brr[0]
            brr[0] = (i + 1) % 8
            return banks[i], bbuf[i]

        ident = sbt(es, "ident", [128, 128])
        ones = sbt(es, "ones", [128, 128])
        masks = sbt(es, "masks", [128, 3, 128])
        epsln = sbt(es, "epsln", [128, 1])
        epsnm = sbt(es, "epsnm", [128, 1])
        bC = Buf("consts")
        S.dma("sp", "cst", ident[:], ident_d[:, :], writes=[bC])
        S.dma("sp", "cst", masks[:], masks_d[:, :, :], writes=[bC])
        S.op("dve", lambda e: e.memset(ones[:], 1.0), writes=[bC])
        S.op("dve", lambda e: e.memset(epsln[:], LN_EPS), writes=[bC])
        S.op("dve", lambda e: e.memset(epsnm[:], NORM_EPS), writes=[bC])

        wbuf = [sbt(es, "wbuf%d" % i, [128, 16, 256], BF16) for i in range(2)]
        wbb = [Buf("wbuf%d" % i) for i in range(2)]
        wrr = [0]

        def load_w(src_ap, kc, ncols):
            i = wrr[0] % len(wbuf)
            wrr[0] = (i + 1) % len(wbuf)
            S.dma("pool", "wl%d" % i, wbuf[i][:, 0:kc, 0:ncols], src_ap.rearrange("(kc p) m -> p kc m", p=128), writes=[wbb[i]])
            return wbuf[i], wbb[i]

        def linear(wsrc, kc, mcols, rhs_fn, rhs_bufs, n, epilogue, group=256):
            mi = 0
            for c0 in range(0, mcols, group):
                gc = min(group, mcols - c0)
                wt, wb = load_w(wsrc[:, c0:c0 + gc], kc, gc)
                for m0 in range(0, gc, 128):
                    mw = min(128, gc - m0)
                    pb, pbb = bank()
                    for k in range(kc):
                        S.op("pe", lambda e, k=k, m0=m0, mw=mw, pb=pb: e.matmul(out=pb[0:mw, 0:n], lhsT=wt[:, k, m0:m0 + mw], rhs=rhs_fn(k), start=(k == 0), stop=(k == kc - 1)),
                             reads=[wb] + list(rhs_bufs), writes=[pbb])
                    epilogue(mi, pb, pbb)
                    mi += 1

        def layer_norm_T(pst, xsrc_rows, gb_tile, bgb, dstT, bdst, tcol0, xt, bxt, xn, bxn, st6, mv, rstd, bst):
            S.dma("sp", "xl", xt[:], xsrc_rows, writes=[bxt])
            for q in range(4):
                S.op("dve", lambda e, q=q: e.bn_stats(out=st6[:, q, :], in_=xt[:, q * 512:(q + 1) * 512]), reads=[bxt], writes=[bst])
            S.op("dve", lambda e: e.bn_aggr(out=mv[:], in_=st6[:].rearrange("p a b -> p (a b)")), reads=[bst], writes=[bst])
            S.op("act", lambda e: e.activation(out=rstd[:], in_=mv[:, 1:2], func=AF.Sqrt, bias=epsln[:, 0:1]), reads=[bst, bC], writes=[bst])
            S.op("dve", lambda e: e.reciprocal(out=rstd[:], in_=rstd[:]), reads=[bst], writes=[bst])
            S.op("dve", lambda e: e.tensor_scalar(out=xn[:], in0=xt[:], scalar1=mv[:, 0:1], scalar2=rstd[:, 0:1], op0=ALU.subtract, op1=ALU.mult), reads=[bxt, bst], writes=[bxn])
            for kq in range(4):
                pb, pbb = bank()
                for r in range(4):
                    k = kq * 4 + r
                    S.op("pe", lambda e, k=k, r=r, pb=pb: e.transpose(out=pb[:, r * 128:(r + 1) * 128], in_=xn[:, k * 128:(k + 1) * 128], identity=ident[:]), reads=[bxn, bC], writes=[pbb])
                for r in range(4):
                    k = kq * 4 + r
                    for (dt_, bd_) in zip(dstT, bdst):
                        S.op("act", lambda e, k=k, r=r, pb=pb, dt_=dt_: e.activation(out=dt_[:, k, tcol0:tcol0 + 128], in_=pb[:, r * 128:(r + 1) * 128], func=AF.Identity, scale=gb_tile[:, 0, k:k + 1], bias=gb_tile[:, 1, k:k + 1]),
                             reads=[pbb, bgb], writes=[bd_])

        with ExitStack() as pa:
            lnin_t = sbt(pa, "lnin_t", [128, 2, 16]); blnin = Buf()
            S.dma("sp", "cst", lnin_t[:], lnin[:, :, :], writes=[blnin])
            xt = sbt(pa, "xt", [128, D]); bxt = Buf()
            xn = sbt(pa, "xn", [128, D]); bxn = Buf()
            st6 = sbt(pa, "st6", [128, 4, 6]); mv = sbt(pa, "mv", [128, 2]); rstd = sbt(pa, "rstd", [128, 1]); bst = Buf()
            hT = sbt(pa, "hT", [128, 16, 512], BF16); bhT = Buf()
            iota = sbt(pa, "iota", [128, 132]); biota = Buf()
            S.dma("sp", "cst", iota[:], iota_d[:, 0:132], writes=[biota])

            p5 = sbt(pa, "p5", [128, 8, 3]); b5 = sbt(pa, "b5", [128, 8, 2, 16]); c5 = sbt(pa, "c5", [128, 8, 2, 16]); d5 = sbt(pa, "d5", [128, 2])
            bp5 = Buf()
            S.dma("sp", "cst", p5[:], s5p[:, :, :], writes=[bp5])
            S.dma("sp", "cst", b5[:], s5b[:, :, :, :], writes=[bp5])
            S.dma("sp", "cst", c5[:], s5c[:, :, :, :], writes=[bp5])
            S.dma("sp", "cst", d5[:], s5d[:, :], writes=[bp5])
            sc = sbt(pa, "s5sc", [128, 16, 8]); bsc = Buf()
            LR, LI, STEP, MAG, TH, RHO, COS1, SIN1, ARE, AIM, DEN, FRE, FIM, T0, T1, T2 = [sc[:, i, :] for i in range(16)]
            V = "dve"

            def ts(out, in0, s1, s2, o0, o1=None):
                if o1 is None:
                    S.op(V, lambda e: e.tensor_scalar(out=out, in0=in0, scalar1=s1, scalar2=None, op0=o0), reads=[bsc, bp5], writes=[bsc])
                else:
                    S.op(V, lambda e: e.tensor_scalar(out=out, in0=in0, scalar1=s1, scalar2=s2, op0=o0, op1=o1), reads=[bsc, bp5], writes=[bsc])

            def tt(out, a, b, op):
                S.op(V, lambda e: e.tensor_tensor(out=out, in0=a, in1=b, op=op), reads=[bsc, bp5], writes=[bsc])

            def act(out, in_, func, scale=1.0, bias=0.0):
                S.op("act", lambda e: e.activation(out=out, in_=in_, func=func, scale=scale, bias=bias), reads=[bsc, bp5, bC], writes=[bsc])

            def sincos(out_s, out_c, ang, t0, t1):
                for (o, sh) in ((out_s, 0.0), (out_c, float(np.pi / 2))):
                    ts(t0, ang, sh, None, ALU.add)
                    ts(t1, t0, float(1 / TWO_PI), MAGIC, ALU.mult, ALU.add)
                    ts(t1, t1, -MAGIC, None, ALU.add)
                    S.op(V, lambda e, t0=t0, t1=t1: e.scalar_tensor_tensor(out=t0, in0=t1, scalar=-TWO_PI, in1=t0, op0=ALU.mult, op1=ALU.add), reads=[bsc], writes=[bsc])
                    act(o, t0, AF.Sin)

            act(STEP, p5[:, :, 2], AF.Exp)
            tt(T0, p5[:, :, 0], STEP, ALU.mult)
            act(RHO, T0, AF.Exp)
            tt(TH, p5[:, :, 1], STEP, ALU.mult)
            sincos(SIN1, COS1, TH, T0, T1)
            tt(ARE, RHO, COS1, ALU.mult)
            tt(AIM, RHO, SIN1, ALU.mult)
            tt(T0, p5[:, :, 0], p5[:, :, 0], ALU.mult)
            tt(T1, p5[:, :, 1], p5[:, :, 1], ALU.mult)
            tt(DEN, T0, T1, ALU.add)
            S.op(V, lambda e: e.reciprocal(out=DEN, in_=DEN), reads=[bsc], writes=[bsc])
            ts(T2, ARE, -1.0, None, ALU.add)
            tt(T0, T2, p5[:, :, 0], ALU.mult)
            tt(T1, AIM, p5[:, :, 1], ALU.mult)
            tt(T0, T0, T1, ALU.add)
            tt(FRE, T0, DEN, ALU.mult)
            tt(T0, AIM, p5[:, :, 0], ALU.mult)
            tt(T1, T2, p5[:, :, 1], ALU.mult)
            tt(T0, T0, T1, ALU.subtract)
            tt(FIM, T0, DEN, ALU.mult)
            bb = sbt(pa, "bb", [128, 8, 2, 16]); tb = sbt(pa, "tbb", [128, 8, 16])
            freB = sc[:, 11, :].unsqueeze(2).to_broadcast([128, 8, 16])
            fimB = sc[:, 12, :].unsqueeze(2).to_broadcast([128, 8, 16])
            tt(bb[:, :, 0, :], b5[:, :, 0, :], freB, ALU.mult)
            tt(tb[:], b5[:, :, 1, :], fimB, ALU.mult)
            tt(bb[:, :, 0, :], bb[:, :, 0, :], tb[:], ALU.subtract)
            tt(bb[:, :, 1, :], b5[:, :, 1, :], freB, ALU.mult)
            tt(tb[:], b5[:, :, 0, :], fimB, ALU.mult)
            tt(bb[:, :, 1, :], bb[:, :, 1, :], tb[:], ALU.add)
            BbTz = sbt(pa, "BbTz", [128, 8, 2, 128]); CTz = sbt(pa, "CTz", [128, 8, 2, 128]); bdp = sbt(pa, "bdp", [128, 128])
            btab = Buf()
            S.op(V, lambda e: e.memset(CTz[:].rearrange("p a b c -> p (a b c)"), 0.0), writes=[btab])
            for gp in range(8):
                gl = gp % 4
                for ri in range(2):
                    S.op(V, lambda e: e.memset(bdp[:], 0.0), reads=[bsc], writes=[bsc])
                    for g2 in range(2):
                        c0 = 32 * gl + 16 * g2
                        S.op(V, lambda e, g2=g2, c0=c0, gp=gp, ri=ri: e.tensor_copy(out=bdp[64 * g2:64 * g2 + 64, c0:c0 + 16], in_=bb[64 * g2:64 * g2 + 64, gp, ri, :]), reads=[bsc], writes=[bsc])
                        if ri == 0:
                            S.op(V, lambda e, g2=g2, c0=c0, gp=gp: e.tensor_copy(out=CTz[64 * g2:64 * g2 + 64, gp, 0, c0:c0 + 16], in_=c5[64 * g2:64 * g2 + 64, gp, 0, :]), reads=[bp5], writes=[btab])
                        else:
                            S.op(V, lambda e, g2=g2, c0=c0, gp=gp: e.tensor_scalar(out=CTz[64 * g2:64 * g2 + 64, gp, 1, c0:c0 + 16], in0=c5[64 * g2:64 * g2 + 64, gp, 1, :], scalar1=-1.0, scalar2=None, op0=ALU.mult), reads=[bp5], writes=[btab])
                    pb, pbb = bank()
                    S.op("pe", lambda e, pb=pb: e.transpose(out=pb[:, 0:128], in_=bdp[:], identity=ident[:]), reads=[bsc, bC], writes=[pbb])
                    S.op("act", lambda e, pb=pb, gp=gp, ri=ri: e.copy(out=BbTz[:, gp, ri, :], in_=pb[:, 0:128]), reads=[pbb], writes=[btab])
            cst = sbt(pa, "cstab", [128, 8, 2, 132]); rhoT = sbt(pa, "rhoT", [128, 8, 128]); ph = sbt(pa, "ph", [128, 132]); pt0 = sbt(pa, "pt0", [128, 132]); pt1 = sbt(pa, "pt1", [128, 132])
            for gp in range(8):
                S.op(V, lambda e, gp=gp: e.tensor_scalar(out=ph[:], in0=iota[:], scalar1=sc[:, 4, gp:gp + 1], scalar2=None, op0=ALU.mult), reads=[bsc, biota], writes=[bsc])
                sincos(cst[:, gp, 1, :], cst[:, gp, 0, :], ph[:], pt0[:], pt1[:])
                S.op(V, lambda e, gp=gp: e.tensor_scalar(out=rhoT[:, gp, :], in0=ones[:], scalar1=sc[:, 5, gp:gp + 1], scalar2=None, op0=ALU.mult), reads=[bsc, bC], writes=[bsc])
            S.op(V, lambda e: e.tensor_copy(out=ph[:, 0:1], in_=ph[:, 0:1]), reads=[bsc], writes=[btab])
            carry = sbt(pa, "carry", [128, 8, 2]); bcar = Buf()
            S.op(V, lambda e: e.memset(carry[:].rearrange("p a b -> p (a b)"), 0.0), writes=[bcar])

            cw = sbt(pa, "cw", [128, 12, 4]); hp = sbt(pa, "hp", [4, 2]); nw = sbt(pa, "nw", [128, 1]); sel4 = sbt(pa, "sel4", [4, 4, 128])
            bgp = Buf()
            S.dma("sp", "cst", cw[:], convw[:, :, :], writes=[bgp])
            S.dma("sp", "cst", hp[:], hpar[:, :], writes=[bgp])
            S.dma("sp", "cst", nw[:], dnw[:, :], writes=[bgp])
            S.dma("sp", "cst", sel4[:], sel4_d[:, :, :], writes=[bgp])
            nexpA = sbt(pa, "nexpA", [4, 1])
            S.op("act", lambda e: e.activation(out=nexpA[:], in_=hp[:, 0:1], func=AF.Exp), reads=[bgp], writes=[bgp])
            S.op(V, lambda e: e.tensor_scalar(out=nexpA[:], in0=nexpA[:], scalar1=-1.0, scalar2=None, op0=ALU.mult), reads=[bgp], writes=[bgp])
            ones4 = sbt(pa, "ones4", [4, 128])
            S.op(V, lambda e: e.memset(ones4[:], 1.0), writes=[bgp])
            halo = sbt(pa, "halo", [128, 12, 3]); bhalo = Buf()
            S.op(V, lambda e: e.memset(halo[:].rearrange("p a b -> p (a b)"), 0.0), writes=[bhalo])
            Sst = sbt(pa, "Sst", [128, 4, 128], F32R); bS = Buf()
            S.op(V, lambda e: e.tensor_scalar(out=Sst[:], in0=ones[:, :].unsqueeze(1).to_broadcast([128, 4, 128]), scalar1=0.0, scalar2=None, op0=ALU.mult), reads=[bC], writes=[bS])

            uT = sbt(pa, "uT", [128, 2, 512]); buT = Buf()
            rtmp = [sbt(pa, "rtmp%d" % i, [128, 515]) for i in range(2)]; brtmp = [Buf(), Buf()]
            qkv = sbt(pa, "qkv", [128, 12, 512]); bqkv = [Buf() for _ in range(12)]
            qe = sbt(pa, "qe", [128, 4, 512], F32R); bqe = Buf()
            zs = sbt(pa, "zs", [128, 4, 512], BF16); bzs = Buf()
            abr = sbt(pa, "abr", [4, 2, 512]); babr = Buf()
            ysb = sbt(pa, "ysb", [128, 2, 512], BF16); bysb = Buf()
            ydn = sbt(pa, "ydn", [128, 4, 512], BF16); bydn = Buf()
            W = [sbt(pa, "wk%d" % i, [128, 512]) for i in range(25)]
            bW = [Buf("wk%d" % i) for i in range(25)]
            gsm = sbt(pa, "gsm", [128, 8, 16]); bgsm = Buf()
            grow = sbt(pa, "grow", [4, 3, 512]); bgrow = Buf()

            for blk in range(NBLK):
                t0 = blk * 512
                for tti in range(4):
                    layer_norm_T(pa, x[t0 + tti * 128:t0 + (tti + 1) * 128, :], lnin_t, blnin, [hT], [bhT], tti * 128, xt, bxt, xn, bxn, st6, mv, rstd, bst)

                def ep_A(mi, pb, pbb):
                    if mi < 2:
                        S.op("act", lambda e: e.copy(out=uT[:, mi, :], in_=pb[:, :]), reads=[pbb], writes=[buT])
                    elif mi < 14:
                        ct = mi - 2
                        r = rtmp[ct % 2]; br_ = brtmp[ct % 2]
                        S.op("act", lambda e: e.copy(out=r[:, 3:515], in_=pb[:, :]), reads=[pbb], writes=[br_])
                        S.op("act", lambda e: e.copy(out=r[:, 0:3], in_=halo[:, ct, :]), reads=[bhalo], writes=[br_])
                        S.op("act", lambda e: e.copy(out=halo[:, ct, :], in_=r[:, 512:515]), reads=[br_], writes=[bhalo])
                        acc = W[0]; bacc = bW[0]
                        S.op(V, lambda e: e.tensor_scalar(out=acc[:], in0=r[:, 3:515], scalar1=cw[:, ct, 3:4], scalar2=None, op0=ALU.mult), reads=[br_, bgp], writes=[bacc])
                        for jj in (2, 1, 0):
                            S.op(V, lambda e, jj=jj: e.scalar_tensor_tensor(out=acc[:], in0=r[:, jj:jj + 512], scalar=cw[:, ct, jj:jj + 1], in1=acc[:], op0=ALU.mult, op1=ALU.add), reads=[br_, bgp, bacc], writes=[bacc])
                        qo = qkv[:, ct, :].bitcast(F32R)
                        S.op("act", lambda e: e.activation(out=qo, in_=acc[:], func=AF.Silu), reads=[bacc], writes=[bqkv[ct]])
                    else:
                        hh = mi - 14
                        S.op("act", lambda e: e.activation(out=zs[:, hh, :], in_=pb[:, :], func=AF.Silu), reads=[pbb], writes=[bzs])

                linear(wA, 16, 2304, lambda k: hT[:, k, :], [bhT], 512, ep_A)

                def ep_ab(mi, pb, pbb):
                    S.op("act", lambda e: e.copy(out=abr[:, mi, :], in_=pb[0:4, :]), reads=[pbb], writes=[babr])
                for which in range(2):
                    i = wrr[0] % len(wbuf); wrr[0] = (i + 1) % len(wbuf)
                    S.dma("pool", "wl%d" % i, wbuf[i][:, 0:16, 0:4], wab[:, which * 4:which * 4 + 4].rearrange("(kc p) m -> p kc m", p=128), writes=[wbb[i]])
                    pb, pbb = bank()
                    for k in range(16):
                        S.op("pe", lambda e, k=k, i=i, pb=pb: e.matmul(out=pb[0:4, :], lhsT=wbuf[i][:, k, 0:4], rhs=hT[:, k, :], start=(k == 0), stop=(k == 15)), reads=[wbb[i], bhT], writes=[pbb])
                    ep_ab(which, pb, pbb)

                for hf in range(2):
                    ypb, ypbb = bank()
                    xs = []
                    for gl in range(4):
                        gp = hf * 4 + gl
                        bure, bbre = bank(); buim, bbim = bank()
                        S.op("pe", lambda e, gp=gp, bure=bure: e.matmul(out=bure[:, :], lhsT=BbTz[:, gp, 0, :], rhs=uT[:, hf, :], start=True, stop=True), reads=[btab, buT], writes=[bbre])
                        S.op("pe", lambda e, gp=gp, buim=buim: e.matmul(out=buim[:, :], lhsT=BbTz[:, gp, 1, :], rhs=uT[:, hf, :], start=True, stop=True), reads=[btab, buT], writes=[bbim])
                        cB = cst[:, gp, 0, 0:128].unsqueeze(1).to_broadcast([128, 4, 128])
                        sB = cst[:, gp, 1, 0:128].unsqueeze(1).to_broadcast([128, 4, 128])
                        t1, t2, btr, bti = W[0], W[1], W[2], W[3]
                        xre, xim = W[4 + 2 * gl], W[5 + 2 * gl]
                        wl = [bW[0], bW[1], bW[2], bW[3]]

                        def v3(t):
                            return t[:].rearrange("p (a b) -> p a b", a=4)

                        def p3(t):
                            return t[:, :].rearrange("p (a b) -> p a b", a=4)
                        S.op(V, lambda e: e.tensor_tensor(out=v3(t1), in0=p3(bure), in1=cB, op=ALU.mult), reads=[bbre, btab], writes=[bW[0]])
                        S.op(V, lambda e: e.tensor_tensor(out=v3(t2), in0=p3(buim), in1=sB, op=ALU.mult), reads=[bbim, btab], writes=[bW[1]])
                        S.op(V, lambda e: e.tensor_tensor(out=btr[:], in0=t1[:], in1=t2[:], op=ALU.add), reads=[bW[0], bW[1]], writes=[bW[2]])
                        S.op(V, lambda e: e.tensor_tensor(out=v3(t1), in0=p3(buim), in1=cB, op=ALU.mult), reads=[bbim, btab], writes=[bW[0]])
                        S.op(V, lambda e: e.tensor_tensor(out=v3(t2), in0=p3(bure), in1=sB, op=ALU.mult), reads=[bbre, btab], writes=[bW[1]])
                        S.op(V, lambda e: e.tensor_tensor(out=bti[:], in0=t1[:], in1=t2[:], op=ALU.subtract), reads=[bW[0], bW[1]], writes=[bW[3]])
                        for sb_ in range(4):
                            cs_ = slice(sb_ * 128, (sb_ + 1) * 128)
                            S.op(V, lambda e, cs_=cs_: e.tensor_tensor_scan(out=t1[:, cs_], data0=rhoT[:, gp, :], data1=btr[:, cs_], initial=carry[:, gp, 0:1], op0=ALU.mult, op1=ALU.add), reads=[bW[2], btab, bcar], writes=[bW[0]])
                            S.op(V, lambda e, cs_=cs_: e.tensor_tensor_scan(out=t2[:, cs_], data0=rhoT[:, gp, :], data1=bti[:, cs_], initial=carry[:, gp, 1:2], op0=ALU.mult, op1=ALU.add), reads=[bW[3], btab, bcar], writes=[bW[1]])
                            e1 = sb_ * 128 + 127
                            cr, ci = cst[:, gp, 0, 128:129], cst[:, gp, 1, 128:129]
                            tmpc = gsm[:, 7, 0:2]
                            S.op(V, lambda e: e.tensor_scalar(out=tmpc[:, 0:1], in0=t2[:, e1:e1 + 1], scalar1=ci, scalar2=-1.0, op0=ALU.mult, op1=ALU.mult), reads=[bW[1], btab], writes=[bgsm])
                            S.op(V, lambda e: e.tensor_scalar(out=tmpc[:, 1:2], in0=t1[:, e1:e1 + 1], scalar1=ci, scalar2=None, op0=ALU.mult), reads=[bW[0], btab], writes=[bgsm])
                            S.op(V, lambda e: e.scalar_tensor_tensor(out=carry[:, gp, 0:1], in0=t1[:, e1:e1 + 1], scalar=cr, in1=tmpc[:, 0:1], op0=ALU.mult, op1=ALU.add), reads=[bW[0], bgsm, btab], writes=[bcar])
                            S.op(V, lambda e: e.scalar_tensor_tensor(out=carry[:, gp, 1:2], in0=t2[:, e1:e1 + 1], scalar=cr, in1=tmpc[:, 1:2], op0=ALU.mult, op1=ALU.add), reads=[bW[1], bgsm, btab], writes=[bcar])
                        S.op(V, lambda e: e.tensor_tensor(out=v3(btr), in0=v3(t1), in1=cB, op=ALU.mult), reads=[bW[0], btab], writes=[bW[2]])
                        S.op(V, lambda e: e.tensor_tensor(out=v3(bti), in0=v3(t2), in1=sB, op=ALU.mult), reads=[bW[1], btab], writes=[bW[3]])
                        S.op(V, lambda e: e.tensor_tensor(out=xre[:], in0=btr[:], in1=bti[:], op=ALU.subtract), reads=[bW[2], bW[3]], writes=[bW[4 + 2 * gl]])
                        S.op(V, lambda e: e.tensor_tensor(out=v3(btr), in0=v3(t2), in1=cB, op=ALU.mult), reads=[bW[1], btab], writes=[bW[2]])
                        S.op(V, lambda e: e.tensor_tensor(out=v3(bti), in0=v3(t1), in1=sB, op=ALU.mult), reads=[bW[0], btab], writes=[bW[3]])
                        S.op(V, lambda e: e.tensor_tensor(out=xim[:], in0=btr[:], in1=bti[:], op=ALU.add), reads=[bW[2], bW[3]], writes=[bW[5 + 2 * gl]])
                        xs.append((gp, xre, xim, bW[4 + 2 * gl], bW[5 + 2 * gl]))
                    for n_, (gp, xre, xim, b1_, b2_) in enumerate(xs):
                        S.op("pe", lambda e, gp=gp, xre=xre: e.matmul(out=ypb[:, :], lhsT=CTz[:, gp, 0, :], rhs=xre[:], start=(n_ == 0), stop=False), reads=[btab, b1_], writes=[ypbb])
                        S.op("pe", lambda e, gp=gp, xim=xim: e.matmul(out=ypb[:, :], lhsT=CTz[:, gp, 1, :], rhs=xim[:], start=False, stop=(n_ == 3)), reads=[btab, b2_], writes=[ypbb])
                    yt = W[12]
                    S.op(V, lambda e: e.scalar_tensor_tensor(out=yt[:], in0=uT[:, hf, :], scalar=d5[:, hf:hf + 1], in1=ypb[:, :], op0=ALU.mult, op1=ALU.add), reads=[buT, bp5, ypbb], writes=[bW[12]])
                    S.op("act", lambda e: e.activation(out=ysb[:, hf, :], in_=yt[:], func=AF.Gelu), reads=[bW[12]], writes=[bysb])
                S.dma("sp", "st", yloc[blk * 768:blk * 768 + 256, :].rearrange("(a p) t -> p a t", p=128), ysb[:], reads=[bysb])

                for ct in range(8):
                    i0_ = 2 * (ct % 4); i1_ = i0_ + 1
                    sq = W[i0_]
                    S.op("act", lambda e: e.activation(out=sq[:], in_=qkv[:, ct, :], func=AF.Square), reads=[bqkv[ct]], writes=[bW[i0_]])
                    pb, pbb = bank()
                    S.op("pe", lambda e, pb=pb: e.matmul(out=pb[:, :], lhsT=ones[:], rhs=sq[:], start=True, stop=True), reads=[bC, bW[i0_]], writes=[pbb])
                    rs = W[i1_]
                    S.op("act", lambda e, pb=pb: e.activation(out=rs[:], in_=pb[:, :], func=AF.Sqrt, bias=epsnm[:, 0:1]), reads=[pbb, bC], writes=[bW[i1_]])
                    S.op(V, lambda e: e.reciprocal(out=rs[:], in_=rs[:]), reads=[bW[i1_]], writes=[bW[i1_]])
                    S.op(V, lambda e, ct=ct: e.scalar_tensor_tensor(out=qkv[:, ct, :].bitcast(F32R), in0=qkv[:, ct, :], scalar=(128.0 ** -0.5 if ct < 4 else 1.0), in1=rs[:], op0=ALU.mult, op1=ALU.mult), reads=[bqkv[ct], bW[i1_]], writes=[bqkv[ct]])
                S.op("act", lambda e: e.activation(out=grow[:, 2, :], in_=abr[:, 1, :], func=AF.Sigmoid), reads=[babr], writes=[bgrow])
                S.op("act", lambda e: e.activation(out=grow[:, 0, :], in_=abr[:, 0, :], func=AF.Exp, bias=hp[:, 1:2]), reads=[babr, bgp], writes=[bgrow])
                S.op("act", lambda e: e.activation(out=grow[:, 0, :], in_=grow[:, 0, :], func=AF.Ln, bias=1.0), reads=[bgrow], writes=[bgrow])
                S.op(V, lambda e: e.tensor_scalar(out=grow[:, 0, :], in0=grow[:, 0, :], scalar1=nexpA[:, 0:1], scalar2=None, op0=ALU.mult), reads=[bgrow, bgp], writes=[bgrow])
                for ch in range(4):
                    cs_ = slice(ch * 128, (ch + 1) * 128)
                    S.op(V, lambda e, cs_=cs_: e.tensor_tensor_scan(out=grow[:, 1, cs_], data0=ones4[:, :], data1=grow[:, 0, cs_], initial=0.0, op0=ALU.mult, op1=ALU.add), reads=[bgrow, bgp], writes=[bgrow])
                pbc, pbcb = bank()
                for ch in range(4):
                    for kind, row in ((0, 1), (1, 2)):
                        S.op("pe", lambda e, ch=ch, kind=kind, row=row: e.transpose(out=pbc[:, kind * 16 + ch * 4:kind * 16 + ch * 4 + 4], in_=grow[:, row, ch * 128:(ch + 1) * 128], identity=ident[0:4, 0:4]), reads=[bgrow, bC], writes=[pbcb])
                S.op("act", lambda e: e.copy(out=gsm[:, 0:2, :], in_=pbc[:, 0:32].rearrange("p (a b) -> p a b", a=2)), reads=[pbcb], writes=[bgsm])

                for ch in range(4):
                    cs_ = slice(ch * 128, (ch + 1) * 128)
                    hc = slice(ch * 4, ch * 4 + 4)
                    pg, pgb = bank(); pbt, pbtb = bank()
                    for h in range(4):
                        S.op("pe", lambda e, h=h, pg=pg: e.matmul(out=pg[:, h * 128:(h + 1) * 128], lhsT=sel4[:, h, :], rhs=grow[:, 1, cs_], start=True, stop=True), reads=[bgp, bgrow], writes=[pgb])
                        S.op("pe", lambda e, h=h, pbt=pbt: e.matmul(out=pbt[:, h * 128:(h + 1) * 128], lhsT=sel4[:, h, :], rhs=grow[:, 2, cs_], start=True, stop=True), reads=[bgp, bgrow], writes=[pbtb])
                    gcRB, beRB, egRB = W[0], W[1], W[2]
                    S.op("act", lambda e, pg=pg: e.copy(out=gcRB[:], in_=pg[:, :]), reads=[pgb], writes=[bW[0]])
                    S.op("act", lambda e, pbt=pbt: e.copy(out=beRB[:], in_=pbt[:, :]), reads=[pbtb], writes=[bW[1]])
                    S.op("act", lambda e, pg=pg: e.activation(out=egRB[:], in_=pg[:, :], func=AF.Exp), reads=[pgb], writes=[bW[2]])

                    def h3(t):
                        return t[:].rearrange("p (a b) -> p a b", a=4)

                    def colB(kind):
                        return gsm[:, kind, hc].unsqueeze(2).to_broadcast([128, 4, 128])
                    S.op("act", lambda e: e.activation(out=gsm[:, 2, hc], in_=gsm[:, 0, hc], func=AF.Exp), reads=[bgsm], writes=[bgsm])
                    S.op(V, lambda e: e.tensor_tensor(out=gsm[:, 3, hc], in0=gsm[:, 1, hc], in1=gsm[:, 2, hc], op=ALU.mult), reads=[bgsm], writes=[bgsm])
                    S.op(V, lambda e: e.tensor_copy(out=gsm[:, 4, hc], in_=h3(gcRB)[:, :, 127]), reads=[bW[0]], writes=[bgsm])
                    S.op(V, lambda e: e.tensor_tensor(out=gsm[:, 5, hc], in0=gsm[:, 4, hc], in1=gsm[:, 0, hc], op=ALU.subtract), reads=[bgsm], writes=[bgsm])
                    S.op("act", lambda e: e.activation(out=gsm[:, 5, hc], in_=gsm[:, 5, hc], func=AF.Exp), reads=[bgsm], writes=[bgsm])
                    S.op("act", lambda e: e.activation(out=gsm[:, 6, hc], in_=gsm[:, 4, hc], func=AF.Exp), reads=[bgsm], writes=[bgsm])
                    for h in range(4):
                        S.op(V, lambda e, h=h: e.tensor_tensor(out=qe[:, h, cs_], in0=qkv[:, h, cs_], in1=egRB[:, h * 128:(h + 1) * 128], op=ALU.mult), reads=[bqkv[h], bW[2]], writes=[bqe])
                    A1, E1, Dm, E2, DqT, DmT = W[3], W[4], W[5], W[6], W[7], W[8]
                    S.op(V, lambda e: e.tensor_tensor(out=h3(A1), in0=h3(gcRB), in1=colB(0), op=ALU.subtract), reads=[bW[0], bgsm], writes=[bW[3]])
                    S.op(V, lambda e: e.tensor_scalar(out=E2[:], in0=A1[:], scalar1=0.0, scalar2=None, op0=ALU.min), reads=[bW[3]], writes=[bW[6]])
                    S.op(V, lambda e: e.tensor_scalar(out=A1[:], in0=A1[:], scalar1=0.0, scalar2=None, op0=ALU.max), reads=[bW[3]], writes=[bW[3]])
                    S.op("act", lambda e: e.activation(out=E1[:], in_=A1[:], func=AF.Exp, scale=-1.0), reads=[bW[3]], writes=[bW[4]])
                    S.op("act", lambda e: e.activation(out=E2[:], in_=E2[:], func=AF.Exp), reads=[bW[6]], writes=[bW[6]])
                    mS = masks[:, 0, :].unsqueeze(1).to_broadcast([128, 4, 128])
                    mIT = masks[:, 1, :].unsqueeze(1).to_broadcast([128, 4, 128])
                    mST = masks[:, 2, :].unsqueeze(1).to_broadcast([128, 4, 128])
                    S.op(V, lambda e: e.tensor_tensor(out=h3(Dm), in0=h3(E1), in1=colB(1), op=ALU.mult), reads=[bW[4], bgsm], writes=[bW[5]])
                    S.op(V, lambda e: e.tensor_tensor(out=h3(Dm), in0=h3(Dm), in1=mS, op=ALU.mult), reads=[bW[5], bC], writes=[bW[5]])
                    S.op(V, lambda e: e.tensor_tensor(out=h3(DqT), in0=h3(E2), in1=mIT, op=ALU.mult), reads=[bW[6], bC], writes=[bW[7]])
                    S.op(V, lambda e: e.tensor_tensor(out=DmT[:], in0=E2[:], in1=beRB[:], op=ALU.mult), reads=[bW[6], bW[1]], writes=[bW[8]])
                    S.op(V, lambda e: e.tensor_tensor(out=h3(DmT), in0=h3(DmT), in1=mST, op=ALU.mult), reads=[bW[8], bC], writes=[bW[8]])
                    pkk, pkkb = bank(); pqk, pqkb = bank()
                    for h in range(4):
                        kT = qkv[:, 4 + h, cs_].bitcast(F32R)
                        qT = qkv[:, h, cs_].bitcast(F32R)
                        S.op("pe", lambda e, h=h, kT=kT: e.matmul(out=pkk[:, h * 128:(h + 1) * 128], lhsT=kT, rhs=kT, start=True, stop=True), reads=[bqkv[4 + h]], writes=[pkkb])
                        S.op("pe", lambda e, h=h, kT=kT, qT=qT: e.matmul(out=pqk[:, h * 128:(h + 1) * 128], lhsT=kT, rhs=qT, start=True, stop=True), reads=[bqkv[4 + h], bqkv[h]], writes=[pqkb])
                    Nn = [W[13], W[14]]; NT = [W[15], W[16]]; RT = [W[17], W[18]]; qkT = W[19]
                    iN, iT_, iR = 13, 15, 17

                    def r32(t):
                        return t[:].bitcast(F32R)
                    S.op(V, lambda e: e.tensor_tensor(out=r32(Nn[0]), in0=pkk[:, :], in1=Dm[:], op=ALU.mult), reads=[pkkb, bW[5]], writes=[bW[13]])
                    S.op(V, lambda e: e.tensor_tensor(out=r32(NT[0]), in0=pkk[:, :], in1=DmT[:], op=ALU.mult), reads=[pkkb, bW[8]], writes=[bW[15]])
                    idB = ident[:, :].unsqueeze(1).to_broadcast([128, 4, 128])
                    S.op(V, lambda e: e.tensor_tensor(out=h3(RT[0]).bitcast(F32R), in0=idB, in1=h3(NT[0]), op=ALU.subtract), reads=[bC, bW[15]], writes=[bW[17]])
                    S.op(V, lambda e: e.tensor_tensor(out=r32(qkT), in0=pqk[:, :], in1=DqT[:], op=ALU.mult), reads=[pqkb, bW[7]], writes=[bW[19]])
                    cur = 0
                    for lev in range(1, 7):
                        nxt = 1 - cur
                        pn, pnb = bank()
                        for h in range(4):
                            hs = slice(h * 128, (h + 1) * 128)
                            S.op("pe", lambda e, hs=hs, pn=pn, cur=cur: e.matmul(out=pn[:, hs], lhsT=r32(NT[cur])[:, hs], rhs=r32(Nn[cur])[:, hs], start=True, stop=True), reads=[bW[iN + cur], bW[iT_ + cur]], writes=[pnb])
                        if lev < 6:
                            pt_, ptb = bank()
                            for h in range(4):
                                hs = slice(h * 128, (h + 1) * 128)
                                S.op("pe", lambda e, hs=hs, pt_=pt_, cur=cur: e.matmul(out=pt_[:, hs], lhsT=r32(Nn[cur])[:, hs], rhs=r32(NT[cur])[:, hs], start=True, stop=True), reads=[bW[iN + cur], bW[iT_ + cur]], writes=[ptb])
                        S.op("act", lambda e, pn=pn, nxt=nxt: e.copy(out=r32(Nn[nxt]), in_=pn[:, :]), reads=[pnb], writes=[bW[iN + nxt]])
                        if lev < 6:
                            S.op("act", lambda e, pt_=pt_, nxt=nxt: e.copy(out=r32(NT[nxt]), in_=pt_[:, :]), reads=[ptb], writes=[bW[iT_ + nxt]])
                        pr, prb = bank()
                        for h in range(4):
                            hs = slice(h * 128, (h + 1) * 128)
                            S.op("pe", lambda e, hs=hs, pr=pr, nxt=nxt, cur=cur: e.matmul(out=pr[:, hs], lhsT=r32(Nn[nxt])[:, hs], rhs=r32(RT[cur])[:, hs], start=True, stop=True), reads=[bW[iN + nxt], bW[iR + cur]], writes=[prb])
                        S.op(V, lambda e, pr=pr, nxt=nxt, cur=cur: e.tensor_tensor(out=r32(RT[nxt]), in0=pr[:, :], in1=RT[cur][:], op=ALU.add), reads=[prb, bW[iR + cur]], writes=[bW[iR + nxt]])
                        cur = nxt
                    TT_ = RT[cur]; bTT = bW[iR + cur]
                    pk, pkb = bank(); pv, pvb = bank()
                    for h in range(4):
                        hs = slice(h * 128, (h + 1) * 128)
                        S.op("pe", lambda e, h=h, hs=hs: e.transpose(out=pk[:, hs], in_=qkv[:, 4 + h, cs_], identity=ident[:]), reads=[bqkv[4 + h], bC], writes=[pkb])
                        S.op("pe", lambda e, h=h, hs=hs: e.transpose(out=pv[:, hs], in_=qkv[:, 8 + h, cs_], identity=ident[:]), reads=[bqkv[8 + h], bC], writes=[pvb])
                    kbg, kdec, vb, usb, wT, vnew = W[20], W[21], W[22], W[9], W[23], W[24]
                    iKBG, iKDEC, iVB, iUSB, iWT, iVN = 20, 21, 22, 9, 23, 24
                    S.op(V, lambda e: e.tensor_tensor(out=h3(kbg).bitcast(F32R), in0=pk[:, :].rearrange("p (a b) -> p a b", a=4), in1=colB(3), op=ALU.mult), reads=[pkb, bgsm], writes=[bW[20]])
                    S.op(V, lambda e: e.tensor_tensor(out=h3(kdec).bitcast(F32R), in0=pk[:, :].rearrange("p (a b) -> p a b", a=4), in1=colB(5), op=ALU.mult), reads=[pkb, bgsm], writes=[bW[21]])
                    S.op(V, lambda e: e.tensor_tensor(out=h3(vb).bitcast(F32R), in0=pv[:, :].rearrange("p (a b) -> p a b", a=4), in1=colB(1), op=ALU.mult), reads=[pvb, bgsm], writes=[bW[22]])
                    pu, pub = bank(); pw_, pwb = bank()
                    for h in range(4):
                        hs = slice(h * 128, (h + 1) * 128)
                        S.op("pe", lambda e, hs=hs: e.matmul(out=pu[:, hs], lhsT=r32(TT_)[:, hs], rhs=r32(vb)[:, hs], start=True, stop=True), reads=[bTT, bW[22]], writes=[pub])
                        S.op("pe", lambda e, hs=hs: e.matmul(out=pw_[:, hs], lhsT=r32(kbg)[:, hs], rhs=r32(TT_)[:, hs], start=True, stop=True), reads=[bTT, bW[20]], writes=[pwb])
                    S.op("act", lambda e: e.copy(out=usb[:], in_=pu[:, :]), reads=[pub], writes=[bW[9]])
                    S.op("act", lambda e: e.copy(out=r32(wT), in_=pw_[:, :]), reads=[pwb], writes=[bW[23]])
                    p1, p1b = bank()
                    for h in range(4):
                        hs = slice(h * 128, (h + 1) * 128)
                        S.op("pe", lambda e, h=h, hs=hs: e.matmul(out=p1[:, hs], lhsT=r32(wT)[:, hs], rhs=Sst[:, h, :], start=True, stop=True), reads=[bW[23], bS], writes=[p1b])
                    S.op(V, lambda e: e.tensor_tensor(out=r32(vnew), in0=usb[:], in1=p1[:, :], op=ALU.subtract), reads=[bW[9], p1b], writes=[bW[24]])
                    po, pob = bank(); p3_, p3b = bank()
                    for h in range(4):
                        hs = slice(h * 128, (h + 1) * 128)
                        S.op("pe", lambda e, h=h, hs=hs: e.matmul(out=po[:, hs], lhsT=Sst[:, h, :], rhs=qe[:, h, cs_], start=True, stop=False), reads=[bS, bqe], writes=[pob])
                        S.op("pe", lambda e, h=h, hs=hs: e.matmul(out=po[:, hs], lhsT=r32(vnew)[:, hs], rhs=r32(qkT)[:, hs], start=False, stop=True), reads=[bW[24], bW[19]], writes=[pob])
                        S.op("pe", lambda e, h=h, hs=hs: e.matmul(out=p3_[:, hs], lhsT=r32(kdec)[:, hs], rhs=r32(vnew)[:, hs], start=True, stop=True), reads=[bW[21], bW[24]], writes=[p3b])
                    S3 = Sst[:].bitcast(F32)
                    S.op(V, lambda e: e.tensor_tensor(out=Sst[:], in0=S3, in1=colB(6), op=ALU.mult), reads=[bS, bgsm], writes=[bS])
                    S.op(V, lambda e: e.tensor_tensor(out=Sst[:], in0=S3, in1=p3_[:, :].rearrange("p (a b) -> p a b", a=4), op=ALU.add), reads=[bS, p3b], writes=[bS])
                    osb, osq, ors = W[10], W[11], W[12]
                    S.op("act", lambda e: e.copy(out=osb[:], in_=po[:, :]), reads=[pob], writes=[bW[10]])
                    S.op("act", lambda e: e.activation(out=osq[:], in_=po[:, :], func=AF.Square), reads=[pob], writes=[bW[11]])
                    ps_, psb = bank()
                    S.op("pe", lambda e: e.matmul(out=ps_[:, :], lhsT=ones[:], rhs=osq[:], start=True, stop=True), reads=[bC, bW[11]], writes=[psb])
                    S.op("act", lambda e: e.activation(out=ors[:], in_=ps_[:, :], func=AF.Sqrt, scale=1.0 / 128.0, bias=epsnm[:, 0:1]), reads=[psb, bC], writes=[bW[12]])
                    S.op(V, lambda e: e.reciprocal(out=ors[:], in_=ors[:]), reads=[bW[12]], writes=[bW[12]])
                    S.op(V, lambda e: e.scalar_tensor_tensor(out=osb[:], in0=osb[:], scalar=nw[:, 0:1], in1=ors[:], op0=ALU.mult, op1=ALU.mult), reads=[bW[10], bW[12], bgp], writes=[bW[10]])
                    S.op(V, lambda e: e.tensor_tensor(out=ydn[:, :, cs_], in0=h3(osb), in1=zs[:, :, cs_], op=ALU.mult), reads=[bW[10], bzs], writes=[bydn])
                S.dma("sp", "st", yloc[blk * 768 + 256:blk * 768 + 768, :].rearrange("(a p) t -> p a t", p=128), ydn[:], reads=[bydn])
                if stage != 1:
                    nc.gpsimd.wait_ge(S.sem["st"], S.cnt["st"])
                    nc.gpsimd.collective_compute("AllGather", ALU.bypass, replica_groups=[[0, 1, 2, 3], [4, 5, 6, 7]], ins=[yloc[blk * 768:(blk + 1) * 768, :].opt()], outs=[yall[blk * 3072:(blk + 1) * 3072, :].opt()]).then_inc(cc_sem)
                    cc_cnt[0] += 1
                    nc.gpsimd.wait_ge(cc_sem, cc_cnt[0])

        S.barrier()
        if stage == 1:
            S.eng["pool"].wait_ge(S.sem["st"], S.cnt["st"])
            i1 = nc.gpsimd.dma_start(out=out_d[:, :].rearrange("(f p) (b t) -> b f p t", p=128, t=512), in_=yloc[:, :].rearrange("(b f p) t -> b f p t", f=6, p=128))
            S.stream("fin"); S.cnt["fin"] += 16; i1.then_inc(S.sem["fin"], 16)
            nc.gpsimd.wait_ge(S.sem["fin"], S.cnt["fin"])
            return nc

        def all_wait_cc():
            for e in ("pool", "sp", "act", "dve", "pe"):
                S.eng[e].wait_ge(cc_sem, cc_cnt[0])
        all_wait_cc()
        if stage == 15:
            i1 = nc.gpsimd.dma_start(out=out_d[:, :].rearrange("(f p) (b t) -> b f p t", p=128, t=512), in_=yall[:, :].rearrange("(b r f p) t -> b r f p t", r=4, f=6, p=128)[:, 1])
            S.stream("fin"); S.cnt["fin"] += 16; i1.then_inc(S.sem["fin"], 16)
            nc.gpsimd.wait_ge(S.sem["fin"], S.cnt["fin"])
            return nc
        core_i = None
        V = "dve"

        with ExitStack() as pb_:
            for i_ in range(2, 4):
                wbuf.append(sbt(pb_, "b_wbuf%d" % i_, [128, 16, 256], BF16)); wbb.append(Buf("wbufB%d" % i_))
            lnin_t = sbt(pb_, "b_lnin", [128, 2, 16]); blnin = Buf()
            S.dma("sp", "cst", lnin_t[:], lnin[:, :, :], writes=[blnin])
            ln1_t = sbt(pb_, "b_ln1", [128, 2, 16]); wr_t = sbt(pb_, "b_wr", [128, 16, 32]); br_t = sbt(pb_, "b_br", [32, 1]); bglu_t = sbt(pb_, "b_bglu", [128, 8])
            bpar = Buf()
            S.dma("sp", "cst", ln1_t[:], ln1[:, :, :], writes=[bpar])
            S.dma("sp", "cst", wr_t[:], wr[:, :, :], writes=[bpar])
            S.dma("sp", "cst", br_t[:], br[:, :], writes=[bpar])
            S.dma("sp", "cst", bglu_t[:], bglu[:, :], writes=[bpar])
            yidx = sbt(pb_, "b_yidx", [128, NBB * 24], I32); byidx = Buf()
            S.dma("sp", "cst", yidx[:], yidx_d[:, :], writes=[byidx])
            xt = sbt(pb_, "b_xt", [128, D]); bxt = Buf()
            xn = sbt(pb_, "b_xn", [128, D]); bxn = Buf()
            st6 = sbt(pb_, "b_st6", [128, 4, 6]); mv = sbt(pb_, "b_mv", [128, 2]); rstd = sbt(pb_, "b_rstd", [128, 1]); bst = Buf()
            hT = sbt(pb_, "b_hT", [128, 16, 512], BF16); bhT = Buf()
            hTf = sbt(pb_, "b_hTf", [128, 16, 512]); bhTf = Buf()
            ysT = sbt(pb_, "b_ysT", [128, 8, 512], BF16); bysT = Buf()
            ydT = sbt(pb_, "b_ydT", [128, 16, 512], BF16); bydT = Buf()
            ygT = sbt(pb_, "b_ygT", [128, 8, 512], BF16); bygT = Buf()
            sgs = sbt(pb_, "b_sgs", [128, 16, 512], BF16); bsgs = Buf()
            sgd = sbt(pb_, "b_sgd", [128, 16, 512], BF16); bsgd = Buf()
            h1b = sbt(pb_, "b_h1b", [128, D], BF16); bh1b = Buf()
            Wb = [sbt(pb_, "b_wk%d" % i, [128, 512]) for i in range(5)]; bWb = [Buf() for _ in range(5)]
            lgT = sbt(pb_, "b_lgT", [32, 512]); blgT = Buf()
            lg = sbt(pb_, "b_lg", [128, 4, 32]); blg = Buf()
            m8 = sbt(pb_, "b_m8", [128, 8]); gt = sbt(pb_, "b_gt", [128, 4, 32]); sm = sbt(pb_, "b_sm", [128, 4]); bsm = Buf(); bgt = Buf()
            for blk in range(NBB):
                c0 = blk * 512
                for tti in range(4):
                    layer_norm_T(pb_, xq[c0 + tti * 128:c0 + (tti + 1) * 128, :], lnin_t, blnin, [hT, hTf], [bhT, bhTf], tti * 128, xt, bxt, xn, bxn, st6, mv, rstd, bst)
                for r in range(4):
                    for ft in range(6):
                        col = blk * 24 + r * 6 + ft
                        if ft < 2:
                            S.idma("yl", ysT[:, 2 * r + ft, :], yall[:, :], yidx[:, col:col + 1], reads=[byidx], writes=[bysT])
                        else:
                            S.idma("yl", ydT[:, 4 * r + ft - 2, :], yall[:, :], yidx[:, col:col + 1], reads=[byidx], writes=[bydT])

                def ep_glu(mi, pb, pbb):
                    S.op("act", lambda e: e.activation(out=Wb[0][:], in_=pb[:, :], func=AF.Sigmoid, bias=bglu_t[:, mi:mi + 1]), reads=[pbb, bpar], writes=[bWb[0]])
                    S.op(V, lambda e: e.tensor_tensor(out=ygT[:, mi, :], in0=ysT[:, mi, :], in1=Wb[0][:], op=ALU.mult), reads=[bysT, bWb[0]], writes=[bygT])
                linear(wglu, 8, 1024, lambda k: ysT[:, k, :], [bysT], 512, ep_glu)

                def ep_gs(mi, pb, pbb):
                    S.op("act", lambda e: e.activation(out=sgs[:, mi, :], in_=pb[:, :], func=AF.Sigmoid), reads=[pbb], writes=[bsgs])

                def ep_gd(mi, pb, pbb):
                    S.op("act", lambda e: e.activation(out=sgd[:, mi, :], in_=pb[:, :], func=AF.Sigmoid), reads=[pbb], writes=[bsgd])
                linear(wg[:, 0:2048], 16, 2048, lambda k: hT[:, k, :], [bhT], 512, ep_gs)
                linear(wg[:, 2048:4096], 16, 2048, lambda k: hT[:, k, :], [bhT], 512, ep_gd)

                def ep_ps(mi, pb, pbb):
                    S.op(V, lambda e: e.tensor_tensor(out=sgs[:, mi, :], in0=sgs[:, mi, :], in1=pb[:, :], op=ALU.mult), reads=[pbb, bsgs], writes=[bsgs])

                def ep_pd(mi, pb, pbb):
                    S.op(V, lambda e: e.tensor_tensor(out=Wb[1][:], in0=sgd[:, mi, :], in1=pb[:, :], op=ALU.mult), reads=[pbb, bsgd], writes=[bWb[1]])
                    S.op(V, lambda e: e.tensor_tensor(out=sgs[:, mi, :], in0=sgs[:, mi, :], in1=Wb[1][:], op=ALU.add), reads=[bsgs, bWb[1]], writes=[bsgs])
                linear(wps, 8, 2048, lambda k: ygT[:, k, :], [bygT], 512, ep_ps)
                linear(wpd, 16, 2048, lambda k: ydT[:, k, :], [bydT], 512, ep_pd)

                def ep_out(mi, pb, pbb):
                    S.op(V, lambda e: e.scalar_tensor_tensor(out=hTf[:, mi, :], in0=hTf[:, mi, :], scalar=ALPHA, in1=pb[:, :], op0=ALU.mult, op1=ALU.add), reads=[pbb, bhTf], writes=[bhTf])
                linear(wout, 16, 2048, lambda k: sgs[:, k, :], [bsgs], 512, ep_out)

                pm, pmb = bank(); pq, pqb = bank()
                for k in range(16):
                    S.op("pe", lambda e, k=k: e.matmul(out=pm[:, :], lhsT=ones[:], rhs=hTf[:, k, :], start=(k == 0), stop=(k == 15)), reads=[bC, bhTf], writes=[pmb])
                for k in range(16):
                    sq = Wb[k % 2]
                    S.op("act", lambda e, k=k, sq=sq: e.activation(out=sq[:], in_=hTf[:, k, :], func=AF.Square), reads=[bhTf], writes=[bWb[k % 2]])
                    S.op("pe", lambda e, k=k, sq=sq: e.matmul(out=pq[:, :], lhsT=ones[:], rhs=sq[:], start=(k == 0), stop=(k == 15)), reads=[bC, bWb[k % 2]], writes=[pqb])
                mu, rs1, tq = Wb[2], Wb[3], Wb[4]
                S.op("act", lambda e: e.activation(out=mu[:], in_=pm[:, :], func=AF.Identity, scale=1.0 / D), reads=[pmb], writes=[bWb[2]])
                S.op(V, lambda e: e.tensor_tensor(out=tq[:], in0=mu[:], in1=mu[:], op=ALU.mult), reads=[bWb[2]], writes=[bWb[4]])
                S.op(V, lambda e: e.scalar_tensor_tensor(out=rs1[:], in0=pq[:, :], scalar=1.0 / D, in1=tq[:], op0=ALU.mult, op1=ALU.subtract), reads=[pqb, bWb[4]], writes=[bWb[3]])
                S.op("act", lambda e: e.activation(out=rs1[:], in_=rs1[:], func=AF.Sqrt, bias=epsln[:, 0:1]), reads=[bWb[3], bC], writes=[bWb[3]])
                S.op(V, lambda e: e.reciprocal(out=rs1[:], in_=rs1[:]), reads=[bWb[3]], writes=[bWb[3]])
                for k in range(16):
                    S.op(V, lambda e, k=k: e.tensor_tensor(out=hTf[:, k, :], in0=hTf[:, k, :], in1=mu[:], op=ALU.subtract), reads=[bhTf, bWb[2]], writes=[bhTf])
                    S.op(V, lambda e, k=k: e.tensor_tensor(out=hTf[:, k, :], in0=hTf[:, k, :], in1=rs1[:], op=ALU.mult), reads=[bhTf, bWb[3]], writes=[bhTf])
                    S.op("act", lambda e, k=k: e.activation(out=hTf[:, k, :], in_=hTf[:, k, :], func=AF.Identity, scale=ln1_t[:, 0, k:k + 1], bias=ln1_t[:, 1, k:k + 1]), reads=[bhTf, bpar], writes=[bhTf])

                pl, plb = bank()
                for k in range(16):
                    S.op("pe", lambda e, k=k: e.matmul(out=pl[0:32, :], lhsT=wr_t[:, k, :], rhs=hTf[:, k, :], start=(k == 0), stop=(k == 15)), reads=[bpar, bhTf], writes=[plb])
                S.op("act", lambda e: e.activation(out=lgT[:], in_=pl[0:32, :], func=AF.Identity, bias=br_t[:, 0:1]), reads=[plb, bpar], writes=[blgT])
                pt_, ptb = bank()
                for tti in range(4):
                    S.op("pe", lambda e, tti=tti: e.transpose(out=pt_[:, tti * 32:(tti + 1) * 32], in_=lgT[:, tti * 128:(tti + 1) * 128], identity=ident[0:32, 0:32]), reads=[blgT, bC], writes=[ptb])
                S.op("act", lambda e: e.copy(out=lg[:], in_=pt_[:, 0:128].rearrange("p (a b) -> p a b", a=4)), reads=[ptb], writes=[blg])
                for tti in range(4):
                    S.op(V, lambda e, tti=tti: e.max(out=m8[:], in_=lg[:, tti, :]), reads=[blg], writes=[bsm])
                    S.op(V, lambda e, tti=tti: e.tensor_scalar(out=gt[:, tti, :], in0=lg[:, tti, :], scalar1=m8[:, 3:4], scalar2=None, op0=ALU.is_ge), reads=[blg, bsm], writes=[bgt])
                    S.op(V, lambda e: e.tensor_scalar(out=sm[:, 0:1], in0=m8[:, 0:1], scalar1=-1.0, scalar2=None, op0=ALU.mult), reads=[bsm], writes=[bsm])
                    S.op("act", lambda e, tti=tti: e.activation(out=lg[:, tti, :], in_=lg[:, tti, :], func=AF.Exp, bias=sm[:, 0:1]), reads=[blg, bsm], writes=[blg])
                    S.op(V, lambda e, tti=tti: e.tensor_tensor(out=gt[:, tti, :], in0=gt[:, tti, :], in1=lg[:, tti, :], op=ALU.mult), reads=[blg, bgt], writes=[bgt])
                    S.op(V, lambda e, tti=tti: e.reduce_sum(out=sm[:, 1:2], in_=gt[:, tti, :], axis=AX.X), reads=[bgt], writes=[bsm])
                    S.op(V, lambda e: e.reciprocal(out=sm[:, 1:2], in_=sm[:, 1:2]), reads=[bsm], writes=[bsm])
                    S.op(V, lambda e, tti=tti: e.tensor_scalar(out=gt[:, tti, :], in0=gt[:, tti, :], scalar1=sm[:, 1:2], scalar2=None, op0=ALU.mult), reads=[bgt, bsm], writes=[bgt])
                S.dma("sp", "st", gate_loc[c0:c0 + 512, :].rearrange("(a p) e -> p a e", p=128), gt[:], reads=[bgt])

                for tti in range(4):
                    for kq in range(4):
                        pb, pbb = bank()
                        for r in range(4):
                            k = kq * 4 + r
                            S.op("pe", lambda e, k=k, r=r, pb=pb, tti=tti: e.transpose(out=pb[:, r * 128:(r + 1) * 128], in_=hTf[:, k, tti * 128:(tti + 1) * 128], identity=ident[:]), reads=[bhTf, bC], writes=[pbb])
                        S.op("act", lambda e, pb=pb, kq=kq: e.copy(out=xn[:, kq * 512:(kq + 1) * 512], in_=pb[:, :]), reads=[pbb], writes=[bxn])
                        S.op(V, lambda e, pb=pb, kq=kq: e.tensor_copy(out=h1b[:, kq * 512:(kq + 1) * 512], in_=pb[:, :]), reads=[pbb], writes=[bh1b])
                    S.dma("sp", "st", h1tok[c0 + tti * 128:c0 + (tti + 1) * 128, :], xn[:], reads=[bxn])
                    S.dma("sp", "st", h1b_loc[c0 + tti * 128:c0 + (tti + 1) * 128, :], h1b[:], reads=[bh1b])
        del wbuf[2:], wbb[2:]
        wrr[0] = 0
        S.barrier()
        if stage == 2:
            nc.gpsimd.wait_ge(S.sem["st"], S.cnt["st"])
            i1 = nc.gpsimd.dma_start(out=out_d[:, :], in_=h1tok[:, :])
            S.stream("fin"); S.cnt["fin"] += 16; i1.then_inc(S.sem["fin"], 16)
            nc.gpsimd.wait_ge(S.sem["fin"], S.cnt["fin"])
            return nc

        nc.gpsimd.wait_ge(S.sem["st"], S.cnt["st"])
        G4 = [[0, 1, 2, 3], [4, 5, 6, 7]]
        for k in range(HC):
            nc.gpsimd.collective_compute("AllGather", ALU.bypass, replica_groups=G4, ins=[h1b_loc[256 * k:256 * (k + 1), :].opt()], outs=[h1b_all[1024 * k:1024 * (k + 1), :].opt()]).then_inc(cc_sem)
            cc_cnt[0] += 1
            nc.gpsimd.wait_ge(cc_sem, cc_cnt[0])
        nc.gpsimd.collective_compute("AllGather", ALU.bypass, replica_groups=G4, ins=[gate_loc.ap().opt()], outs=[gate_all.ap().opt()]).then_inc(cc_sem)
        cc_cnt[0] += 1
        all_wait_cc()

        NTt = TPC // 128
        SL = 4 * CAP
        with ExitStack() as pr_:
            posm = sbt(pr_, "r_posm", [128, 4, NTt, NEX]); Gr = sbt(pr_, "r_G", [128, 4, NTt, NEX]); brt = Buf()
            iotaC = sbt(pr_, "r_iotaC", [128, CAP]); iotap = sbt(pr_, "r_iotap", [128, 3]); sel8 = sbt(pr_, "r_sel8", [8, 8, 128]); brc = Buf()
            S.dma("sp", "cst", iotaC[:], iota_d[:, 0:CAP], writes=[brc])
            S.dma("sp", "cst", iotap[:], iotap_d[:, :], writes=[brc])
            S.dma("sp", "cst", sel8[:], sel8_d[:, :, :], writes=[brc])
            with ExitStack() as p1_:
                Gf = sbt(p1_, "r_Gf", [128, NTt, 32]); bGf = Buf()
                Gp = sbt(p1_, "r_Gp", [128, NTt, NEX, 32]); bGp = Buf()
                esel = sbt(p1_, "r_esel", [128, NEX, 32]); besel = Buf()
                mk = sbt(p1_, "r_mk", [128, NTt, NEX]); cum = sbt(p1_, "r_cum", [128, NTt, NEX]); bmk = Buf()
                S.dma("sp", "cst", esel[:], esel_d[:, :, :], writes=[besel])
                for r in range(4):
                    S.dma("sp", "xl", Gf[:], gate_all[r * TPC:(r + 1) * TPC, :].rearrange("(a p) e -> p a e", p=128), writes=[bGf])
                    S.op(V, lambda e: e.tensor_tensor(out=Gp[:], in0=Gf[:].unsqueeze(2).to_broadcast([128, NTt, NEX, 32]), in1=esel[:].unsqueeze(1).to_broadcast([128, NTt, NEX, 32]), op=ALU.mult), reads=[bGf, besel], writes=[bGp])
                    S.op(V, lambda e, r=r: e.reduce_sum(out=Gr[:, r].rearrange("p a b -> p (a b)"), in_=Gp[:].rearrange("p a b c -> p (a b) c"), axis=AX.X), reads=[bGp], writes=[brt])
                    S.op(V, lambda e, r=r: e.tensor_scalar(out=mk[:], in0=Gr[:, r], scalar1=0.0, scalar2=None, op0=ALU.is_gt), reads=[brt], writes=[bmk])
                    S.op(V, lambda e: e.memset(cum[:, 0, :], 0.0), reads=[bmk], writes=[bmk])
                    for i in range(1, NTt):
                        S.op(V, lambda e, i=i: e.tensor_tensor(out=cum[:, i, :], in0=cum[:, i - 1, :], in1=mk[:, i - 1, :], op=ALU.add), reads=[bmk], writes=[bmk])
                    for i0 in range(0, NTt, 32):
                        pb, pbb = bank()
                        for i in range(i0, min(NTt, i0 + 32)):
                            oc = (i - i0) * NEX
                            S.op("pe", lambda e, i=i, oc=oc, pb=pb: e.matmul(out=pb[:, oc:oc + NEX], lhsT=masks[:, 2, :], rhs=mk[:, i, :], start=True, stop=False), reads=[bC, bmk], writes=[pbb])
                            S.op("pe", lambda e, i=i, oc=oc, pb=pb: e.matmul(out=pb[:, oc:oc + NEX], lhsT=ones[:], rhs=cum[:, i, :], start=False, stop=True), reads=[bC, bmk], writes=[pbb])
                        n_ = min(NTt, i0 + 32) - i0
                        S.op(V, lambda e, i0=i0, n_=n_, pb=pb, r=r: e.scalar_tensor_tensor(out=posm[:, r, i0:i0 + n_, :], in0=pb[:, 0:n_ * NEX].rearrange("p (a b) -> p a b", b=NEX), scalar=1.0, in1=mk[:, i0:i0 + n_, :], op0=ALU.add, op1=ALU.mult), reads=[pbb, bmk], writes=[brt])
                        S.op(V, lambda e, i0=i0, n_=n_, r=r: e.tensor_scalar(out=posm[:, r, i0:i0 + n_, :], in0=posm[:, r, i0:i0 + n_, :], scalar1=-1.0, scalar2=None, op0=ALU.add), reads=[brt], writes=[brt])
            S.barrier()

            with ExitStack() as p2_:
                H = sbt(p2_, "g_H", [128, NTt, D], BF16); bH = Buf()
                Sels = [sbt(p2_, "g_Sel%d" % i_, [128, NTt, CAP], BF16) for i_ in range(2)]; bSels = [Buf(), Buf()]
                XTss = [sbt(p2_, "g_XTs%d" % i_, [128, 16, CAP], BF16) for i_ in range(2)]; bXTss = [Buf(), Buf()]
                g_rr = 0
                for r in range(4):
                    for k in range(HC):
                        S.dma("sp", "xl", H[:, 2 * k:2 * k + 2, :], h1b_all[(k * 4 + r) * 256:(k * 4 + r + 1) * 256, :].rearrange("(a p) d -> p a d", p=128), writes=[bH])
                    for ex in range(NEX):
                        Sel, bSel, XTs, bXTs = Sels[g_rr % 2], bSels[g_rr % 2], XTss[g_rr % 2], bXTss[g_rr % 2]
                        g_rr += 1
                        for i in range(NTt):
                            S.op(V, lambda e, i=i: e.tensor_scalar(out=Sel[:, i, :], in0=iotaC[:], scalar1=posm[:, r, i, ex:ex + 1], scalar2=None, op0=ALU.is_equal), reads=[brc, brt], writes=[bSel])
                        for kc in range(16):
                            pb, pbb = bank()
                            for i in range(NTt):
                                S.op("pe", lambda e, i=i, kc=kc, pb=pb: e.matmul(out=pb[:, 0:CAP], lhsT=H[:, i, kc * 128:(kc + 1) * 128], rhs=Sel[:, i, :], start=(i == 0), stop=(i == NTt - 1)), reads=[bH, bSel], writes=[pbb])
                            S.op("act", lambda e, kc=kc, pb=pb: e.copy(out=XTs[:, kc, :], in_=pb[:, 0:CAP]), reads=[pbb], writes=[bXTs])
                        S.dma("sp", "st", XTd[ex * D:(ex + 1) * D, r * CAP:(r + 1) * CAP].rearrange("(k p) c -> p k c", p=128), XTs[:], reads=[bXTs])
            S.barrier()
            nc.sync.wait_ge(S.sem["st"], S.cnt["st"])

            with ExitStack() as pm_:
                for i_ in range(2, 4):
                    wbuf.append(sbt(pm_, "m_wbuf%d" % i_, [128, 16, 256], BF16)); wbb.append(Buf("wbufM%d" % i_))
                XT = sbt(pm_, "m_XT", [128, 16, SL], BF16); bXT = Buf()
                AT = sbt(pm_, "m_AT", [128, 16, SL], BF16); bAT = Buf()
                w2 = [sbt(pm_, "m_w2%d" % i, [128, 16, 512], BF16) for i in range(2)]; bw2 = [Buf(), Buf()]
                w2rr = [0]
                Wm = [sbt(pm_, "m_wk%d" % i, [128, 512]) for i in range(3)]; bWm = [Buf() for _ in range(3)]
                Yst = [sbt(pm_, "m_Yst%d" % i, [128, 512], BF16) for i in range(2)]; bYst = [Buf(), Buf()]
                bdB = sbt(pm_, "m_bdB", [128, D]); bbdB = Buf()
                bgu_t = sbt(pm_, "m_bgu", [128, NEX, 32]); bbgu = Buf()
                S.dma("sp", "cst", bgu_t[:], bgu[:, :, :], writes=[bbgu])
                yrr = 0
                for ex in range(NEX):
                    S.dma("sp", "xl", XT[:], XTd[ex * D:(ex + 1) * D, :].rearrange("(k p) c -> p k c", p=128), writes=[bXT])
                    S.dma("sp", "bl", bdB[:], bdn[ex:ex + 1, :].to_broadcast([128, D]), writes=[bbdB])
                    for ft in range(16):
                        i = wrr[0] % len(wbuf); wrr[0] = (i + 1) % len(wbuf)
                        S.dma("pool", "wl%d" % i, wbuf[i][:, :, 0:128], wgu[ex, :, ft * 128:(ft + 1) * 128].rearrange("(kc p) m -> p kc m", p=128), writes=[wbb[i]])
                        S.dma("pool", "wl%d" % i, wbuf[i][:, :, 128:256], wgu[ex, :, 2048 + ft * 128:2048 + (ft + 1) * 128].rearrange("(kc p) m -> p kc m", p=128), writes=[wbb[i]])
                        for nh in range(SL // 512):
                            ns = slice(nh * 512, (nh + 1) * 512)
                            pg, pgb = bank(); pu, pub = bank()
                            for k in range(16):
                                S.op("pe", lambda e, k=k, i=i, pg=pg: e.matmul(out=pg[:, :], lhsT=wbuf[i][:, k, 0:128], rhs=XT[:, k, ns], start=(k == 0), stop=(k == 15)), reads=[wbb[i], bXT], writes=[pgb])
                            for k in range(16):
                                S.op("pe", lambda e, k=k, i=i, pu=pu: e.matmul(out=pu[:, :], lhsT=wbuf[i][:, k, 128:256], rhs=XT[:, k, ns], start=(k == 0), stop=(k == 15)), reads=[wbb[i], bXT], writes=[pub])
                            g_, s_, u_ = Wm
                            S.op(V, lambda e, pg=pg: e.tensor_scalar(out=g_[:], in0=pg[:, :], scalar1=bgu_t[:, ex, ft:ft + 1], scalar2=7.0, op0=ALU.add, op1=ALU.min), reads=[pgb, bbgu], writes=[bWm[0]])
                            S.op("act", lambda e: e.activation(out=s_[:], in_=g_[:], func=AF.Sigmoid, scale=1.702), reads=[bWm[0]], writes=[bWm[1]])
                            S.op(V, lambda e, pu=pu: e.tensor_scalar(out=u_[:], in0=pu[:, :], scalar1=bgu_t[:, ex, 16 + ft:17 + ft], scalar2=7.0, op0=ALU.add, op1=ALU.min), reads=[pub, bbgu], writes=[bWm[2]])
                            S.op(V, lambda e: e.tensor_scalar(out=u_[:], in0=u_[:], scalar1=-7.0, scalar2=1.0, op0=ALU.max, op1=ALU.add), reads=[bWm[2]], writes=[bWm[2]])
                            S.op(V, lambda e: e.tensor_tensor(out=g_[:], in0=g_[:], in1=s_[:], op=ALU.mult), reads=[bWm[0], bWm[1]], writes=[bWm[0]])
                            S.op(V, lambda e: e.tensor_tensor(out=AT[:, ft, ns], in0=g_[:], in1=u_[:], op=ALU.mult), reads=[bWm[0], bWm[2]], writes=[bAT])
                    for dq in range(4):
                        j = w2rr[0]; w2rr[0] = 1 - j
                        S.dma("pool", "w2l%d" % j, w2[j][:], wdn[ex, :, dq * 512:(dq + 1) * 512].rearrange("(kc p) m -> p kc m", p=128), writes=[bw2[j]])
                        ds_ = slice(dq * 512, (dq + 1) * 512)
                        for sti in range(SL // 128):
                            pd, pdb = bank()
                            for k in range(16):
                                S.op("pe", lambda e, k=k, j=j, pd=pd, sti=sti: e.matmul(out=pd[:, :], lhsT=AT[:, k, sti * 128:(sti + 1) * 128], rhs=w2[j][:, k, :], start=(k == 0), stop=(k == 15)), reads=[bw2[j], bAT], writes=[pdb])
                            yb = yrr % 2; yrr += 1
                            S.op(V, lambda e, pd=pd, yb=yb: e.tensor_tensor(out=Yst[yb][:], in0=pd[:, :], in1=bdB[:, ds_], op=ALU.add), reads=[pdb, bbdB], writes=[bYst[yb]])
                            S.dma("sp", "st", Yd[ex * SL + sti * 128:ex * SL + (sti + 1) * 128, ds_], Yst[yb][:], reads=[bYst[yb]])
            del wbuf[2:], wbb[2:]
            wrr[0] = 0
            S.barrier()
            nc.sync.wait_ge(S.sem["st"], S.cnt["st"])

            with ExitStack() as p4_:
                SG = sbt(p4_, "s_SG", [128, NEX, 3, TPC], BF16); bSG = Buf()
                Yts = [sbt(p4_, "s_Yt%d" % i_, [128, NEX, 3, 512], BF16) for i_ in range(2)]; bYts = [Buf(), Buf()]
                y_rr = 0
                posT = sbt(p4_, "s_posT", [8, TPC]); gT = sbt(p4_, "s_gT", [8, TPC]); bpT = Buf()
                gsb = sbt(p4_, "s_gsb", [128, 512]); bgsb = Buf()
                ost = [sbt(p4_, "s_ost%d" % i, [128, 512]) for i in range(2)]; bost = [Buf(), Buf()]
                orr = 0
                for r in range(4):
                    for i0 in range(0, NTt, 4):
                        pp_, ppb = bank(); pg_, pgb_ = bank()
                        for i in range(i0, min(NTt, i0 + 4)):
                            oc = (i - i0) * 128
                            S.op("pe", lambda e, i=i, oc=oc, pp_=pp_: e.transpose(out=pp_[0:8, oc:oc + 128], in_=posm[:, r, i, :], identity=ident[:]), reads=[brt, bC], writes=[ppb])
                            S.op("pe", lambda e, i=i, oc=oc, pg_=pg_: e.transpose(out=pg_[0:8, oc:oc + 128], in_=Gr[:, r, i, :], identity=ident[:]), reads=[brt, bC], writes=[pgb_])
                        n_ = (min(NTt, i0 + 4) - i0) * 128
                        S.op("act", lambda e, i0=i0, n_=n_, pp_=pp_: e.copy(out=posT[:, i0 * 128:i0 * 128 + n_], in_=pp_[0:8, 0:n_]), reads=[ppb], writes=[bpT])
                        S.op("act", lambda e, i0=i0, n_=n_, pg_=pg_: e.copy(out=gT[:, i0 * 128:i0 * 128 + n_], in_=pg_[0:8, 0:n_]), reads=[pgb_], writes=[bpT])
                    for ex in range(NEX):
                        for cb in range(TPC // 512):
                            cs_ = slice(cb * 512, (cb + 1) * 512)
                            pp_, ppb = bank(); pg_, pgb_ = bank()
                            S.op("pe", lambda e, pp_=pp_: e.matmul(out=pp_[:, :], lhsT=sel8[:, ex, :], rhs=posT[:, cs_], start=True, stop=True), reads=[brc, bpT], writes=[ppb])
                            S.op("pe", lambda e, pg_=pg_: e.matmul(out=pg_[:, :], lhsT=sel8[:, ex, :], rhs=gT[:, cs_], start=True, stop=True), reads=[brc, bpT], writes=[pgb_])
                            S.op("act", lambda e, pg_=pg_: e.copy(out=gsb[:], in_=pg_[:, :]), reads=[pgb_], writes=[bgsb])
                            for cc in range(3):
                                S.op(V, lambda e, cc=cc, pp_=pp_: e.scalar_tensor_tensor(out=SG[:, ex, cc, cs_], in0=pp_[:, :], scalar=iotap[:, cc:cc + 1], in1=gsb[:], op0=ALU.is_equal, op1=ALU.mult), reads=[ppb, brc, bgsb], writes=[bSG])
                    for dq in range(4):
                        ds_ = slice(dq * 512, (dq + 1) * 512)
                        Yt, bYt = Yts[y_rr % 2], bYts[y_rr % 2]
                        y_rr += 1
                        for ex in range(NEX):
                            S.dma("sp", "yt%d" % (y_rr % 2), Yt[:, ex, :, :], Yd[ex * SL + r * CAP:ex * SL + (r + 1) * CAP, ds_].rearrange("(a p) d -> p a d", p=128), writes=[bYt])
                        for i in range(NTt):
                            po_, pob_ = bank()
                            n = 0
                            for ex in range(NEX):
                                for cc in range(3):
                                    S.op("pe", lambda e, ex=ex, cc=cc, i=i, n=n, po_=po_: e.matmul(out=po_[:, :], lhsT=SG[:, ex, cc, i * 128:(i + 1) * 128], rhs=Yt[:, ex, cc, :], start=(n == 0), stop=(n == NEX * 3 - 1)), reads=[bSG, bYt], writes=[pob_])
                                    n += 1
                            ob = orr % 2; orr += 1
                            S.op("act", lambda e, po_=po_, ob=ob: e.copy(out=ost[ob][:], in_=po_[:, :]), reads=[pob_], writes=[bost[ob]])
                            l = i * 128
                            row = ((l // RPC) * 4 + r) * RPC + (l % RPC)
                            S.dma("sp", "st", dense[row:row + 128, ds_], ost[ob][:], reads=[bost[ob]])
        S.barrier()
        nc.gpsimd.wait_ge(S.sem["st"], S.cnt["st"])
        for k in range(NRS):
            nc.gpsimd.collective_compute("ReduceScatter", ALU.add, replica_groups=[[0, 1, 2, 3], [4, 5, 6, 7]], ins=[dense[k * 4 * RPC:(k + 1) * 4 * RPC, :].opt()], outs=[ffn[k * RPC:(k + 1) * RPC, :].opt()]).then_inc(cc_sem)
            cc_cnt[0] += 1
            nc.gpsimd.wait_ge(cc_sem, cc_cnt[0])
        all_wait_cc()

        with ExitStack() as pf:
            g2 = sbt(pf, "f_g2", [128, D]); b2 = sbt(pf, "f_b2", [128, D]); bg2 = Buf()
            S.dma("sp", "cst", g2[:], ln2[0:1, :].to_broadcast([128, D]), writes=[bg2])
            S.dma("sp", "cst", b2[:], ln2[1:2, :].to_broadcast([128, D]), writes=[bg2])
            has = [sbt(pf, "f_ha%d" % i_, [128, D]) for i_ in range(2)]; fas = [sbt(pf, "f_fa%d" % i_, [128, D]) for i_ in range(2)]
            bhas = [Buf(), Buf()]; bfas = [Buf(), Buf()]
            st6 = sbt(pf, "f_st6", [128, 4, 6]); mv = sbt(pf, "f_mv", [128, 2]); rstd = sbt(pf, "f_rstd", [128, 1]); bst = Buf()
            fin_toks = []
            for tti in range(TPC // 128):
                rows = slice(tti * 128, (tti + 1) * 128)
                ha, fa, bha, bfa = has[tti % 2], fas[tti % 2], bhas[tti % 2], bfas[tti % 2]
                S.dma("sp", "fl%d" % (tti % 2), ha[:], h1tok[rows, :], writes=[bha])
                S.dma("sp", "fl%d" % (tti % 2), fa[:], ffn[rows, :], writes=[bfa])
                S.op(V, lambda e: e.scalar_tensor_tensor(out=ha[:], in0=ha[:], scalar=ALPHA, in1=fa[:], op0=ALU.mult, op1=ALU.add), reads=[bha, bfa], writes=[bha])
                for q in range(4):
                    S.op(V, lambda e, q=q: e.bn_stats(out=st6[:, q, :], in_=ha[:, q * 512:(q + 1) * 512]), reads=[bha], writes=[bst])
                S.op(V, lambda e: e.bn_aggr(out=mv[:], in_=st6[:].rearrange("p a b -> p (a b)")), reads=[bst], writes=[bst])
                S.op("act", lambda e: e.activation(out=rstd[:], in_=mv[:, 1:2], func=AF.Sqrt, bias=epsln[:, 0:1]), reads=[bst, bC], writes=[bst])
                S.op(V, lambda e: e.reciprocal(out=rstd[:], in_=rstd[:]), reads=[bst], writes=[bst])
                S.op(V, lambda e: e.tensor_scalar(out=fa[:], in0=ha[:], scalar1=mv[:, 0:1], scalar2=rstd[:, 0:1], op0=ALU.subtract, op1=ALU.mult), reads=[bha, bst], writes=[bfa])
                S.op(V, lambda e: e.tensor_tensor(out=fa[:], in0=fa[:], in1=g2[:], op=ALU.mult), reads=[bfa, bg2], writes=[bfa])
                S.op(V, lambda e: e.tensor_tensor(out=fa[:], in0=fa[:], in1=b2[:], op=ALU.add), reads=[bfa, bg2], writes=[bfa])
                fin_toks.append(S.dma("sp", "fin", out_d[rows, :], fa[:], reads=[bfa]))
            S.eng["sp"].wait_ge(S.sem["fin"], S.cnt["fin"])
    return nc


def _consts():
    c = np.arange(128)
    ident = np.eye(128, dtype=np.float32)
    m0 = (c[:, None] > c[None, :]).astype(np.float32)
    m1 = (c[None, :] >= c[:, None]).astype(np.float32)
    m2 = (c[None, :] > c[:, None]).astype(np.float32)
    masks = np.stack([m0, m1, m2], axis=1)
    iota = np.broadcast_to(np.arange(384, dtype=np.float32)[None, :], (128, 384)).copy()
    iotap = (np.arange(128, dtype=np.float32)[:, None] + 128.0 * np.arange(3, dtype=np.float32)[None, :]).copy()
    sel8 = np.zeros((8, 8, 128), np.float32)
    for h in range(8):
        sel8[h, h, :] = 1.0
    sel4 = np.zeros((4, 4, 128), np.float32)
    for h in range(4):
        sel4[h, h, :] = 1.0
    return dict(ident=ident, masks=masks, iota=iota, sel4=sel4, iotap=iotap, sel8=sel8)


def _core_inputs(inp, core, SEQ, stage):
    b, j = core // 4, core % 4
    f = np.float32
    m = {}
    m["x"] = np.ascontiguousarray(inp["x"][b, :SEQ])
    m["lnin"] = np.ascontiguousarray(np.stack([inp["ln_in_g"].reshape(16, 128).T, inp["ln_in_b"].reshape(16, 128).T], axis=1)).astype(f)
    w_in = inp["w_in"][0]
    o = 0
    u_c = w_in[:, 256 * j:256 * (j + 1)]
    base = 1024
    q_c = w_in[:, base + 512 * j: base + 512 * (j + 1)]
    k_c = w_in[:, base + 2048 + 512 * j: base + 2048 + 512 * (j + 1)]
    v_c = w_in[:, base + 4096 + 512 * j: base + 4096 + 512 * (j + 1)]
    z_c = w_in[:, base + 6144 + 512 * j: base + 6144 + 512 * (j + 1)]
    a_c = w_in[:, base + 8192 + 4 * j: base + 8192 + 4 * (j + 1)]
    b_c = w_in[:, base + 8192 + 16 + 4 * j: base + 8192 + 16 + 4 * (j + 1)]
    m["wA"] = np.ascontiguousarray(np.concatenate([u_c, q_c, k_c, v_c, z_c], axis=1))
    m["wab"] = np.ascontiguousarray(np.concatenate([a_c, b_c], axis=1))
    g0 = 16 * j

    def pairlay(a):
        sh = a.shape[2:]
        a = a.reshape((8, 2, 64) + sh)
        a = np.moveaxis(a, 0, 2)
        return np.ascontiguousarray(a.reshape((128, 8) + sh))
    lam_re = inp["lam_re"][0][g0:g0 + 16]
    lam_im = inp["lam_im"][0][g0:g0 + 16]
    lstep = np.broadcast_to(inp["log_step"][0][g0:g0 + 16][:, None], (16, 64))
    m["s5p"] = pairlay(np.stack([lam_re, lam_im, lstep], axis=-1)).astype(f)
    m["s5b"] = pairlay(np.stack([inp["ssm_b_re"][0][g0:g0 + 16], inp["ssm_b_im"][0][g0:g0 + 16]], axis=2)).astype(f)
    cre = np.transpose(inp["ssm_c_re"][0][g0:g0 + 16], (0, 2, 1))
    cim = np.transpose(inp["ssm_c_im"][0][g0:g0 + 16], (0, 2, 1))
    m["s5c"] = pairlay(np.stack([cre, cim], axis=2)).astype(f)
    m["s5d"] = np.ascontiguousarray(inp["ssm_d"][0][g0:g0 + 16].reshape(2, 128).T).astype(f)
    cwf = inp["conv_w"][0][:, 0, :]
    tiles = []
    for part in range(3):
        for h in range(4):
            c0 = part * 2048 + (4 * j + h) * 128
            tiles.append(cwf[:, c0:c0 + 128].T)
    m["convw"] = np.ascontiguousarray(np.stack(tiles, axis=1)).astype(f)
    m["hpar"] = np.ascontiguousarray(np.stack([inp["a_log"][0][4 * j:4 * j + 4], inp["dt_bias"][0][4 * j:4 * j + 4]], axis=1)).astype(f)
    m["dnw"] = np.ascontiguousarray(inp["dn_norm_w"][0].reshape(128, 1)).astype(f)
    m.update(_consts())
    if stage == 15:
        return m
    if stage >= 2:
        NBLK = SEQ // 512
        TPC = SEQ // 4
        NBB = TPC // 512
        i = j
        m["xq"] = np.ascontiguousarray(inp["x"][b, i * TPC:(i + 1) * TPC])
        yidx = np.zeros((128, NBB * 24), np.int32)
        p = np.arange(128, dtype=np.int64)
        for blk in range(NBB):
            gb = i * NBB + blk
            for r in range(4):
                for ft in range(6):
                    yidx[:, blk * 24 + r * 6 + ft] = ((gb * 4 + r) * 6 + ft) * 128 + p
        m["yidx"] = yidx
        m["wg"] = np.ascontiguousarray(w_in[:, 9248:13344])
        m["wglu"] = np.ascontiguousarray(inp["w_glu"][0])
        m["bglu"] = np.ascontiguousarray(inp["b_glu"][0].reshape(8, 128).T).astype(f)
        m["wps"] = np.ascontiguousarray(inp["w_proj_ssm"][0])
        m["wpd"] = np.ascontiguousarray(inp["w_proj_dn"][0])
        m["wout"] = np.ascontiguousarray(inp["w_out"][0])
        m["ln1"] = np.ascontiguousarray(np.stack([inp["ln1_g"][0].reshape(16, 128).T, inp["ln1_b"][0].reshape(16, 128).T], axis=1)).astype(f)
        m["wr"] = np.ascontiguousarray(np.transpose(inp["w_router"][0].reshape(16, 128, 32), (1, 0, 2))).astype(f)
        m["br"] = np.ascontiguousarray(inp["b_router"][0].reshape(32, 1)).astype(f)
    if stage >= 9:
        e0 = 8 * j
        m["wgu"] = np.ascontiguousarray(inp["w_gate_up"][0][e0:e0 + 8])
        m["bgu"] = np.ascontiguousarray(np.transpose(inp["b_gate_up"][0][e0:e0 + 8].reshape(8, 32, 128), (2, 0, 1))).astype(f)
        m["wdn"] = np.ascontiguousarray(inp["w_down"][0][e0:e0 + 8])
        m["bdn"] = np.ascontiguousarray(inp["b_down"][0][e0:e0 + 8]).astype(f)
        m["ln2"] = np.ascontiguousarray(np.stack([inp["ln2_g"][0], inp["ln2_b"][0]], axis=0)).astype(f)
        es = np.zeros((128, 8, 32), f)
        for jj in range(8):
            es[:, jj, e0 + jj] = 1.0
        m["esel"] = es
    return m


def _run(inputs, SEQ, stage):
    nc = build(SEQ, stage)
    in_maps = [_core_inputs(inputs, c, SEQ, stage) for c in range(8)]
    res = run_bass_kernel_spmd(nc, in_maps, core_ids=list(range(8)))
    return [r["out"] for r in res.results]


def kernel(**inputs):
    SEQ = inputs["x"].shape[1]
    outs = _run(inputs, SEQ, 9)
    TPC = SEQ // 4
    out = np.zeros((2, SEQ, D), np.float32)
    for c in range(8):
        b, i = c // 4, c % 4
        out[b, i * TPC:(i + 1) * TPC] = outs[c]
    return out
```
